# Optimizing a Trainium2 kernel written in Bass

```python
import math
import jax
import jax.numpy as jnp
from jax import lax
import numpy as np

D_MODEL = 2048
BATCH = 4
SEQ = 2048
DEPTH = 2

MEM_LEN = 256
MIX_W = D_MODEL
LRU_W = MIX_W // 4
LRU_BLOCKS = 4
LRU_CONV = 4
LRU_C = 8.0
ML_W = 3 * MIX_W // 8
ML_HEADS = 4
ML_DH = ML_W // ML_HEADS
ML_QKV_BLOCK = 4
ML_CONV = 4
ML_CHUNK = 128
MO_W = MIX_W - LRU_W - ML_W
MO_HEADS = 6
MO_DH = MO_W // MO_HEADS
MOBA_BLOCK = 256
MOBA_TOPK = 3
MOBA_Q_CHUNK = 16
XA_HEADS = 4
XA_DH = 128
XA_W = XA_HEADS * XA_DH

RMS_EPS = 1e-6
LN_EPS = 1e-5
NEG_INF = -1e30

SPLIT_SIZES = (LRU_W, LRU_W, ML_W, ML_W, ML_W, ML_HEADS, ML_HEADS, MO_W, MO_W, MO_W, MO_W)
IN_COLS = sum(SPLIT_SIZES)
SPLIT_POINTS = tuple(int(v) for v in np.cumsum(SPLIT_SIZES)[:-1])

kernel_name = "hybrid_lru_mlstm_moba_block"


def rms_norm(x, g):
    x32 = x.astype(jnp.float32)
    y = x32 * lax.rsqrt(jnp.mean(x32 * x32, axis=-1, keepdims=True) + RMS_EPS)
    return (y * g.astype(jnp.float32)).astype(x.dtype)


def causal_conv(x, w, b):
    K = w.shape[0]
    T = x.shape[1]
    xp = jnp.pad(x, ((0, 0), (K - 1, 0), (0, 0)))
    return sum(xp[:, j:j + T] * w[j] for j in range(K)) + b


def block_diag(x, w):
    G, bi, bo = w.shape
    xs = x.reshape(x.shape[:-1] + (G, bi))
    return jnp.einsum('...gi,gio->...go', xs, w).reshape(x.shape[:-1] + (G * bo,))


def alibi_slopes(n):
    def pow2(m):
        start = 2.0 ** (-8.0 / m)
        return [start ** (i + 1) for i in range(m)]
    if math.log2(n).is_integer():
        s = pow2(n)
    else:
        c = 2 ** int(math.floor(math.log2(n)))
        s = pow2(c) + pow2(2 * c)[0::2][:n - c]
    return jnp.array(s, dtype=jnp.float32)


def rg_lru_branch(xb, zb, conv_w, conv_b, wa, ba, wx, bx, lam):
    xc = causal_conv(xb, conv_w, conv_b).astype(jnp.float32)
    r = jax.nn.sigmoid(block_diag(xc, wa) + ba)
    i = jax.nn.sigmoid(block_diag(xc, wx) + bx)
    log_a = -LRU_C * r * jax.nn.softplus(-lam.astype(jnp.float32))
    a = jnp.exp(log_a)
    u = jnp.sqrt(-jnp.expm1(2.0 * log_a)) * (i * xc)

    def combine(left, right):
        a1, b1 = left
        a2, b2 = right
        return a1 * a2, a2 * b1 + b2

    _, h = lax.associative_scan(combine, (a, u), axis=1)
    return (h * jax.nn.silu(zb.astype(jnp.float32))).astype(xb.dtype)


def mlstm_chunkwise(q, k, v, ig, lf):
    B, H, T, dh = q.shape
    L = ML_CHUNK
    nc = T // L
    q, k, v = (t.reshape(B, H, nc, L, dh) for t in (q, k, v))
    ig = ig.reshape(B, H, nc, L)
    lf = lf.reshape(B, H, nc, L)
    b = jnp.cumsum(lf, axis=-1)
    g = b[..., -1]
    a = g[..., None] - b + ig
    m_loc = jnp.max(a, axis=-1)
    w = jnp.exp(a - m_loc[..., None])
    c_loc = jnp.einsum('bhcl,bhcld,bhcle->bhcde', w, k, v)
    n_loc = jnp.einsum('bhcl,bhcld->bhcd', w, k)

    def step(carry, inp):
        c_st, n_st, m_st = carry
        c_l, n_l, m_l, g_c = inp
        m_new = jnp.maximum(g_c + m_st, m_l)
        s_old = jnp.exp(g_c + m_st - m_new)
        s_new = jnp.exp(m_l - m_new)
        c_new = s_old[..., None, None] * c_st + s_new[..., None, None] * c_l
        n_new = s_old[..., None] * n_st + s_new[..., None] * n_l
        return (c_new, n_new, m_new), (c_st, n_st, m_st)

    init = (jnp.zeros((B, H, dh, dh), jnp.float32), jnp.zeros((B, H, dh), jnp.float32),
            jnp.zeros((B, H), jnp.float32))
    xs = tuple(jnp.moveaxis(t, 2, 0) for t in (c_loc, n_loc, m_loc, g))
    _, (c_prev, n_prev, m_prev) = lax.scan(step, init, xs)
    c_prev = jnp.moveaxis(c_prev, 0, 2)
    n_prev = jnp.moveaxis(n_prev, 0, 2)
    m_prev = jnp.moveaxis(m_prev, 0, 2)
    causal = jnp.tril(jnp.ones((L, L), dtype=bool))
    d = jnp.where(causal, b[..., :, None] - b[..., None, :] + ig[..., None, :], -jnp.inf)
    inter = b + m_prev[..., None]
    m = jnp.maximum(inter, jnp.max(d, axis=-1))
    s = jnp.einsum('bhcjd,bhcsd->bhcjs', q, k) * jnp.exp(d - m[..., None])
    s_int = jnp.exp(inter - m)
    num = (jnp.einsum('bhcjs,bhcsd->bhcjd', s, v)
           + s_int[..., None] * jnp.einsum('bhcjd,bhcde->bhcje', q, c_prev))
    den = jnp.sum(s, axis=-1) + s_int * jnp.einsum('bhcjd,bhcd->bhcj', q, n_prev)
    h = num / jnp.maximum(jnp.abs(den), jnp.exp(-m))[..., None]
    return h.reshape(B, H, T, dh)


def mlstm_branch(u, o, z, ig_pre, fg_pre, conv_w, conv_b, wq, wk, wv, bi, bf, norm_g):
    B, T, _ = u.shape
    uc = jax.nn.silu(causal_conv(u, conv_w, conv_b))
    q = block_diag(uc, wq)
    k = block_diag(uc, wk) * (ML_DH ** -0.5)
    v = block_diag(u, wv)

    def heads(t):
        return t.astype(jnp.float32).reshape(B, T, ML_HEADS, ML_DH).transpose(0, 2, 1, 3)

    ig = (ig_pre.astype(jnp.float32) + bi).transpose(0, 2, 1)
    lf = jax.nn.log_sigmoid(fg_pre.astype(jnp.float32) + bf).transpose(0, 2, 1)
    h = mlstm_chunkwise(heads(q), heads(k), heads(v), ig, lf).transpose(0, 2, 1, 3)
    h = jax.nn.sigmoid(o.astype(jnp.float32)).reshape(B, T, ML_HEADS, ML_DH) * h
    mu = jnp.mean(h, axis=-1, keepdims=True)
    var = jnp.mean(jnp.square(h - mu), axis=-1, keepdims=True)
    hn = (h - mu) * lax.rsqrt(var + LN_EPS) * norm_g.astype(jnp.float32).reshape(ML_HEADS, ML_DH)
    return (hn.reshape(B, T, ML_W) * jax.nn.silu(z.astype(jnp.float32))).astype(u.dtype)


def moba_attention(q, k, v):
    B, H, T, dh = q.shape
    nb = -(-T // MOBA_BLOCK)
    tp = nb * MOBA_BLOCK
    pad = ((0, 0), (0, 0), (0, tp - T), (0, 0))
    q, k, v = (jnp.pad(t, pad) for t in (q, k, v))
    kb = k.reshape(B, H, nb, MOBA_BLOCK, dh)
    vb = v.reshape(B, H, nb, MOBA_BLOCK, dh)
    kmean = jnp.mean(kb.astype(jnp.float32), axis=3)
    n_sel = min(MOBA_TOPK, nb - 1)
    slopes = alibi_slopes(H)
    scale = dh ** -0.5
    nq = tp // MOBA_Q_CHUNK
    qc = jnp.moveaxis(q.reshape(B, H, nq, MOBA_Q_CHUNK, dh), 2, 0)
    bidx = jnp.arange(B)[:, None, None, None]
    hidx = jnp.arange(H)[None, :, None, None]
    s_local = jnp.arange(MOBA_BLOCK)

    def one_chunk(args):
        qch, ci = args
        qch = qch.astype(jnp.float32)
        t = ci * MOBA_Q_CHUNK + jnp.arange(MOBA_Q_CHUNK)
        bq = (ci * MOBA_Q_CHUNK) // MOBA_BLOCK
        k_own = lax.dynamic_index_in_dim(kb, bq, axis=2, keepdims=False).astype(jnp.float32)
        v_own = lax.dynamic_index_in_dim(vb, bq, axis=2, keepdims=False).astype(jnp.float32)
        dist_own = (t[:, None] - (bq * MOBA_BLOCK + s_local)[None, :]).astype(jnp.float32)
        l_own = jnp.einsum('bhqd,bhsd->bhqs', qch, k_own) * scale - slopes[:, None, None] * dist_own
        l_own = jnp.where(dist_own >= 0, l_own, NEG_INF)
        if n_sel == 0:
            p_own = jax.nn.softmax(l_own, axis=-1)
            return jnp.einsum('bhqs,bhsd->bhqd', p_own, v_own)
        gate = jnp.einsum('bhqd,bhnd->bhqn', qch, kmean)
        gate = jnp.where(jnp.arange(nb) < bq, gate, -jnp.inf)
        _, idx = lax.top_k(gate, n_sel)
        k_sel = kb[bidx, hidx, idx].astype(jnp.float32)
        v_sel = vb[bidx, hidx, idx].astype(jnp.float32)
        dist_sel = (t[:, None, None] - (idx[..., None] * MOBA_BLOCK + s_local)).astype(jnp.float32)
        l_sel = (jnp.einsum('bhqd,bhqnsd->bhqns', qch, k_sel) * scale
                 - slopes[:, None, None, None] * dist_sel)
        l_sel = jnp.where((jnp.arange(n_sel) < bq)[:, None], l_sel, NEG_INF)
        n_past = n_sel * MOBA_BLOCK
        logits = jnp.concatenate(
            [l_sel.reshape(B, H, MOBA_Q_CHUNK, n_past), l_own], axis=-1)
        p = jax.nn.softmax(logits, axis=-1)
        p_sel = p[..., :n_past].reshape(B, H, MOBA_Q_CHUNK, n_sel, MOBA_BLOCK)
        p_own = p[..., n_past:]
        return (jnp.einsum('bhqns,bhqnsd->bhqd', p_sel, v_sel)
                + jnp.einsum('bhqs,bhsd->bhqd', p_own, v_own))

    out = lax.map(one_chunk, (qc, jnp.arange(nq)))
    out = jnp.moveaxis(out, 0, 2).reshape(B, H, tp, dh)
    return out[:, :, :T]


def moba_branch(qb, kb, vb, zb):
    B, T, _ = qb.shape

    def heads(t):
        return t.reshape(B, T, MO_HEADS, MO_DH).transpose(0, 2, 1, 3)

    o = moba_attention(heads(qb), heads(kb), heads(vb)).transpose(0, 2, 1, 3).reshape(B, T, MO_W)
    return (o * jax.nn.silu(zb.astype(jnp.float32))).astype(qb.dtype)


def memory_cross_attention(h, mem_n, wq, wkv, wo):
    B, T, _ = h.shape
    M = mem_n.shape[1]
    q = (h @ wq).reshape(B, T, XA_HEADS, XA_DH)
    k, v = jnp.split(mem_n @ wkv, 2, axis=-1)
    k = k.reshape(B, M, XA_HEADS, XA_DH)
    v = v.reshape(B, M, XA_HEADS, XA_DH)
    s = jnp.einsum('bthd,bmhd->bhtm', q, k).astype(jnp.float32) * (XA_DH ** -0.5)
    p = jax.nn.softmax(s, axis=-1).astype(v.dtype)
    o = jnp.einsum('bhtm,bmhd->bthd', p, v).reshape(B, T, XA_W)
    return o @ wo


def setup_inputs(seed: int = 0) -> dict:
    key = jax.random.key(seed)
    ks = jax.random.split(key, 26)

    def nrm(k, shape, scale):
        return scale * jax.random.normal(k, shape, jnp.float32)

    def gain(k, shape):
        return 1.0 + 0.02 * jax.random.normal(k, shape, jnp.float32)

    u = jax.random.uniform(ks[10], (DEPTH, LRU_W), jnp.float32, 0.9, 0.999)
    a = u ** (1.0 / LRU_C)
    lam = jnp.log(a) - jnp.log1p(-a)
    bl = LRU_W // LRU_BLOCKS
    nqkv = ML_W // ML_QKV_BLOCK
    return {
        "x": nrm(ks[0], (BATCH, SEQ, D_MODEL), 1.0),
        "mem": nrm(ks[1], (BATCH, MEM_LEN, D_MODEL), 1.0),
        "mix_norm_g": gain(ks[2], (DEPTH, D_MODEL)),
        "w_in": nrm(ks[3], (DEPTH, D_MODEL, IN_COLS), D_MODEL ** -0.5),
        "lru_conv_w": nrm(ks[4], (DEPTH, LRU_CONV, LRU_W), LRU_CONV ** -0.5),
        "lru_conv_b": nrm(ks[5], (DEPTH, LRU_W), 0.02),
        "lru_wa": nrm(ks[6], (DEPTH, LRU_BLOCKS, bl, bl), bl ** -0.5),
        "lru_ba": nrm(ks[7], (DEPTH, LRU_W), 0.02),
        "lru_wx": nrm(ks[8], (DEPTH, LRU_BLOCKS, bl, bl), bl ** -0.5),
        "lru_bx": nrm(ks[9], (DEPTH, LRU_W), 0.02),
        "lru_lambda": lam,
        "ml_conv_w": nrm(ks[11], (DEPTH, ML_CONV, ML_W), ML_CONV ** -0.5),
        "ml_conv_b": nrm(ks[12], (DEPTH, ML_W), 0.02),
        "ml_wq": nrm(ks[13], (DEPTH, nqkv, ML_QKV_BLOCK, ML_QKV_BLOCK), ML_QKV_BLOCK ** -0.5),
        "ml_wk": nrm(ks[14], (DEPTH, nqkv, ML_QKV_BLOCK, ML_QKV_BLOCK), ML_QKV_BLOCK ** -0.5),
        "ml_wv": nrm(ks[15], (DEPTH, nqkv, ML_QKV_BLOCK, ML_QKV_BLOCK), ML_QKV_BLOCK ** -0.5),
        "ml_bi": nrm(ks[16], (DEPTH, ML_HEADS), 0.1),
        "ml_bf": jnp.linspace(3.0, 6.0, ML_HEADS, dtype=jnp.float32)[None, :] + nrm(ks[17], (DEPTH, ML_HEADS), 0.02),
        "ml_norm_g": gain(ks[18], (DEPTH, ML_W)),
        "w_out": nrm(ks[19], (DEPTH, MIX_W, D_MODEL), MIX_W ** -0.5),
        "xa_norm_g": gain(ks[20], (DEPTH, D_MODEL)),
        "mem_norm_g": gain(ks[21], (DEPTH, D_MODEL)),
        "xa_wq": nrm(ks[22], (DEPTH, D_MODEL, XA_W), D_MODEL ** -0.5),
        "xa_wkv": nrm(ks[23], (DEPTH, D_MODEL, 2 * XA_W), D_MODEL ** -0.5),
        "xa_wo": nrm(ks[24], (DEPTH, XA_W, D_MODEL), XA_W ** -0.5),
        "final_norm_g": gain(ks[25], (D_MODEL,)),
    }


def reference(x, mem, mix_norm_g, w_in, lru_conv_w, lru_conv_b, lru_wa, lru_ba, lru_wx, lru_bx,
              lru_lambda, ml_conv_w, ml_conv_b, ml_wq, ml_wk, ml_wv, ml_bi, ml_bf, ml_norm_g,
              w_out, xa_norm_g, mem_norm_g, xa_wq, xa_wkv, xa_wo, final_norm_g):
    for l in range(DEPTH):
        h = rms_norm(x, mix_norm_g[l])
        p = h @ w_in[l]
        (lru_x, lru_z, ml_u, ml_o, ml_z, ml_i, ml_f,
         mo_q, mo_k, mo_v, mo_z) = jnp.split(p, SPLIT_POINTS, axis=-1)
        y_lru = rg_lru_branch(lru_x, lru_z, lru_conv_w[l], lru_conv_b[l], lru_wa[l], lru_ba[l],
                              lru_wx[l], lru_bx[l], lru_lambda[l])
        y_ml = mlstm_branch(ml_u, ml_o, ml_z, ml_i, ml_f, ml_conv_w[l], ml_conv_b[l], ml_wq[l],
                            ml_wk[l], ml_wv[l], ml_bi[l], ml_bf[l], ml_norm_g[l])
        y_mo = moba_branch(mo_q, mo_k, mo_v, mo_z)
        y = jnp.concatenate([y_lru, y_ml, y_mo], axis=-1)
        x = x + y @ w_out[l]
        hx = rms_norm(x, xa_norm_g[l])
        hm = rms_norm(mem, mem_norm_g[l])
        x = x + memory_cross_attention(hx, hm, xa_wq[l], xa_wkv[l], xa_wo[l])
    return rms_norm(x, final_norm_g)
```

```python
import math
import numpy as np
import ml_dtypes
from contextlib import ExitStack
import concourse.bass as bass
import concourse.mybir as mybir
from concourse.bass_utils import run_bass_kernel_spmd

F32 = mybir.dt.float32
BF16 = mybir.dt.bfloat16
AF = mybir.ActivationFunctionType
ALU = mybir.AluOpType
AX = mybir.AxisListType

T = 2048
D = 2048
KD = 16
MEM = 256
L = 2
ENG = ('pe', 'act', 'dve', 'pool', 'sp')
NEG = -1.0e30
ARENA_WORDS = 50600


class Op:
    __slots__ = ('eng', 'fn', 'deps', 'signal', 'sig', 'sigval', 'dma', 'sem', 'semval')


class Prog:
    EPOCH = 6000
    NQ = 12

    def __init__(self, nc, es):
        self.nc = nc
        self.es = es
        self.ops = {e: [] for e in ENG}
        self.state = {}
        self.dma_cnt = {e: 0 for e in ENG}
        self.dma_last = {e: {} for e in ENG}
        self.dma_sems = {}
        self.eng_sems = {}
        self.pending = {e: set() for e in ENG}

    def _sem(self, name):
        return self.es.enter_context(self.nc.semaphore(name))

    limit = None
    count = 0

    def add(self, eng, fn, r=(), w=(), dma=False):
        self.count += 1
        if self.limit is not None and self.count > self.limit:
            return None
        op = Op()
        op.eng = eng; op.fn = fn; op.dma = dma; op.signal = False
        op.sig = 0; op.sigval = 0; op.sem = None; op.semval = 0
        deps = set(self.pending[eng])
        self.pending[eng] = set()
        for k in r:
            st = self.state.get(k)
            if st is not None and st[0] is not None:
                deps.add(st[0])
        for k in w:
            st = self.state.get(k)
            if st is not None:
                if st[0] is not None:
                    deps.add(st[0])
                deps.update(st[1].values())
        for k in r:
            rd = self.state.setdefault(k, [None, {}])[1]
            if dma:
                rd[('dma', id(op))] = op
            else:
                rd[eng] = op
        for k in w:
            self.state[k] = [op, {}]
        if dma:
            if eng not in self.dma_sems:
                self.dma_sems[eng] = [self._sem(f"dq_{eng}_{i}") for i in range(self.NQ)]
            i = self.dma_cnt[eng]
            self.dma_cnt[eng] += 1
            slot = i % self.NQ
            op.sem = self.dma_sems[eng][slot]
            op.semval = 16 * (i // self.NQ + 1)
            prev = self.dma_last[eng].get(slot)
            if prev is not None:
                deps.add(prev)
            self.dma_last[eng][slot] = op
        deps.discard(op)
        fin = []
        for d in deps:
            if (not d.dma) and (not dma) and d.eng == 'pe' and eng == 'pe':
                continue
            if not d.dma:
                d.signal = True
            fin.append(d)
        op.deps = fin
        self.ops[eng].append(op)
        return op

    def barrier(self):
        last = []
        for e in ENG:
            comp = [o for o in self.ops[e] if not o.dma]
            if comp:
                last.append(comp[-1])
            last.extend(self.dma_last[e].values())
        for e in ENG:
            self.pending[e].update(last)

    def emit(self):
        for e in ENG:
            cnt = 0
            for op in self.ops[e]:
                if (not op.dma) and op.signal:
                    op.sig = cnt // self.EPOCH
                    op.sigval = cnt % self.EPOCH + 1
                    cnt += 1
                    if (e, op.sig) not in self.eng_sems:
                        self.eng_sems[(e, op.sig)] = self._sem(f"es_{e}_{op.sig}")
        prog = self

        def run(e, h):
            waited = {}
            for op in prog.ops[e]:
                need = {}
                for d in op.deps:
                    if d.dma:
                        s, v = d.sem, d.semval
                    else:
                        s, v = prog.eng_sems[(d.eng, d.sig)], d.sigval
                    if need.get(id(s), (None, 0))[1] < v:
                        need[id(s)] = (s, v)
                for k, (s, v) in need.items():
                    if waited.get(k, 0) < v:
                        h.wait_ge(s, v)
                        waited[k] = v
                ins = op.fn(h)
                if op.dma:
                    ins.then_inc(op.sem, 16)
                elif op.signal:
                    ins.then_inc(prog.eng_sems[(e, op.sig)], 1)
            if e == 'sp':
                for q in ENG:
                    for slot, o in prog.dma_last[q].items():
                        h.wait_ge(o.sem, o.semval)

        with self.nc.Block() as block:
            @block.tensor
            def _(h):
                run('pe', h)

            @block.scalar
            def _(h):
                run('act', h)

            @block.vector
            def _(h):
                run('dve', h)

            @block.gpsimd
            def _(h):
                run('pool', h)

            @block.sync
            def _(h):
                run('sp', h)


class Arena:
    def __init__(self, t, n):
        self.t = t; self.n = n; self.off = 0

    def alloc(self, shape, dt=F32):
        nel = 1
        for s in shape[1:]:
            nel *= s
        words = nel if dt == F32 else (nel + 1) // 2
        words = (words + 1) // 2 * 2
        assert self.off + words <= self.n, f"arena overflow {self.off}+{words}>{self.n}"
        v = self.t[0:shape[0], self.off:self.off + words]
        self.off += words
        if dt != F32:
            v = v.bitcast(dt)
        v = v[:, 0:nel]
        if len(shape) == 3:
            v = v.rearrange("p (a b) -> p a b", a=shape[1])
        elif len(shape) == 4:
            v = v.rearrange("p (a b c) -> p a b c", a=shape[1], b=shape[2])
        return v


def seg_offsets(NL, NM, NO):
    sizes = [('lx', NL * 128), ('lz', NL * 128), ('mu', NM * 192), ('mo', NM * 192), ('mz', NM * 192),
             ('mi', NM), ('mf', NM), ('oq', NO * 128), ('ok', NO * 128), ('ov', NO * 128), ('oz', NO * 128)]
    off = {}
    o = 0
    for n, s in sizes:
        off[n] = o
        o += s
    return off, o


def const_layout(NO):
    CO = {}
    o = 0
    for n, s in [('ident', 128), ('tri', 128), ('negmask', 64), ('slope', NO), ('etab', NO * 256), ('misc', 4)]:
        CO[n] = o
        o += s
    return CO, o


def pp_layout(NL, NM):
    PPO = {'lru': 0, 'ml': NL * 8, 'gb': NL * 8 + NM * 10}
    return PPO, NL * 8 + NM * 10 + 2


def build(cfg):
    NL, NM, NO = cfg['NL'], cfg['NM'], cfg['NO']
    nlayers = cfg.get('layers', L)
    stop = cfg.get('stop', 'all')
    dbg = cfg.get('dbg', False)
    SEG, WC = seg_offsets(NL, NM, NO)
    NCH = NL + 2 * NM + NO
    CO, NCF = const_layout(NO)
    PPO, NPP = pp_layout(NL, NM)
    nc = bass.Bass("TRN2", target_bir_lowering=False)
    es = ExitStack()
    P = Prog(nc, es)
    P.limit = cfg.get('limit', None)

    def din(name, shape, dt=F32):
        return nc.dram_tensor(name, list(shape), dt, kind="ExternalInput").ap()

    def dscr(name, shape, dt=F32, out=False):
        return nc.dram_tensor(name, list(shape), dt, kind="ExternalOutput" if out else "Internal").ap()

    x_in = din("x", [T, D])
    mem_in = din("mem", [MEM, D])
    w_in = din("w_in", [L, D, WC])
    w_out = din("w_out", [L, D, D])
    gains = din("gains", [L, 3, D])
    final_g = din("final_g", [D])
    pp_in = din("pp", [L, 128, NPP])
    lru_w = din("lru_w", [L, 2, NL, 128, 128])
    ml_w = din("ml_w", [L, 3, NM, 2, 96, 96])
    ml_ng = din("ml_ng", [L, NM * 192])
    xa_wq = din("xa_wq", [L, D, 512])
    xa_wkv = din("xa_wkv", [L, D, 1024])
    xa_wo = din("xa_wo", [L, 512, D])
    cst_f = din("cst_f", [128, NCF])
    cst_b = din("cst_b", [128, NO * 512], BF16)
    out_d = nc.dram_tensor("out", [T, D], F32, kind="ExternalOutput").ap()
    xs0 = dscr("xs0", [T, D], F32, out=dbg)
    yT_d = dscr("yT", [L, NCH, 128, T], BF16, out=dbg)
    dbg_f = dscr("dbgf", [128, 4096], F32, out=True) if dbg else None

    arena_t = es.enter_context(nc.sbuf_tensor("arena", [128, ARENA_WORDS], F32))
    A = Arena(arena_t, ARENA_WORDS)

    def ps(name, shape, dt=F32):
        return es.enter_context(nc.psum_tensor(name, list(shape), dt))

    def DMA(out, in_, r, w, eng='sp', **kw):
        P.add(eng, lambda e: e.dma_start(out=out, in_=in_, **kw), r, w, dma=True)

    def MM(out, lhsT, rhs, start, stop, r, w, **kw):
        P.add('pe', lambda e: e.matmul(out, lhsT, rhs, start=start, stop=stop, **kw), r, w)

    def TR(out, in_, ident, r, w):
        P.add('pe', lambda e: e.transpose(out, in_, ident), r, w)

    def ACT(out, in_, func, r, w, **kw):
        P.add('act', lambda e: e.activation(out=out, in_=in_, func=func, **kw), r, w)

    def TS(out, in0, s1, s2, op0, op1, r, w, eng='dve', **kw):
        if op1 is None:
            P.add(eng, lambda e: e.tensor_scalar(out, in0, s1, None, op0, **kw), r, w)
        else:
            P.add(eng, lambda e: e.tensor_scalar(out, in0, s1, s2, op0, op1, **kw), r, w)

    def TT(out, in0, in1, op, r, w, eng='dve'):
        P.add(eng, lambda e: e.tensor_tensor(out, in0, in1, op), r, w)

    def STT(out, in0, scalar, in1, op0, op1, r, w, **kw):
        P.add('dve', lambda e: e.scalar_tensor_tensor(out, in0, scalar, in1, op0, op1, **kw), r, w)

    def CP(out, in_, r, w, eng='dve'):
        if eng == 'act':
            ACT(out, in_, AF.Copy, r, w)
        else:
            P.add(eng, lambda e: e.tensor_copy(out, in_), r, w)

    def RECIP(out, in_, r, w):
        P.add('dve', lambda e: e.reciprocal(out, in_), r, w)

    def SCAN(out, d0, d1, init, op0, op1, r, w):
        P.add('dve', lambda e: e.tensor_tensor_scan(out, d0, d1, init, op0, op1), r, w)

    def MEMSET(ap, val, w, eng='pool'):
        P.add(eng, lambda e: e.memset(ap, val), (), w)

    def chv(ap):
        return ap.rearrange("p (c t) -> p c t", t=128)

    cf = A.alloc([128, NCF])
    DMA(cf[:, :], cst_f[:, :], (), ['cf'])
    ident_f = cf[:, CO['ident']:CO['ident'] + 128]
    tri_f = cf[:, CO['tri']:CO['tri'] + 128]
    negmask = cf[:, CO['negmask']:CO['negmask'] + 64]
    slopecol = cf[:, CO['slope']:CO['slope'] + NO]
    etab = cf[:, CO['etab']:CO['etab'] + NO * 256]
    eps6 = cf[:, CO['misc']:CO['misc'] + 1]
    eps5 = cf[:, CO['misc'] + 1:CO['misc'] + 2]
    one1 = cf[:, CO['misc'] + 2:CO['misc'] + 3]
    gdiag = A.alloc([128, NO * 512], BF16)
    DMA(gdiag[:, :], cst_b[:, :], (), ['gdiag'])
    identb = A.alloc([128, 128], BF16)
    CP(identb[:, :], ident_f, ['cf'], ['identb'])
    ones_f = A.alloc([128, 128])
    MEMSET(ones_f[:, :], 1.0, ['ones_f'])
    zeros_f = A.alloc([128, 128])
    MEMSET(zeros_f[:, :], 0.0, ['zeros_f'])
    pp = A.alloc([128, NPP])
    nst = A.alloc([128, 8])
    persist_mark = A.off

    NPS = 6
    psf = [ps(f"psf{i}", [128, 512]) for i in range(NPS)]
    psb = [ps(f"psb{i}", [128, 1024], BF16) for i in range(2)]
    pctr = [0, 0]

    def PSF():
        i = pctr[0] % NPS
        pctr[0] += 1
        return psf[i], f"psf{i}"

    def PSB():
        i = pctr[1] % 2
        pctr[1] += 1
        return psb[i], f"psb{i}"

    def norm_tile(xt_ap, xkey, gbc_ap, gkey, out_ap, okey, junk_ap, jkey):
        STT(junk_ap, xt_ap, 1.0, xt_ap, ALU.mult, ALU.mult, [xkey], [jkey, 'n_ss'], accum_out=nst[:, 0:1])
        ACT(nst[:, 1:2], nst[:, 0:1], AF.Sqrt, ['n_ss', 'cf'], ['n_sd'], scale=1.0 / D, bias=eps6)
        RECIP(nst[:, 2:3], nst[:, 1:2], ['n_sd'], ['n_rstd'])
        STT(out_ap, xt_ap, nst[:, 2:3], gbc_ap, ALU.mult, ALU.mult, [xkey, 'n_rstd', gkey], [okey])

    def transpose_16(hb, hbkey, dst_fn, dkey):
        for half in range(2):
            pb, pk = PSB()
            for kk in range(8):
                k = half * 8 + kk
                TR(pb[:, kk * 128:(kk + 1) * 128], hb[:, k * 128:(k + 1) * 128], identb[:, :],
                   [hbkey, 'identb'], [pk])
            src = pb[:, :].rearrange("p (k t) -> p k t", k=8)
            CP(dst_fn(half), src, [pk], [dkey], eng=('act' if half == 0 else 'dve'))

    def mix_phase(l, src):
        A.off = persist_mark
        P.barrier()
        hT = A.alloc([128, KD, T], BF16)
        mb = [A.alloc([128, T + 8]) for _ in range(5)]
        mbb = [A.alloc([128, T], BF16) for _ in range(2)]
        wbuf = [A.alloc([128, KD, 192], BF16) for _ in range(2)]
        hba = A.alloc([128, 2, T], BF16)
        hbq = A.alloc([128, 2, T], BF16)
        hbk = A.alloc([128, 2, T], BF16)
        vaug = A.alloc([128, 16, 193], BF16)
        hsb = A.alloc([128, 16, 192])
        ngbc = A.alloc([128, NM * 192])
        colbuf = A.alloc([128, 16, 5 * NM])
        soldbc = A.alloc([128, NM * 16])
        soldrow = A.alloc([1, NM * 16])
        sm = A.alloc([128, 160])
        lw = A.alloc([128, 2, 128], BF16)
        mlw = A.alloc([96, 3, 2, 96], BF16)
        Cst = A.alloc([96, 2, 193])
        Cb = A.alloc([96, 2, 193], BF16)
        Stb = A.alloc([128, 128], BF16)
        Hs = A.alloc([128, 193])
        comb = A.alloc([128, 193])
        sob = A.alloc([128, 192])
        kwb = A.alloc([128, 192], BF16)
        st6 = A.alloc([128, 6])
        mv = A.alloc([128, 16, 2])
        dn = A.alloc([128, 4])
        km = A.alloc([128, 8])
        gm = A.alloc([128, 8])
        top8 = A.alloc([128, 8])
        sel = A.alloc([128, 8])
        Fq = A.alloc([128, 16, 16])
        Ptb = [A.alloc([128, 256], BF16) for _ in range(3)]
        Ptd = [A.alloc([128, 256], BF16) for _ in range(2)]
        ofb = A.alloc([128, 128])
        l_c8 = A.alloc([128, 4])

        DMA(pp[:, :], pp_in[l], (), ['pp'])
        DMA(ngbc[:, :], ml_ng[l].partition_broadcast(128), (), ['ngbc'])

        gbc = mb[3][:, 0:D]
        DMA(gbc, gains[l, 0].partition_broadcast(128), (), ['mb3'])
        for i in range(16):
            xt = mb[i % 2][:, 0:D]
            xk = f"mb{i % 2}"
            DMA(xt, src[i * 128:(i + 1) * 128, :], (), [xk])
            hb = mbb[i % 2][:, 0:D]
            hk = f"mbb{i % 2}"
            norm_tile(xt, xk, gbc, 'mb3', hb, hk, hb, hk)
            transpose_16(hb, hk, lambda half, i=i: hT[:, half * 8:(half + 1) * 8, i * 128:(i + 1) * 128],
                         f"hT{i // 4}")
        if stop == 'A':
            DMA(yT_d[l, 0:16].rearrange("c p t -> p c t"), hT[:, :, :], [f"hT{i}" for i in range(4)], ['yTd'])
            return

        wctr = [0]

        def load_w(src2d, ncols):
            i = wctr[0] % 2
            wctr[0] += 1
            wt = wbuf[i]
            key = f"wbuf{i}"
            DMA(wt[:, :, 0:ncols], src2d.rearrange("(k p) c -> p k c", p=128), (), [key], eng='pool')
            return wt, key

        def proj_cm(wt, wkey, c0, M, tb):
            pt, pk = PSF()
            for k in range(KD):
                MM(pt[0:M, 0:512], wt[:, k, c0:c0 + M], hT[:, k, tb * 512:(tb + 1) * 512], k == 0, k == KD - 1,
                   [wkey, f"hT{tb}"], [pk])
            return pt, pk

        def proj_tm(wt, wkey, c0, N, tt):
            pt, pk = PSF()
            for k in range(KD):
                MM(pt[:, 0:N], hT[:, k, tt * 128:(tt + 1) * 128], wt[:, k, c0:c0 + N], k == 0, k == KD - 1,
                   [wkey, f"hT{tt // 4}"], [pk])
            return pt, pk

        def lru_group(g):
            wx_t, wxk = load_w(w_in[l][:, SEG['lx'] + g * 128: SEG['lx'] + (g + 1) * 128], 128)
            wz_t, wzk = load_w(w_in[l][:, SEG['lz'] + g * 128: SEG['lz'] + (g + 1) * 128], 128)
            DMA(lw[:, 0, :], lru_w[l, 0, g], (), ['lw0'], eng='pool')
            DMA(lw[:, 1, :], lru_w[l, 1, g], (), ['lw1'], eng='pool')
            po = PPO['lru'] + g * 8
            xpad = mb[0]
            MEMSET(xpad[:, 0:3], 0.0, ['mb0'])
            for tb in range(4):
                pt, pk = proj_cm(wx_t, wxk, 0, 128, tb)
                CP(xpad[:, 3 + tb * 512: 3 + (tb + 1) * 512], pt[:, :], [pk], ['mb0'], eng='act')
            acc = mb[1]
            TS(acc[:, 0:T], xpad[:, 0:T], pp[:, po:po + 1], pp[:, po + 4:po + 5], ALU.mult, ALU.add,
               ['mb0', 'pp'], ['mb1'])
            for j in range(1, 4):
                STT(acc[:, 0:T], xpad[:, j:j + T], pp[:, po + j:po + j + 1], acc[:, 0:T], ALU.mult, ALU.add,
                    ['mb0', 'mb1', 'pp'], ['mb1'])
            xcb = mbb[0]
            CP(xcb[:, 0:T], acc[:, 0:T], ['mb1'], ['mbb0'], eng='pool')
            r_, ig = mb[2], mb[3]
            for tb in range(4):
                sl = slice(tb * 512, (tb + 1) * 512)
                pt, pk = PSF()
                MM(pt[:, :], lw[:, 0, :], xcb[:, sl], True, True, ['lw0', 'mbb0'], [pk])
                ACT(r_[:, sl], pt[:, :], AF.Sigmoid, [pk, 'pp'], ['mb2'], bias=pp[:, po + 5:po + 6])
                pt, pk = PSF()
                MM(pt[:, :], lw[:, 1, :], xcb[:, sl], True, True, ['lw1', 'mbb0'], [pk])
                ACT(ig[:, sl], pt[:, :], AF.Sigmoid, [pk, 'pp'], ['mb3'], bias=pp[:, po + 6:po + 7])
            ACT(l_c8[:, 0:1], pp[:, po + 7:po + 8], AF.Exp, ['pp'], ['l_c8'], scale=-1.0)
            ACT(l_c8[:, 1:2], l_c8[:, 0:1], AF.Ln, ['l_c8', 'cf'], ['l_c8b'], bias=one1)
            TS(l_c8[:, 2:3], l_c8[:, 1:2], -8.0, None, ALU.mult, None, ['l_c8b'], ['l_c8c'])
            a_ = mb[4]
            ACT(a_[:, 0:T], r_[:, 0:T], AF.Exp, ['mb2', 'l_c8c'], ['mb4'], scale=l_c8[:, 2:3])
            a2 = mb[2]
            TT(a2[:, 0:T], a_[:, 0:T], a_[:, 0:T], ALU.mult, ['mb4'], ['mb2'])
            ACT(a2[:, 0:T], a2[:, 0:T], AF.Sqrt, ['mb2', 'cf'], ['mb2'], scale=-1.0, bias=one1)
            TT(ig[:, 0:T], ig[:, 0:T], acc[:, 0:T], ALU.mult, ['mb3', 'mb1'], ['mb3'])
            TT(ig[:, 0:T], ig[:, 0:T], a2[:, 0:T], ALU.mult, ['mb3', 'mb2'], ['mb3'])
            hh = mb[1]
            SCAN(hh[:, 0:T], a_[:, 0:T], ig[:, 0:T], 0.0, ALU.mult, ALU.add, ['mb4', 'mb3'], ['mb1'])
            zs = mb[2]
            for tb in range(4):
                sl = slice(tb * 512, (tb + 1) * 512)
                pt, pk = proj_cm(wz_t, wzk, 0, 128, tb)
                ACT(zs[:, sl], pt[:, :], AF.Silu, [pk], ['mb2'])
            yb = mbb[1]
            TT(yb[:, 0:T], hh[:, 0:T], zs[:, 0:T], ALU.mult, ['mb1', 'mb2'], ['mbb1'])
            DMA(yT_d[l, g, :, :], yb[:, 0:T], ['mbb1'], ['yTd'])

        for g in range(NL):
            lru_group(g)
        if stop == 'lru':
            return

        PK = mb[3]
        NR = 5 * NM

        def ml_gates():
            wgi, k1 = load_w(w_in[l][:, SEG['mi']:SEG['mi'] + NM], NM)
            wgf, k2 = load_w(w_in[l][:, SEG['mf']:SEG['mf'] + NM], NM)
            ig, sp_, bb, mm_ = mb[0], mb[1], mb[2], mb[4]
            gb = PPO['gb']
            for tb in range(4):
                sl = slice(tb * 512, (tb + 1) * 512)
                pt, pk = proj_cm(wgi, k1, 0, NM, tb)
                ACT(ig[0:NM, sl], pt[0:NM, :], AF.Identity, [pk, 'pp'], ['mb0'], bias=pp[0:NM, gb:gb + 1])
                pt, pk = proj_cm(wgf, k2, 0, NM, tb)
                ACT(sp_[0:NM, sl], pt[0:NM, :], AF.Exp, [pk, 'pp'], ['mb1'], scale=-1.0,
                    bias=pp[0:NM, gb + 1:gb + 2])
            ACT(sp_[0:NM, 0:T], sp_[0:NM, 0:T], AF.Ln, ['mb1', 'cf'], ['mb1'], bias=one1[0:NM, :])
            for c_ in range(16):
                sl = slice(c_ * 128, (c_ + 1) * 128)
                SCAN(bb[0:NM, sl], ones_f[0:NM, 0:128], sp_[0:NM, sl], 0.0, ALU.mult, ALU.subtract,
                     ['mb1', 'ones_f'], ['mb2'])
            beta = mb[0]
            TT(beta[0:NM, 0:T], ig[0:NM, 0:T], bb[0:NM, 0:T], ALU.subtract, ['mb0', 'mb2'], ['mb0'])
            cmx = mb[1]
            for c_ in range(16):
                sl = slice(c_ * 128, (c_ + 1) * 128)
                SCAN(cmx[0:NM, sl], zeros_f[0:NM, 0:128], beta[0:NM, sl], NEG, ALU.add, ALU.max,
                     ['mb0', 'zeros_f'], ['mb1'])
            gc, mxb, mloc, mnew, mprev, d1, sold = [sm[0:NM, 16 * i:16 * (i + 1)] for i in range(7)]
            CP(gc, chv(bb[0:NM, 0:T])[:, :, 127], ['mb2'], ['sm_gc'])
            CP(mxb, chv(cmx[0:NM, 0:T])[:, :, 127], ['mb1'], ['sm_mxb'])
            TT(mloc, gc, mxb, ALU.add, ['sm_gc', 'sm_mxb'], ['sm_mloc'])
            SCAN(mnew, gc, mloc, 0.0, ALU.add, ALU.max, ['sm_gc', 'sm_mloc'], ['sm_mnew'])
            MEMSET(mprev[:, 0:1], 0.0, ['sm_mprev'], eng='dve')
            CP(mprev[:, 1:16], mnew[:, 0:15], ['sm_mnew'], ['sm_mprev'])
            bc = lambda a: a.unsqueeze(2).to_broadcast([NM, 16, 128])
            TT(chv(mm_[0:NM, 0:T]), chv(cmx[0:NM, 0:T]), bc(mprev), ALU.max, ['mb1', 'sm_mprev'], ['mb4'])
            tmp = mb[1]
            rows = []
            ACT(tmp[0:NM, 0:T], beta[0:NM, 0:T], AF.Exp, ['mb0'], ['mb1'])
            for h in range(NM):
                DMA(PK[0 * NM + h:0 * NM + h + 1, 0:T], tmp[h:h + 1, 0:T], ['mb1'], ['mb3'])
            TT(tmp[0:NM, 0:T], mm_[0:NM, 0:T], bb[0:NM, 0:T], ALU.add, ['mb4', 'mb2'], ['mb1'])
            ACT(tmp[0:NM, 0:T], tmp[0:NM, 0:T], AF.Exp, ['mb1'], ['mb1'], scale=-1.0)
            for h in range(NM):
                DMA(PK[3 * NM + h:3 * NM + h + 1, 0:T], tmp[h:h + 1, 0:T], ['mb1'], ['mb3'])
            TT(chv(tmp[0:NM, 0:T]), bc(mprev), chv(mm_[0:NM, 0:T]), ALU.subtract, ['mb4', 'sm_mprev'], ['mb1'])
            ACT(tmp[0:NM, 0:T], tmp[0:NM, 0:T], AF.Exp, ['mb1'], ['mb1'])
            for h in range(NM):
                DMA(PK[2 * NM + h:2 * NM + h + 1, 0:T], tmp[h:h + 1, 0:T], ['mb1'], ['mb3'])
            ACT(tmp[0:NM, 0:T], mm_[0:NM, 0:T], AF.Exp, ['mb4'], ['mb1'], scale=-1.0)
            for h in range(NM):
                DMA(PK[4 * NM + h:4 * NM + h + 1, 0:T], tmp[h:h + 1, 0:T], ['mb1'], ['mb3'])
            TT(d1, gc, mnew, ALU.subtract, ['sm_gc', 'sm_mnew'], ['sm_d1'])
            TT(chv(tmp[0:NM, 0:T]), chv(beta[0:NM, 0:T]), bc(d1), ALU.add, ['mb0', 'sm_d1'], ['mb1'])
            ACT(tmp[0:NM, 0:T], tmp[0:NM, 0:T], AF.Exp, ['mb1'], ['mb1'])
            TS(tmp[0:NM, 0:T], tmp[0:NM, 0:T], 192.0 ** -0.5, None, ALU.mult, None, ['mb1'], ['mb1'])
            for h in range(NM):
                DMA(PK[1 * NM + h:1 * NM + h + 1, 0:T], tmp[h:h + 1, 0:T], ['mb1'], ['mb3'])
            TT(sold, d1, mprev, ALU.add, ['sm_d1', 'sm_mprev'], ['sm_sold'])
            ACT(sold, sold, AF.Exp, ['sm_sold'], ['sm_sold'])
            for h in range(NM):
                DMA(soldrow[0:1, h * 16:(h + 1) * 16], sold[h:h + 1, 0:16], ['sm_sold'], ['soldrow'])
            pt, pk = PSF()
            MM(pt[0:96, 0:NM * 16], ones_f[0:1, 0:96], soldrow[0:1, :], True, True, ['ones_f', 'soldrow'], [pk])
            CP(soldbc[0:96, :], pt[0:96, 0:NM * 16], [pk], ['soldbc'])
            for c_ in range(16):
                sl = slice(c_ * 128, (c_ + 1) * 128)
                pt, pk = PSF()
                TR(pt[:, 0:NR], PK[0:NR, sl], ident_f[0:NR, 0:NR], ['mb3', 'cf'], [pk])
                CP(colbuf[:, c_, :], pt[:, 0:NR], [pk], ['colbuf'], eng=('act' if c_ % 2 else 'dve'))

        ml_gates()
        if stop == 'mlg':
            DMA(dbg_f[:, 0:16 * NR], colbuf[:, :, :].rearrange("p c q -> p (c q)"), ['colbuf'], ['dbgf'])
            DMA(dbg_f[0:96, 2048:2048 + NM * 16], soldbc[0:96, :], ['soldbc'], ['dbgf'])
            return

        def ml_head(h):
            ch0 = NL + 2 * h
            wu, wuk = load_w(w_in[l][:, SEG['mu'] + h * 192: SEG['mu'] + (h + 1) * 192], 192)
            for a_i in range(3):
                DMA(mlw[0:96, a_i, :, :], ml_w[l, a_i, h].rearrange("c i o -> i c o"), (), ['mlw'], eng='pool')
            ucb, qTb, kTb = hba, hbq, hbk
            acc = mb[2]
            if cfg.get('verbose'):
                print("count at mlh_a0", P.count)
            if stop == 'mlh_a0':
                CP(sm[0:96, 0:96], mlw[0:96, 0, 0, :], ['mlw'], ['sm_x'])
                return
            for cc in range(2):
                upad = mb[cc]
                uk = f"mb{cc}"
                ub = mbb[cc]
                ubk = f"mbb{cc}"
                MEMSET(upad[0:96, 0:3], 0.0, [uk])
                for tb in range(4):
                    sl = slice(tb * 512, (tb + 1) * 512)
                    pt, pk = proj_cm(wu, wuk, cc * 96, 96, tb)
                    CP(upad[0:96, 3 + tb * 512:3 + (tb + 1) * 512], pt[0:96, :], [pk], [uk], eng='act')
                    CP(ub[0:96, sl], upad[0:96, 3 + tb * 512:3 + (tb + 1) * 512], [uk], [ubk], eng='pool')
                pc = PPO['ml'] + (h * 2 + cc) * 5
                TS(acc[0:96, 0:T], upad[0:96, 0:T], pp[0:96, pc:pc + 1], None, ALU.mult, None, [uk, 'pp'], ['mb2'])
                for j in range(1, 4):
                    STT(acc[0:96, 0:T], upad[0:96, j:j + T], pp[0:96, pc + j:pc + j + 1], acc[0:96, 0:T],
                        ALU.mult, ALU.add, [uk, 'mb2', 'pp'], ['mb2'])
                ACT(ucb[0:96, cc, :], acc[0:96, 0:T], AF.Silu, ['mb2', 'pp'], ['hba'], bias=pp[0:96, pc + 4:pc + 5])
                if cfg.get('verbose'):
                    print("count at mlh_a1", P.count)
                if stop == 'mlh_a1':
                    return
                for tb in range(4):
                    sl = slice(tb * 512, (tb + 1) * 512)
                    pt, pk = PSF()
                    MM(pt[0:96, :], mlw[0:96, 0, cc, :], ucb[0:96, cc, sl], True, True, ['mlw', 'hba'], [pk])
                    CP(qTb[0:96, cc, sl], pt[0:96, :], [pk], ['hbq'], eng='act')
                    pt, pk = PSF()
                    MM(pt[0:96, :], mlw[0:96, 1, cc, :], ucb[0:96, cc, sl], True, True, ['mlw', 'hba'], [pk])
                    ACT(kTb[0:96, cc, sl], pt[0:96, :], AF.Identity, [pk], ['hbk'], scale=192.0 ** -0.5)
            if stop == 'mlh_a':
                return
            MEMSET(vaug[:, :, 192:193], 1.0, ['vaug'])
            for c_ in range(16):
                sl = slice(c_ * 128, (c_ + 1) * 128)
                pt, pk = PSF()
                for cc in range(2):
                    MM(pt[:, cc * 96:(cc + 1) * 96], mbb[cc][0:96, sl], mlw[0:96, 2, cc, :], True, True,
                       [f"mbb{cc}", 'mlw'], [pk])
                CP(vaug[:, c_, 0:192], pt[:, 0:192], [pk], ['vaug'], eng=('act' if c_ % 2 else 'dve'))
            wo_, wok = load_w(w_in[l][:, SEG['mo'] + h * 192: SEG['mo'] + (h + 1) * 192], 192)
            wz_, wzk = load_w(w_in[l][:, SEG['mz'] + h * 192: SEG['mz'] + (h + 1) * 192], 192)
            zsb = mb[4][:, 0:1536].bitcast(BF16).rearrange("p (c e) -> p c e", c=16)
            MEMSET(Cst[0:96, :, :], 0.0, ['Cst'])
            MEMSET(Cb[0:96, :, :], 0.0, ['Cb'])
            for c_ in range(16):
                sl = slice(c_ * 128, (c_ + 1) * 128)
                col = lambda q: colbuf[:, c_, q * NM + h:q * NM + h + 1]
                pt, pk = PSF()
                for cc in range(2):
                    MM(pt[:, cc * 96:(cc + 1) * 96], ucb[0:96, cc, sl], mlw[0:96, 1, cc, :], True, True,
                       ['hba', 'mlw'], [pk])
                ACT(kwb[:, 0:192], pt[:, 0:192], AF.Identity, [pk, 'colbuf'], ['kwb'], scale=col(1))
                pS, kS = PSF()
                for cc in range(2):
                    MM(pS[:, 0:128], kTb[0:96, cc, sl], qTb[0:96, cc, sl], cc == 0, cc == 1, ['hbk', 'hbq'], [kS])
                STT(Stb[:, :], pS[:, 0:128], col(0), tri_f, ALU.mult, ALU.mult, [kS, 'colbuf', 'cf'], ['Stb'])
                pH, kH = PSF()
                MM(pH[:, 0:193], Stb[:, :], vaug[:, c_, :], True, True, ['Stb', 'vaug'], [kH])
                ACT(Hs[:, :], pH[:, 0:193], AF.Identity, [kH, 'colbuf'], ['Hs'], scale=col(4))
                if c_ > 0:
                    pI, kI = PSF()
                    for cc in range(2):
                        MM(pI[:, 0:193], qTb[0:96, cc, sl], Cb[0:96, cc, :], cc == 0, cc == 1, ['hbq', 'Cb'], [kI])
                    STT(comb[:, :], pI[:, 0:193], col(2), Hs[:, :], ALU.mult, ALU.add, [kI, 'colbuf', 'Hs'], ['comb'])
                    cm, ck = comb, 'comb'
                else:
                    cm, ck = Hs, 'Hs'
                TS(dn[:, 3:4], cm[:, 192:193], -1.0, None, ALU.mult, None, [ck], ['dn3'])
                TT(dn[:, 3:4], dn[:, 3:4], cm[:, 192:193], ALU.max, [ck, 'dn3'], ['dn3'])
                TT(dn[:, 0:1], dn[:, 3:4], col(3), ALU.max, ['dn3', 'colbuf'], ['dn'])
                RECIP(dn[:, 1:2], dn[:, 0:1], ['dn'], ['dn1'])
                pO, kO = proj_tm(wo_, wok, 0, 192, c_)
                ACT(sob[:, :], pO[:, 0:192], AF.Sigmoid, [kO], ['sob'])
                STT(hsb[:, c_, :], cm[:, 0:192], dn[:, 1:2], sob[:, :], ALU.mult, ALU.mult, [ck, 'dn1', 'sob'], ['hsb'])
                P.add('dve', lambda e, c_=c_: e.bn_stats(st6[:, :], hsb[:, c_, :]), ['hsb'], ['st6'])
                P.add('dve', lambda e, c_=c_: e.bn_aggr(mv[:, c_, :], st6[:, :]), ['st6'], ['mv'])
                pZ, kZ = proj_tm(wz_, wzk, 0, 192, c_)
                ACT(zsb[:, c_, :], pZ[:, 0:192], AF.Silu, [kZ], ['mb4'])
                if c_ < 15:
                    for cc in range(2):
                        pU, kU = PSF()
                        MM(pU[0:96, 0:193], kwb[:, cc * 96:(cc + 1) * 96], vaug[:, c_, :], True, True,
                           ['kwb', 'vaug'], [kU])
                        STT(Cst[0:96, cc, :], Cst[0:96, cc, :], soldbc[0:96, h * 16 + c_:h * 16 + c_ + 1],
                            pU[0:96, 0:193], ALU.mult, ALU.add, ['Cst', 'soldbc', kU], ['Cst'])
                    CP(Cb[0:96, :, :], Cst[0:96, :, :], ['Cst'], ['Cb'], eng='pool')
            if stop == 'mlh_c':
                return
            rs = sm[:, 128:144]
            ACT(rs, mv[:, :, 1], AF.Sqrt, ['mv', 'cf'], ['sm_rs'], bias=eps5)
            RECIP(rs, rs, ['sm_rs'], ['sm_rs'])
            for c_ in range(16):
                TS(hsb[:, c_, :], hsb[:, c_, :], mv[:, c_, 0:1], rs[:, c_:c_ + 1], ALU.subtract, ALU.mult,
                   ['hsb', 'mv', 'sm_rs'], ['hsb'])
            ngv = ngbc[:, h * 192:(h + 1) * 192].unsqueeze(1).to_broadcast([128, 16, 192])
            TT(hsb[:, :, :], hsb[:, :, :], ngv, ALU.mult, ['hsb', 'ngbc'], ['hsb'])
            ytm = hbk[:, :, :].rearrange("p a t -> p (a t)")[:, 0:16 * 192].rearrange("p (c e) -> p c e", c=16)
            TT(ytm, hsb[:, :, :], zsb, ALU.mult, ['hsb', 'mb4'], ['hbk'])
            yTb = hbq
            for cc in range(2):
                for half in range(2):
                    pb, pk = PSB()
                    for kk in range(8):
                        c_ = half * 8 + kk
                        TR(pb[0:96, kk * 128:(kk + 1) * 128], ytm[:, c_, cc * 96:(cc + 1) * 96], identb[:, :],
                           ['hbk', 'identb'], [pk])
                    CP(yTb[0:96, cc, half * 1024:(half + 1) * 1024], pb[0:96, :], [pk], ['hbq'],
                       eng=('act' if half else 'dve'))
                DMA(yT_d[l, ch0 + cc, 0:96, :], yTb[0:96, cc, :], ['hbq'], ['yTd'])

        for h in range(NM):
            ml_head(h)
            if stop in ('mlh1', 'mlh_a', 'mlh_c', 'mlh_a0', 'mlh_a1'):
                return
        if stop == 'ml':
            return

        def mo_head(h):
            chn = NL + 2 * NM + h
            qf, kf, zs = mb[0], mb[1], mb[2]
            qb = hba[:, 0, :]
            kb = hba[:, 1, :]
            wq_, kq = load_w(w_in[l][:, SEG['oq'] + h * 128: SEG['oq'] + (h + 1) * 128], 128)
            for tb in range(4):
                sl = slice(tb * 512, (tb + 1) * 512)
                pt, pk = proj_cm(wq_, kq, 0, 128, tb)
                CP(qf[:, sl], pt[:, :], [pk], ['mb0'], eng='act')
                CP(qb[:, sl], qf[:, sl], ['mb0'], ['hba'], eng='pool')
            wk_, kk_ = load_w(w_in[l][:, SEG['ok'] + h * 128: SEG['ok'] + (h + 1) * 128], 128)
            for tb in range(4):
                sl = slice(tb * 512, (tb + 1) * 512)
                pt, pk = proj_cm(wk_, kk_, 0, 128, tb)
                CP(kf[:, sl], pt[:, :], [pk], ['mb1'], eng='act')
                CP(kb[:, sl], kf[:, sl], ['mb1'], ['hba'], eng='pool')
            wz_, kz = load_w(w_in[l][:, SEG['oz'] + h * 128: SEG['oz'] + (h + 1) * 128], 128)
            for tb in range(4):
                sl = slice(tb * 512, (tb + 1) * 512)
                pt, pk = proj_cm(wz_, kz, 0, 128, tb)
                ACT(zs[:, sl], pt[:, :], AF.Silu, [pk], ['mb2'])
            wv_, kv = load_w(w_in[l][:, SEG['ov'] + h * 128: SEG['ov'] + (h + 1) * 128], 128)
            MEMSET(vaug[:, :, 128:129], 1.0, ['vaug'])
            for st in range(16):
                pt, pk = proj_tm(wv_, kv, 0, 128, st)
                CP(vaug[:, st, 0:128], pt[:, 0:128], [pk], ['vaug'], eng=('act' if st % 2 else 'dve'))
            P.add('dve', lambda e: e.tensor_reduce(km[:, 0:8], kf[:, 0:T].rearrange("p (b s) -> p b s", s=256),
                                                   AX.X, ALU.add), ['mb1'], ['km'])
            TS(km[:, 0:8], km[:, 0:8], 1.0 / 256, None, ALU.mult, None, ['km'], ['km'])
            et = etab[:, h * 256:(h + 1) * 256].rearrange("p (q s) -> p q s", q=16)
            for qt in range(16):
                bq = qt // 2
                if bq == 0:
                    continue
                if bq >= 4:
                    pg, kg = PSF()
                    MM(pg[:, 0:8], qf[:, qt * 128:(qt + 1) * 128], km[:, 0:8], True, True, ['mb0', 'km'], [kg])
                    TT(gm[:, :], pg[:, 0:8], negmask[:, bq * 8:(bq + 1) * 8], ALU.add, [kg, 'cf'], ['gm'])
                    P.add('dve', lambda e: e.max(top8[:, :], gm[:, :]), ['gm'], ['top8'])
                    TS(sel[:, :], gm[:, :], top8[:, 2:3], None, ALU.is_ge, None, ['gm', 'top8'], ['sel'])
                    TT(Fq[:, qt, :].rearrange("p (j k) -> p j k", k=2), et[:, qt, :].rearrange("p (j k) -> p j k", k=2),
                       sel[:, :].unsqueeze(2).to_broadcast([128, 8, 2]), ALU.mult, ['sel', 'cf'], ['Fq'])
                else:
                    CP(Fq[:, qt, :], et[:, qt, :], ['cf'], ['Fq'])
            acc = hsb[:, :, 0:129]
            sc = 128.0 ** -0.5
            bias = slopecol[:, h:h + 1]
            pctr_t = [0]
            for bq in range(8):
                qs = slice(bq * 256, (bq + 1) * 256)
                for kt in range(2):
                    st = 2 * bq + kt
                    pS, kS = PSF()
                    MM(pS[:, 0:256], kb[:, st * 128:(st + 1) * 128], qb[:, qs], True, True, ['hba'], [kS])
                    ACT(Ptd[kt][:, :], pS[:, 0:256], AF.Exp, [kS, 'cf'], [f"Ptd{kt}"], scale=sc, bias=bias)
                    TT(Ptd[kt][:, :], Ptd[kt][:, :], gdiag[:, (h * 2 + kt) * 256:(h * 2 + kt + 1) * 256], ALU.mult,
                       [f"Ptd{kt}", 'gdiag'], [f"Ptd{kt}"], eng='pool')
                pO, kO = PSF()
                MM(pO[:, 0:129], Ptd[0][:, 0:128], vaug[:, 2 * bq, 0:129], True, True, ['Ptd0', 'vaug'], [kO])
                CP(acc[:, 2 * bq, :], pO[:, 0:129], [kO], ['hsb'], eng='act')
                pO, kO = PSF()
                MM(pO[:, 0:129], Ptd[0][:, 128:256], vaug[:, 2 * bq, 0:129], True, False, ['Ptd0', 'vaug'], [kO])
                MM(pO[:, 0:129], Ptd[1][:, 128:256], vaug[:, 2 * bq + 1, 0:129], False, True, ['Ptd1', 'vaug'], [kO])
                CP(acc[:, 2 * bq + 1, :], pO[:, 0:129], [kO], ['hsb'], eng='act')
                for j in range(bq):
                    for kt in range(2):
                        st = 2 * j + kt
                        pi = pctr_t[0] % 3
                        pctr_t[0] += 1
                        pS, kS = PSF()
                        MM(pS[:, 0:256], kb[:, st * 128:(st + 1) * 128], qb[:, qs], True, True, ['hba'], [kS])
                        ACT(Ptb[pi][:, :], pS[:, 0:256], AF.Exp, [kS, 'cf'], [f"Ptb{pi}"], scale=sc, bias=bias)
                        for qh in range(2):
                            qt = 2 * bq + qh
                            pO, kO = PSF()
                            MM(pO[:, 0:129], Ptb[pi][:, qh * 128:(qh + 1) * 128], vaug[:, st, 0:129], True, True,
                               [f"Ptb{pi}", 'vaug'], [kO])
                            STT(acc[:, qt, :], pO[:, 0:129], Fq[:, qt, st:st + 1], acc[:, qt, :], ALU.mult, ALU.add,
                                [kO, 'Fq', 'hsb'], ['hsb'])
            yb = hbq[:, 0, :]
            for qt in range(16):
                RECIP(dn[:, 2:3], acc[:, qt, 128:129], ['hsb'], ['dn2'])
                TS(ofb[:, :], acc[:, qt, 0:128], dn[:, 2:3], None, ALU.mult, None, ['hsb', 'dn2'], ['ofb'])
                pT, kT = PSF()
                TR(pT[:, 0:128], ofb[:, :], ident_f, ['ofb', 'cf'], [kT])
                TT(yb[:, qt * 128:(qt + 1) * 128], pT[:, 0:128], zs[:, qt * 128:(qt + 1) * 128], ALU.mult,
                   [kT, 'mb2'], ['hbq'])
            DMA(yT_d[l, chn, :, :], yb, ['hbq'], ['yTd'])

        for h in range(NO):
            mo_head(h)

    chunks = []
    for g in range(NL):
        chunks.append((g * 128, 128))
    for h in range(NM):
        for cc in range(2):
            chunks.append((512 + h * 192 + cc * 96, 96))
    for h in range(NO):
        chunks.append((1280 + h * 128, 128))

    def out_phase(l, xsrc, xdst, final):
        A.off = persist_mark
        P.barrier()
        wout = A.alloc([128, NCH, D], BF16)
        wq = A.alloc([128, KD, 512], BF16)
        wo = A.alloc([128, 4, D], BF16)
        hmT = A.alloc([128, KD, MEM], BF16)
        kT = A.alloc([128, 4, MEM], BF16)
        vx = A.alloc([128, 2, 4, 129], BF16)
        xblk = A.alloc([128, 2, D])
        yblk = A.alloc([128, NCH, 256], BF16)
        hx = A.alloc([128, 2, D], BF16)
        hxT = A.alloc([128, KD, 256], BF16)
        qT = A.alloc([128, 4, 256], BF16)
        PT = A.alloc([128, 2, 256], BF16)
        oh = A.alloc([128, 2, 512], BF16)
        ohT = A.alloc([128, 4, 256], BF16)
        gbx = A.alloc([128, D])
        gbf = A.alloc([128, D])
        rd = A.alloc([128, 2])
        for ci, (r0, K) in enumerate(chunks):
            DMA(wout[0:K, ci, :], w_out[l, r0:r0 + K, :], (), ['wout'], eng='pool')
        DMA(wq[:, :, :], xa_wq[l].rearrange("(k p) c -> p k c", p=128), (), ['wq'], eng='pool')
        DMA(wo[:, :, :], xa_wo[l].rearrange("(h p) n -> p h n", p=128), (), ['wo'], eng='pool')
        DMA(gbx[:, :], gains[l, 2].partition_broadcast(128), (), ['gbx'])
        for i in range(2):
            DMA(xblk[:, i, :], mem_in[i * 128:(i + 1) * 128, :], (), ['xblk'])
            norm_tile(xblk[:, i, :], 'xblk', gbx[:, :], 'gbx', hx[:, i, :], 'hx', hx[:, i, :], 'hx')
            transpose_16(hx[:, i, :], 'hx', lambda half, i=i: hmT[:, half * 8:(half + 1) * 8, i * 128:(i + 1) * 128],
                         'hmT')
        wkv = xblk[:, :, :].rearrange("p a d -> p (a d)").bitcast(BF16).rearrange("p (k c) -> p k c", k=KD)
        DMA(wkv, xa_wkv[l][:, 0:512].rearrange("(k p) c -> p k c", p=128), (), ['xblk'], eng='pool')
        for h in range(4):
            pt, pk = PSF()
            for k in range(KD):
                MM(pt[:, 0:MEM], wkv[:, k, h * 128:(h + 1) * 128], hmT[:, k, :], k == 0, k == KD - 1, ['xblk', 'hmT'], [pk])
            CP(kT[:, h, :], pt[:, 0:MEM], [pk], ['kT'], eng='act')
        DMA(wkv, xa_wkv[l][:, 512:1024].rearrange("(k p) c -> p k c", p=128), (), ['xblk'], eng='pool')
        MEMSET(vx[:, :, :, 128:129], 1.0, ['vx'])
        for mt in range(2):
            pt, pk = PSF()
            for k in range(KD):
                MM(pt[:, 0:512], hmT[:, k, mt * 128:(mt + 1) * 128], wkv[:, k, :], k == 0, k == KD - 1, ['xblk', 'hmT'], [pk])
            CP(vx[:, mt, :, 0:128], pt[:, 0:512].rearrange("p (h e) -> p h e", h=4), [pk], ['vx'])
        DMA(gbx[:, :], gains[l, 1].partition_broadcast(128), ['gbx'], ['gbx'])
        if final:
            DMA(gbf[:, :], final_g.partition_broadcast(128), (), ['gbf'])
        sc = 128.0 ** -0.5
        for tb in range(8):
            rows = slice(tb * 256, (tb + 1) * 256)
            c1, c2 = NL, NL + 2 * NM
            DMA(yblk[:, 0:c1, :], yT_d[l, 0:c1, :, rows].rearrange("c p t -> p c t"), ['yTd'], ['yblk'])
            DMA(yblk[0:96, c1:c2, :], yT_d[l, c1:c2, 0:96, rows].rearrange("c p t -> p c t"), ['yTd'], ['yblk'])
            DMA(yblk[:, c2:NCH, :], yT_d[l, c2:NCH, :, rows].rearrange("c p t -> p c t"), ['yTd'], ['yblk'])
            DMA(xblk[:, :, :], xsrc[rows, :].rearrange("(a p) d -> p a d", p=128), (), ['xblk'])
            for tt in range(2):
                for nb in range(4):
                    ns = slice(nb * 512, (nb + 1) * 512)
                    pt, pk = PSF()
                    for ci, (r0, K) in enumerate(chunks):
                        MM(pt[:, :], yblk[0:K, ci, tt * 128:(tt + 1) * 128], wout[0:K, ci, ns], ci == 0, ci == NCH - 1,
                           ['yblk', 'wout'], [pk])
                    TT(xblk[:, tt, ns], xblk[:, tt, ns], pt[:, :], ALU.add, ['xblk', pk], ['xblk'])
            if cfg.get('no_xa', False):
                DMA(xdst[rows, :].rearrange("(a p) d -> p a d", p=128), xblk[:, :, :], ['xblk'], ['xdst'])
                continue
            for tt in range(2):
                norm_tile(xblk[:, tt, :], 'xblk', gbx[:, :], 'gbx', hx[:, tt, :], 'hx', hx[:, tt, :], 'hx')
                transpose_16(hx[:, tt, :], 'hx',
                             lambda half, tt=tt: hxT[:, half * 8:(half + 1) * 8, tt * 128:(tt + 1) * 128], 'hxT')
            for h in range(4):
                pt, pk = PSF()
                for k in range(KD):
                    MM(pt[:, 0:256], wq[:, k, h * 128:(h + 1) * 128], hxT[:, k, :], k == 0, k == KD - 1, ['wq', 'hxT'], [pk])
                CP(qT[:, h, :], pt[:, 0:256], [pk], ['qT'], eng='act')
            for h in range(4):
                for mt in range(2):
                    pS, kS = PSF()
                    MM(pS[:, 0:256], kT[:, h, mt * 128:(mt + 1) * 128], qT[:, h, :], True, True, ['kT', 'qT'], [kS])
                    ACT(PT[:, mt, :], pS[:, 0:256], AF.Exp, [kS], ['PT'], scale=sc)
                for tt in range(2):
                    pO, kO = PSF()
                    for mt in range(2):
                        MM(pO[:, 0:129], PT[:, mt, tt * 128:(tt + 1) * 128], vx[:, mt, h, :], mt == 0, mt == 1,
                           ['PT', 'vx'], [kO])
                    RECIP(rd[:, 0:1], pO[:, 128:129], [kO], ['rd'])
                    TS(oh[:, tt, h * 128:(h + 1) * 128], pO[:, 0:128], rd[:, 0:1], None, ALU.mult, None, [kO, 'rd'], ['oh'])
            for tt in range(2):
                pb, pk = PSB()
                for h in range(4):
                    TR(pb[:, h * 128:(h + 1) * 128], oh[:, tt, h * 128:(h + 1) * 128], identb[:, :], ['oh', 'identb'], [pk])
                CP(ohT[:, :, tt * 128:(tt + 1) * 128], pb[:, 0:512].rearrange("p (h t) -> p h t", h=4), [pk], ['ohT'])
            for tt in range(2):
                for nb in range(4):
                    ns = slice(nb * 512, (nb + 1) * 512)
                    pt, pk = PSF()
                    for h in range(4):
                        MM(pt[:, :], ohT[:, h, tt * 128:(tt + 1) * 128], wo[:, h, ns], h == 0, h == 3, ['ohT', 'wo'], [pk])
                    TT(xblk[:, tt, ns], xblk[:, tt, ns], pt[:, :], ALU.add, ['xblk', pk], ['xblk'])
            if final:
                for tt in range(2):
                    norm_tile(xblk[:, tt, :], 'xblk', gbf[:, :], 'gbf', xblk[:, tt, :], 'xblk', hx[:, tt, :], 'hx')
            DMA(xdst[rows, :].rearrange("(a p) d -> p a d", p=128), xblk[:, :, :], ['xblk'], ['xdst'])

    src = x_in
    for l in range(nlayers):
        mix_phase(l, src)
        if stop in ('A', 'lru', 'mlg', 'mlh1', 'mlh_a', 'mlh_c', 'mlh_a0', 'mlh_a1', 'ml', 'mo'):
            break
        last = (l == nlayers - 1)
        out_phase(l, src, out_d if last else xs0, final=(last and nlayers == L))
        src = xs0
        if stop == 'out':
            break
    P.emit()
    return nc, es


def alibi_slopes(n):
    def pow2(m):
        start = 2.0 ** (-8.0 / m)
        return [start ** (i + 1) for i in range(m)]
    if math.log2(n).is_integer():
        s = pow2(n)
    else:
        c = 2 ** int(math.floor(math.log2(n)))
        s = pow2(c) + pow2(2 * c)[0::2][:n - c]
    return np.array(s, dtype=np.float64)


def make_consts(mo_heads):
    NO = len(mo_heads)
    CO, NCF = const_layout(NO)
    cf = np.zeros((128, NCF), np.float32)
    cf[:, CO['ident']:CO['ident'] + 128] = np.eye(128, dtype=np.float32)
    s = np.arange(128)[:, None]
    j = np.arange(128)[None, :]
    cf[:, CO['tri']:CO['tri'] + 128] = (s <= j).astype(np.float32)
    nm = np.zeros((8, 8), np.float32)
    for bq in range(8):
        nm[bq, bq:] = NEG
    cf[:, CO['negmask']:CO['negmask'] + 64] = nm.reshape(1, 64)
    slopes = alibi_slopes(6)
    p = np.arange(128, dtype=np.float64)
    gd = np.zeros((128, NO, 2, 256), np.float64)
    for hi, hg in enumerate(mo_heads):
        sl = slopes[hg]
        cf[:, CO['slope'] + hi] = (sl * (p - 127.0)).astype(np.float32)
        et = np.zeros((128, 16, 16), np.float64)
        for qt in range(16):
            bq = qt // 2
            for st in range(2 * bq):
                t = qt * 128 + p
                sref = st * 128 + 127
                et[:, qt, st] = np.exp(-sl * (t - sref))
        cf[:, CO['etab'] + hi * 256: CO['etab'] + (hi + 1) * 256] = et.reshape(128, 256).astype(np.float32)
        for kt in range(2):
            sabs = kt * 128 + p[:, None]
            sref = kt * 128 + 127
            t = np.arange(256, dtype=np.float64)[None, :]
            g = np.exp(-sl * (t - sref)) * (sabs <= t)
            gd[:, hi, kt, :] = g
    cf[:, CO['misc'] + 0] = 1e-6
    cf[:, CO['misc'] + 1] = 1e-5
    cf[:, CO['misc'] + 2] = 1.0
    gd = np.minimum(gd, 3.0e38).astype(np.float32).reshape(128, NO * 512).astype(ml_dtypes.bfloat16)
    return cf, gd


def pack_core(inp, b, lru_blocks, ml_heads, mo_heads):
    NL, NM, NO = len(lru_blocks), len(ml_heads), len(mo_heads)
    PPO, NPP = pp_layout(NL, NM)
    w_in = inp['w_in']
    o_lx, o_lz, o_mu, o_mo, o_mz, o_mi, o_mf, o_oq, o_ok, o_ov, o_oz = (
        0, 512, 1024, 1792, 2560, 3328, 3332, 3336, 4104, 4872, 5640)
    cols = []
    for base in (o_lx, o_lz):
        for g in lru_blocks:
            cols.append(np.arange(base + g * 128, base + (g + 1) * 128))
    for base in (o_mu, o_mo, o_mz):
        for h in ml_heads:
            cols.append(np.arange(base + h * 192, base + (h + 1) * 192))
    for base in (o_mi, o_mf):
        cols.append(np.array([base + h for h in ml_heads]))
    for base in (o_oq, o_ok, o_ov, o_oz):
        for h in mo_heads:
            cols.append(np.arange(base + h * 128, base + (h + 1) * 128))
    cols = np.concatenate(cols)
    full = (len(cols) == w_in.shape[2]) and np.array_equal(cols, np.arange(w_in.shape[2]))
    w_in_c = w_in if full else np.ascontiguousarray(w_in[:, :, cols])
    pp = np.zeros((L, 128, NPP), np.float32)
    for l in range(L):
        for gi, g in enumerate(lru_blocks):
            sl = slice(g * 128, (g + 1) * 128)
            o = PPO['lru'] + gi * 8
            pp[l, :, o:o + 4] = inp['lru_conv_w'][l][:, sl].T
            pp[l, :, o + 4] = inp['lru_conv_b'][l][sl]
            pp[l, :, o + 5] = inp['lru_ba'][l][sl]
            pp[l, :, o + 6] = inp['lru_bx'][l][sl]
            pp[l, :, o + 7] = inp['lru_lambda'][l][sl]
        for hi, h in enumerate(ml_heads):
            for cc in range(2):
                sl = slice(h * 192 + cc * 96, h * 192 + (cc + 1) * 96)
                o = PPO['ml'] + (hi * 2 + cc) * 5
                pp[l, 0:96, o:o + 4] = inp['ml_conv_w'][l][:, sl].T
                pp[l, 0:96, o + 4] = inp['ml_conv_b'][l][sl]
            pp[l, hi, PPO['gb']] = inp['ml_bi'][l][h]
            pp[l, hi, PPO['gb'] + 1] = -inp['ml_bf'][l][h]
    lru_w = np.stack([inp['lru_wa'][:, lru_blocks], inp['lru_wx'][:, lru_blocks]], axis=1)
    ml_w = np.zeros((L, 3, NM, 2, 96, 96), np.float32)
    for ai, nm_ in enumerate(('ml_wq', 'ml_wk', 'ml_wv')):
        w = inp[nm_]
        for hi, h in enumerate(ml_heads):
            for cc in range(2):
                for bl in range(24):
                    gidx = h * 48 + cc * 24 + bl
                    ml_w[:, ai, hi, cc, bl * 4:(bl + 1) * 4, bl * 4:(bl + 1) * 4] = w[:, gidx]
    ml_ng = np.concatenate([inp['ml_norm_g'][:, h * 192:(h + 1) * 192] for h in ml_heads], axis=1)
    gains = np.stack([inp['mix_norm_g'], inp['xa_norm_g'], inp['mem_norm_g']], axis=1)
    return {
        "x": np.ascontiguousarray(inp['x'][b]), "mem": np.ascontiguousarray(inp['mem'][b]),
        "w_in": w_in_c, "w_out": inp['w_out'], "gains": np.ascontiguousarray(gains),
        "final_g": inp['final_norm_g'], "pp": pp, "lru_w": np.ascontiguousarray(lru_w),
        "ml_w": ml_w, "ml_ng": np.ascontiguousarray(ml_ng), "xa_wq": inp['xa_wq'], "xa_wkv": inp['xa_wkv'],
        "xa_wo": inp['xa_wo'],
    }


def kernel(**inputs):
    inp = {k: np.asarray(v) for k, v in inputs.items()}
    cfg = dict(NL=4, NM=4, NO=6)
    nc, es = build(cfg)
    cf, gd = make_consts(list(range(6)))
    in_maps = []
    for c in range(8):
        m = pack_core(inp, c // 2, [0, 1, 2, 3], [0, 1, 2, 3], [0, 1, 2, 3, 4, 5])
        m["cst_f"] = cf
        m["cst_b"] = gd
        in_maps.append(m)
    res = run_bass_kernel_spmd(nc, in_maps, core_ids=list(range(8)))
    out = np.stack([res.results[2 * b]["out"] for b in range(4)], axis=0)
    return out.astype(np.float32)
```

```python
import math
import numpy as np
import ml_dtypes
from contextlib import ExitStack
import concourse.bass as bass
import concourse.mybir as mybir
from concourse.bass_utils import run_bass_kernel_spmd

F32 = mybir.dt.float32
BF16 = mybir.dt.bfloat16
AF = mybir.ActivationFunctionType
ALU = mybir.AluOpType
AX = mybir.AxisListType

T = 2048
D = 2048
KD = 16
MEM = 256
L = 2
ENG = ('pe', 'act', 'dve', 'pool', 'sp')
NEG = -1.0e30
ARENA_WORDS = 52400


class Op:
    __slots__ = ('eng', 'fn', 'deps', 'signal', 'sig', 'sigval', 'dma', 'sem', 'semval')


class Prog:
    EPOCH = 6000
    NQ = 12

    def __init__(self, nc, es):
        self.nc = nc
        self.es = es
        self.ops = {e: [] for e in ENG}
        self.state = {}
        self.dma_cnt = {e: 0 for e in ENG}
        self.dma_last = {e: {} for e in ENG}
        self.dma_sems = {}
        self.eng_sems = {}
        self.pending = {e: set() for e in ENG}

    def _sem(self, name):
        return self.es.enter_context(self.nc.semaphore(name))

    limit = None
    count = 0

    def add(self, eng, fn, r=(), w=(), dma=False):
        self.count += 1
        if self.limit is not None and self.count > self.limit:
            return None
        op = Op()
        op.eng = eng; op.fn = fn; op.dma = dma; op.signal = False
        op.sig = 0; op.sigval = 0; op.sem = None; op.semval = 0
        deps = set(self.pending[eng])
        self.pending[eng] = set()
        for k in r:
            st = self.state.get(k)
            if st is not None and st[0] is not None:
                deps.add(st[0])
        for k in w:
            st = self.state.get(k)
            if st is not None:
                if st[0] is not None:
                    deps.add(st[0])
                deps.update(st[1].values())
        for k in r:
            rd = self.state.setdefault(k, [None, {}])[1]
            if dma:
                rd[('dma', id(op))] = op
            else:
                rd[eng] = op
        for k in w:
            self.state[k] = [op, {}]
        if dma:
            if eng not in self.dma_sems:
                self.dma_sems[eng] = [self._sem(f"dq_{eng}_{i}") for i in range(self.NQ)]
            i = self.dma_cnt[eng]
            self.dma_cnt[eng] += 1
            slot = i % self.NQ
            op.sem = self.dma_sems[eng][slot]
            op.semval = 16 * (i // self.NQ + 1)
            prev = self.dma_last[eng].get(slot)
            if prev is not None:
                deps.add(prev)
            self.dma_last[eng][slot] = op
        deps.discard(op)
        fin = []
        for d in deps:
            if (not d.dma) and (not dma) and d.eng == 'pe' and eng == 'pe':
                continue
            if not d.dma:
                d.signal = True
            fin.append(d)
        op.deps = fin
        self.ops[eng].append(op)
        return op

    def barrier(self):
        last = []
        for e in ENG:
            comp = [o for o in self.ops[e] if not o.dma]
            if comp:
                last.append(comp[-1])
            last.extend(self.dma_last[e].values())
        for e in ENG:
            self.pending[e].update(last)

    def emit(self):
        for e in ENG:
            cnt = 0
            for op in self.ops[e]:
                if (not op.dma) and op.signal:
                    op.sig = cnt // self.EPOCH
                    op.sigval = cnt % self.EPOCH + 1
                    cnt += 1
                    if (e, op.sig) not in self.eng_sems:
                        self.eng_sems[(e, op.sig)] = self._sem(f"es_{e}_{op.sig}")
        prog = self

        def run(e, h):
            waited = {}
            for op in prog.ops[e]:
                need = {}
                for d in op.deps:
                    if d.dma:
                        s, v = d.sem, d.semval
                    else:
                        s, v = prog.eng_sems[(d.eng, d.sig)], d.sigval
                    if need.get(id(s), (None, 0))[1] < v:
                        need[id(s)] = (s, v)
                for k, (s, v) in need.items():
                    if waited.get(k, 0) < v:
                        h.wait_ge(s, v)
                        waited[k] = v
                ins = op.fn(h)
                if op.dma:
                    ins.then_inc(op.sem, 16)
                elif op.signal:
                    ins.then_inc(prog.eng_sems[(e, op.sig)], 1)
            if e == 'sp':
                for q in ENG:
                    for slot, o in prog.dma_last[q].items():
                        h.wait_ge(o.sem, o.semval)

        with self.nc.Block() as block:
            @block.tensor
            def _(h):
                run('pe', h)

            @block.scalar
            def _(h):
                run('act', h)

            @block.vector
            def _(h):
                run('dve', h)

            @block.gpsimd
            def _(h):
                run('pool', h)

            @block.sync
            def _(h):
                run('sp', h)


class Arena:
    def __init__(self, t, n):
        self.t = t; self.n = n; self.off = 0

    def alloc(self, shape, dt=F32):
        nel = 1
        for s in shape[1:]:
            nel *= s
        words = nel if dt == F32 else (nel + 1) // 2
        words = (words + 1) // 2 * 2
        assert self.off + words <= self.n, f"arena overflow {self.off}+{words}>{self.n}"
        v = self.t[0:shape[0], self.off:self.off + words]
        self.off += words
        if dt != F32:
            v = v.bitcast(dt)
        v = v[:, 0:nel]
        if len(shape) == 3:
            v = v.rearrange("p (a b) -> p a b", a=shape[1])
        elif len(shape) == 4:
            v = v.rearrange("p (a b c) -> p a b c", a=shape[1], b=shape[2])
        return v


def seg_offsets(NL, NM, NO):
    sizes = [('lx', NL * 128), ('lz', NL * 128), ('mu', NM * 192), ('mo', NM * 192), ('mz', NM * 192),
             ('mi', NM), ('mf', NM), ('oq', NO * 128), ('ok', NO * 128), ('ov', NO * 128), ('oz', NO * 128)]
    off = {}
    o = 0
    for n, s in sizes:
        off[n] = o
        o += s
    return off, o


def const_layout(NO):
    CO = {}
    o = 0
    for n, s in [('ident', 128), ('tri', 128), ('negmask', 64), ('slope', NO * 2), ('etab', NO * 128), ('misc', 4)]:
        CO[n] = o
        o += s
    return CO, o


def pp_layout(NL, NM):
    PPO = {'lru': 0, 'ml': NL * 8, 'gb': NL * 8 + NM * 10}
    return PPO, NL * 8 + NM * 10 + 2


def build(cfg):
    NL, NM, NO = cfg['NL'], cfg['NM'], cfg['NO']
    nlayers = cfg.get('layers', L)
    stop = cfg.get('stop', 'all')
    dbg = cfg.get('dbg', False)
    SEG, WC = seg_offsets(NL, NM, NO)
    NCH = NL + 2 * NM + NO
    CO, NCF = const_layout(NO)
    PPO, NPP = pp_layout(NL, NM)
    nc = bass.Bass("TRN2", target_bir_lowering=False)
    es = ExitStack()
    P = Prog(nc, es)
    P.limit = cfg.get('limit', None)

    def din(name, shape, dt=F32):
        return nc.dram_tensor(name, list(shape), dt, kind="ExternalInput").ap()

    def dscr(name, shape, dt=F32, out=False):
        return nc.dram_tensor(name, list(shape), dt, kind="ExternalOutput" if out else "Internal").ap()

    x_in = din("x", [T, D])
    mem_in = din("mem", [MEM, D])
    w_in = din("w_in", [L, D, WC])
    w_out = din("w_out", [L, D, D])
    gains = din("gains", [L, 3, D])
    final_g = din("final_g", [D])
    pp_in = din("pp", [L, 128, NPP])
    lru_w = din("lru_w", [L, 2, NL, 128, 128])
    ml_w = din("ml_w", [L, 3, NM, 2, 96, 96])
    ml_ng = din("ml_ng", [L, NM * 192])
    xa_wq = din("xa_wq", [L, D, 512])
    xa_wkv = din("xa_wkv", [L, D, 1024])
    xa_wo = din("xa_wo", [L, 512, D])
    cst_f = din("cst_f", [128, NCF])
    cst_b = din("cst_b", [128, NO * 512], BF16)
    out_d = nc.dram_tensor("out", [T, D], F32, kind="ExternalOutput").ap()
    xs0 = dscr("xs0", [T, D], F32, out=dbg)
    yT_d = dscr("yT", [L, NCH, 128, T], BF16, out=dbg)
    dbg_f = dscr("dbgf", [128, 4096], F32, out=True) if dbg else None

    arena_t = es.enter_context(nc.sbuf_tensor("arena", [128, ARENA_WORDS], F32))
    A = Arena(arena_t, ARENA_WORDS)

    def ps(name, shape, dt=F32):
        return es.enter_context(nc.psum_tensor(name, list(shape), dt))

    def DMA(out, in_, r, w, eng='sp', **kw):
        P.add(eng, lambda e: e.dma_start(out=out, in_=in_, **kw), r, w, dma=True)

    def MM(out, lhsT, rhs, start, stop, r, w, **kw):
        P.add('pe', lambda e: e.matmul(out, lhsT, rhs, start=start, stop=stop, **kw), r, w)

    def TR(out, in_, ident, r, w):
        P.add('pe', lambda e: e.transpose(out, in_, ident), r, w)

    def ACT(out, in_, func, r, w, **kw):
        P.add('act', lambda e: e.activation(out=out, in_=in_, func=func, **kw), r, w)

    def TS(out, in0, s1, s2, op0, op1, r, w, eng='dve', **kw):
        if op1 is None:
            P.add(eng, lambda e: e.tensor_scalar(out, in0, s1, None, op0, **kw), r, w)
        else:
            P.add(eng, lambda e: e.tensor_scalar(out, in0, s1, s2, op0, op1, **kw), r, w)

    def TT(out, in0, in1, op, r, w, eng='dve'):
        P.add(eng, lambda e: e.tensor_tensor(out, in0, in1, op), r, w)

    def STT(out, in0, scalar, in1, op0, op1, r, w, **kw):
        P.add('dve', lambda e: e.scalar_tensor_tensor(out, in0, scalar, in1, op0, op1, **kw), r, w)

    def CP(out, in_, r, w, eng='dve'):
        if eng == 'act':
            ACT(out, in_, AF.Copy, r, w)
        else:
            P.add(eng, lambda e: e.tensor_copy(out, in_), r, w)

    def RECIP(out, in_, r, w):
        P.add('dve', lambda e: e.reciprocal(out, in_), r, w)

    def SCAN(out, d0, d1, init, op0, op1, r, w):
        P.add('dve', lambda e: e.tensor_tensor_scan(out, d0, d1, init, op0, op1), r, w)

    def MEMSET(ap, val, w, eng='pool'):
        P.add(eng, lambda e: e.memset(ap, val), (), w)

    def chv(ap):
        return ap.rearrange("p (c t) -> p c t", t=128)

    cf = A.alloc([128, NCF])
    DMA(cf[:, :], cst_f[:, :], (), ['cf'])
    ident_f = cf[:, CO['ident']:CO['ident'] + 128]
    tri_f = cf[:, CO['tri']:CO['tri'] + 128]
    negmask = cf[:, CO['negmask']:CO['negmask'] + 64]
    slopecol = cf[:, CO['slope']:CO['slope'] + NO * 2]
    etab = cf[:, CO['etab']:CO['etab'] + NO * 128]
    eps6 = cf[:, CO['misc']:CO['misc'] + 1]
    eps5 = cf[:, CO['misc'] + 1:CO['misc'] + 2]
    one1 = cf[:, CO['misc'] + 2:CO['misc'] + 3]
    gdiag = A.alloc([128, NO * 512], BF16)
    DMA(gdiag[:, :], cst_b[:, :], (), ['gdiag'])
    identb = A.alloc([128, 128], BF16)
    CP(identb[:, :], ident_f, ['cf'], ['identb'])
    ones_f = A.alloc([128, 128])
    MEMSET(ones_f[:, :], 1.0, ['ones_f'])
    zeros_f = A.alloc([128, 128])
    MEMSET(zeros_f[:, :], 0.0, ['zeros_f'])
    pp = A.alloc([128, NPP])
    nst = A.alloc([128, 8])
    persist_mark = A.off

    NPS = 6
    psf = [ps(f"psf{i}", [128, 512]) for i in range(NPS)]
    psb = [ps(f"psb{i}", [128, 1024], BF16) for i in range(2)]
    pctr = [0, 0]

    def PSF():
        i = pctr[0] % NPS
        pctr[0] += 1
        return psf[i], f"psf{i}"

    def PSB():
        i = pctr[1] % 2
        pctr[1] += 1
        return psb[i], f"psb{i}"

    def norm_tile(xt_ap, xkey, gbc_ap, gkey, out_ap, okey, junk_ap, jkey):
        STT(junk_ap, xt_ap, 1.0, xt_ap, ALU.mult, ALU.mult, [xkey], [jkey, 'n_ss'], accum_out=nst[:, 0:1])
        ACT(nst[:, 1:2], nst[:, 0:1], AF.Sqrt, ['n_ss', 'cf'], ['n_sd'], scale=1.0 / D, bias=eps6)
        RECIP(nst[:, 2:3], nst[:, 1:2], ['n_sd'], ['n_rstd'])
        STT(out_ap, xt_ap, nst[:, 2:3], gbc_ap, ALU.mult, ALU.mult, [xkey, 'n_rstd', gkey], [okey])

    def transpose_16(hb, hbkey, dst_fn, dkey):
        for half in range(2):
            pb, pk = PSB()
            for kk in range(8):
                k = half * 8 + kk
                TR(pb[:, kk * 128:(kk + 1) * 128], hb[:, k * 128:(k + 1) * 128], identb[:, :],
                   [hbkey, 'identb'], [pk])
            src = pb[:, :].rearrange("p (k t) -> p k t", k=8)
            CP(dst_fn(half), src, [pk], [dkey], eng=('act' if half == 0 else 'dve'))

    def mix_phase(l, src):
        A.off = persist_mark
        P.barrier()
        hT = A.alloc([128, KD, T], BF16)
        mb = [A.alloc([128, T + 8]) for _ in range(5)]
        mbb = [A.alloc([128, T], BF16) for _ in range(2)]
        wbuf = [A.alloc([128, KD, 192], BF16) for _ in range(3)]
        hba = A.alloc([128, 2, T], BF16)
        hbq = A.alloc([128, 2, T], BF16)
        hbk = A.alloc([128, 2, T], BF16)
        vaug = A.alloc([128, 16, 193], BF16)
        hsb = A.alloc([128, 16, 193])
        ngbc = A.alloc([128, NM * 192])
        colbuf = A.alloc([128, 16, 5 * NM])
        soldbc = A.alloc([128, NM * 16])
        soldrow = A.alloc([1, NM * 16])
        sm = A.alloc([128, 160])
        lw = A.alloc([128, 2, 128], BF16)
        mlw = A.alloc([96, 3, 2, 96], BF16)
        Cst = A.alloc([96, 2, 193])
        Cb = A.alloc([96, 2, 193], BF16)
        Stb2 = [A.alloc([128, 128], BF16) for _ in range(2)]
        sob2 = [A.alloc([128, 192]) for _ in range(2)]
        kwb2 = [A.alloc([128, 192], BF16) for _ in range(2)]
        st6 = A.alloc([128, 16, 6])
        dn16 = A.alloc([128, 64])
        mv = A.alloc([128, 16, 2])
        dn = A.alloc([128, 4])
        km = A.alloc([128, 8])
        gm = A.alloc([128, 8])
        top8 = A.alloc([128, 8])
        sel = A.alloc([128, 8])
        Fq = A.alloc([128, 16, 8])
        Ptb = [A.alloc([128, 256], BF16) for _ in range(6)]
        Ptd4 = [A.alloc([128, 256], BF16) for _ in range(4)]
        ofb = A.alloc([128, 128])
        l_c8 = A.alloc([128, 4])

        DMA(pp[:, :], pp_in[l], (), ['pp'])
        DMA(ngbc[:, :], ml_ng[l].partition_broadcast(128), (), ['ngbc'])

        gbc = mb[3][:, 0:D]
        DMA(gbc, gains[l, 0].partition_broadcast(128), (), ['mb3'])
        for i in range(16):
            xt = mb[i % 2][:, 0:D]
            xk = f"mb{i % 2}"
            DMA(xt, src[i * 128:(i + 1) * 128, :], (), [xk])
            hb = mbb[i % 2][:, 0:D]
            hk = f"mbb{i % 2}"
            norm_tile(xt, xk, gbc, 'mb3', hb, hk, hb, hk)
            transpose_16(hb, hk, lambda half, i=i: hT[:, half * 8:(half + 1) * 8, i * 128:(i + 1) * 128],
                         f"hT{i // 4}")
        if stop == 'A':
            DMA(yT_d[l, 0:16].rearrange("c p t -> p c t"), hT[:, :, :], [f"hT{i}" for i in range(4)], ['yTd'])
            return

        wctr = [0]

        def load_w(src2d, ncols):
            i = wctr[0] % 3
            wctr[0] += 1
            wt = wbuf[i]
            key = f"wbuf{i}"
            DMA(wt[:, :, 0:ncols], src2d.rearrange("(k p) c -> p k c", p=128), (), [key], eng='pool')
            return wt, key

        def proj_cm(wt, wkey, c0, M, tb):
            pt, pk = PSF()
            for k in range(KD):
                MM(pt[0:M, 0:512], wt[:, k, c0:c0 + M], hT[:, k, tb * 512:(tb + 1) * 512], k == 0, k == KD - 1,
                   [wkey, f"hT{tb}"], [pk])
            return pt, pk

        def proj_tm(wt, wkey, c0, N, tt):
            pt, pk = PSF()
            for k in range(KD):
                MM(pt[:, 0:N], hT[:, k, tt * 128:(tt + 1) * 128], wt[:, k, c0:c0 + N], k == 0, k == KD - 1,
                   [wkey, f"hT{tt // 4}"], [pk])
            return pt, pk

        def lru_group(g):
            wx_t, wxk = load_w(w_in[l][:, SEG['lx'] + g * 128: SEG['lx'] + (g + 1) * 128], 128)
            wz_t, wzk = load_w(w_in[l][:, SEG['lz'] + g * 128: SEG['lz'] + (g + 1) * 128], 128)
            DMA(lw[:, 0, :], lru_w[l, 0, g], (), ['lw0'], eng='pool')
            DMA(lw[:, 1, :], lru_w[l, 1, g], (), ['lw1'], eng='pool')
            po = PPO['lru'] + g * 8
            xpad = mb[0]
            MEMSET(xpad[:, 0:3], 0.0, ['mb0'])
            for tb in range(4):
                pt, pk = proj_cm(wx_t, wxk, 0, 128, tb)
                CP(xpad[:, 3 + tb * 512: 3 + (tb + 1) * 512], pt[:, :], [pk], ['mb0'], eng='act')
            acc = mb[1]
            TS(acc[:, 0:T], xpad[:, 0:T], pp[:, po:po + 1], pp[:, po + 4:po + 5], ALU.mult, ALU.add,
               ['mb0', 'pp'], ['mb1'])
            for j in range(1, 4):
                STT(acc[:, 0:T], xpad[:, j:j + T], pp[:, po + j:po + j + 1], acc[:, 0:T], ALU.mult, ALU.add,
                    ['mb0', 'mb1', 'pp'], ['mb1'])
            xcb = mbb[0]
            CP(xcb[:, 0:T], acc[:, 0:T], ['mb1'], ['mbb0'], eng='pool')
            r_, ig = mb[2], mb[3]
            for tb in range(4):
                sl = slice(tb * 512, (tb + 1) * 512)
                pt, pk = PSF()
                MM(pt[:, :], lw[:, 0, :], xcb[:, sl], True, True, ['lw0', 'mbb0'], [pk])
                ACT(r_[:, sl], pt[:, :], AF.Sigmoid, [pk, 'pp'], ['mb2'], bias=pp[:, po + 5:po + 6])
                pt, pk = PSF()
                MM(pt[:, :], lw[:, 1, :], xcb[:, sl], True, True, ['lw1', 'mbb0'], [pk])
                ACT(ig[:, sl], pt[:, :], AF.Sigmoid, [pk, 'pp'], ['mb3'], bias=pp[:, po + 6:po + 7])
            ACT(l_c8[:, 0:1], pp[:, po + 7:po + 8], AF.Exp, ['pp'], ['l_c8'], scale=-1.0)
            ACT(l_c8[:, 1:2], l_c8[:, 0:1], AF.Ln, ['l_c8', 'cf'], ['l_c8b'], bias=one1)
            TS(l_c8[:, 2:3], l_c8[:, 1:2], -8.0, None, ALU.mult, None, ['l_c8b'], ['l_c8c'])
            a_ = mb[4]
            ACT(a_[:, 0:T], r_[:, 0:T], AF.Exp, ['mb2', 'l_c8c'], ['mb4'], scale=l_c8[:, 2:3])
            a2 = mb[2]
            TT(a2[:, 0:T], a_[:, 0:T], a_[:, 0:T], ALU.mult, ['mb4'], ['mb2'])
            ACT(a2[:, 0:T], a2[:, 0:T], AF.Sqrt, ['mb2', 'cf'], ['mb2'], scale=-1.0, bias=one1)
            TT(ig[:, 0:T], ig[:, 0:T], acc[:, 0:T], ALU.mult, ['mb3', 'mb1'], ['mb3'])
            TT(ig[:, 0:T], ig[:, 0:T], a2[:, 0:T], ALU.mult, ['mb3', 'mb2'], ['mb3'])
            hh = mb[1]
            SCAN(hh[:, 0:T], a_[:, 0:T], ig[:, 0:T], 0.0, ALU.mult, ALU.add, ['mb4', 'mb3'], ['mb1'])
            zs = mb[2]
            for tb in range(4):
                sl = slice(tb * 512, (tb + 1) * 512)
                pt, pk = proj_cm(wz_t, wzk, 0, 128, tb)
                ACT(zs[:, sl], pt[:, :], AF.Silu, [pk], ['mb2'])
            yb = mbb[1]
            TT(yb[:, 0:T], hh[:, 0:T], zs[:, 0:T], ALU.mult, ['mb1', 'mb2'], ['mbb1'])
            DMA(yT_d[l, g, :, :], yb[:, 0:T], ['mbb1'], ['yTd'])

        for g in range(NL):
            lru_group(g)
        if stop == 'lru':
            return

        PK = mb[3]
        NR = 5 * NM

        def ml_gates():
            wgi, k1 = load_w(w_in[l][:, SEG['mi']:SEG['mi'] + NM], NM)
            wgf, k2 = load_w(w_in[l][:, SEG['mf']:SEG['mf'] + NM], NM)
            ig, sp_, bb, mm_ = mb[0], mb[1], mb[2], mb[4]
            gb = PPO['gb']
            for tb in range(4):
                sl = slice(tb * 512, (tb + 1) * 512)
                pt, pk = proj_cm(wgi, k1, 0, NM, tb)
                ACT(ig[0:NM, sl], pt[0:NM, :], AF.Identity, [pk, 'pp'], ['mb0'], bias=pp[0:NM, gb:gb + 1])
                pt, pk = proj_cm(wgf, k2, 0, NM, tb)
                ACT(sp_[0:NM, sl], pt[0:NM, :], AF.Exp, [pk, 'pp'], ['mb1'], scale=-1.0,
                    bias=pp[0:NM, gb + 1:gb + 2])
            ACT(sp_[0:NM, 0:T], sp_[0:NM, 0:T], AF.Ln, ['mb1', 'cf'], ['mb1'], bias=one1[0:NM, :])
            for c_ in range(16):
                sl = slice(c_ * 128, (c_ + 1) * 128)
                SCAN(bb[0:NM, sl], ones_f[0:NM, 0:128], sp_[0:NM, sl], 0.0, ALU.mult, ALU.subtract,
                     ['mb1', 'ones_f'], ['mb2'])
            beta = mb[0]
            TT(beta[0:NM, 0:T], ig[0:NM, 0:T], bb[0:NM, 0:T], ALU.subtract, ['mb0', 'mb2'], ['mb0'])
            cmx = mb[1]
            for c_ in range(16):
                sl = slice(c_ * 128, (c_ + 1) * 128)
                SCAN(cmx[0:NM, sl], zeros_f[0:NM, 0:128], beta[0:NM, sl], NEG, ALU.add, ALU.max,
                     ['mb0', 'zeros_f'], ['mb1'])
            gc, mxb, mloc, mnew, mprev, d1, sold = [sm[0:NM, 16 * i:16 * (i + 1)] for i in range(7)]
            CP(gc, chv(bb[0:NM, 0:T])[:, :, 127], ['mb2'], ['sm_gc'])
            CP(mxb, chv(cmx[0:NM, 0:T])[:, :, 127], ['mb1'], ['sm_mxb'])
            TT(mloc, gc, mxb, ALU.add, ['sm_gc', 'sm_mxb'], ['sm_mloc'])
            SCAN(mnew, gc, mloc, 0.0, ALU.add, ALU.max, ['sm_gc', 'sm_mloc'], ['sm_mnew'])
            MEMSET(mprev[:, 0:1], 0.0, ['sm_mprev'], eng='dve')
            CP(mprev[:, 1:16], mnew[:, 0:15], ['sm_mnew'], ['sm_mprev'])
            bc = lambda a: a.unsqueeze(2).to_broadcast([NM, 16, 128])
            TT(chv(mm_[0:NM, 0:T]), chv(cmx[0:NM, 0:T]), bc(mprev), ALU.max, ['mb1', 'sm_mprev'], ['mb4'])
            tmp = mb[1]
            rows = []
            ACT(tmp[0:NM, 0:T], beta[0:NM, 0:T], AF.Exp, ['mb0'], ['mb1'])
            for h in range(NM):
                DMA(PK[0 * NM + h:0 * NM + h + 1, 0:T], tmp[h:h + 1, 0:T], ['mb1'], ['mb3'])
            TT(tmp[0:NM, 0:T], mm_[0:NM, 0:T], bb[0:NM, 0:T], ALU.add, ['mb4', 'mb2'], ['mb1'])
            ACT(tmp[0:NM, 0:T], tmp[0:NM, 0:T], AF.Exp, ['mb1'], ['mb1'], scale=-1.0)
            for h in range(NM):
                DMA(PK[3 * NM + h:3 * NM + h + 1, 0:T], tmp[h:h + 1, 0:T], ['mb1'], ['mb3'])
            TT(chv(tmp[0:NM, 0:T]), bc(mprev), chv(mm_[0:NM, 0:T]), ALU.subtract, ['mb4', 'sm_mprev'], ['mb1'])
            ACT(tmp[0:NM, 0:T], tmp[0:NM, 0:T], AF.Exp, ['mb1'], ['mb1'])
            for h in range(NM):
                DMA(PK[2 * NM + h:2 * NM + h + 1, 0:T], tmp[h:h + 1, 0:T], ['mb1'], ['mb3'])
            ACT(tmp[0:NM, 0:T], mm_[0:NM, 0:T], AF.Exp, ['mb4'], ['mb1'], scale=-1.0)
            for h in range(NM):
                DMA(PK[4 * NM + h:4 * NM + h + 1, 0:T], tmp[h:h + 1, 0:T], ['mb1'], ['mb3'])
            TT(d1, gc, mnew, ALU.subtract, ['sm_gc', 'sm_mnew'], ['sm_d1'])
            TT(chv(tmp[0:NM, 0:T]), chv(beta[0:NM, 0:T]), bc(d1), ALU.add, ['mb0', 'sm_d1'], ['mb1'])
            ACT(tmp[0:NM, 0:T], tmp[0:NM, 0:T], AF.Exp, ['mb1'], ['mb1'])
            TS(tmp[0:NM, 0:T], tmp[0:NM, 0:T], 192.0 ** -0.5, None, ALU.mult, None, ['mb1'], ['mb1'])
            for h in range(NM):
                DMA(PK[1 * NM + h:1 * NM + h + 1, 0:T], tmp[h:h + 1, 0:T], ['mb1'], ['mb3'])
            TT(sold, d1, mprev, ALU.add, ['sm_d1', 'sm_mprev'], ['sm_sold'])
            ACT(sold, sold, AF.Exp, ['sm_sold'], ['sm_sold'])
            for h in range(NM):
                DMA(soldrow[0:1, h * 16:(h + 1) * 16], sold[h:h + 1, 0:16], ['sm_sold'], ['soldrow'])
            pt, pk = PSF()
            MM(pt[0:96, 0:NM * 16], ones_f[0:1, 0:96], soldrow[0:1, :], True, True, ['ones_f', 'soldrow'], [pk])
            CP(soldbc[0:96, :], pt[0:96, 0:NM * 16], [pk], ['soldbc'])
            for c_ in range(16):
                sl = slice(c_ * 128, (c_ + 1) * 128)
                pt, pk = PSF()
                TR(pt[:, 0:NR], PK[0:NR, sl], ident_f[0:NR, 0:NR], ['mb3', 'cf'], [pk])
                CP(colbuf[:, c_, :], pt[:, 0:NR], [pk], ['colbuf'], eng=('act' if c_ % 2 else 'dve'))

        ml_gates()
        if stop == 'mlg':
            DMA(dbg_f[:, 0:16 * NR], colbuf[:, :, :].rearrange("p c q -> p (c q)"), ['colbuf'], ['dbgf'])
            DMA(dbg_f[0:96, 2048:2048 + NM * 16], soldbc[0:96, :], ['soldbc'], ['dbgf'])
            return

        def ml_partU(h):
            wu, wuk = load_w(w_in[l][:, SEG['mu'] + h * 192: SEG['mu'] + (h + 1) * 192], 192)
            for a_i in range(3):
                DMA(mlw[0:96, a_i, :, :], ml_w[l, a_i, h].rearrange("c i o -> i c o"), (), ['mlw'], eng='pool')
            for cc in range(2):
                upad = mb[cc]
                uk = f"mb{cc}"
                ub = mbb[cc]
                ubk = f"mbb{cc}"
                MEMSET(upad[0:96, 0:3], 0.0, [uk])
                for tb in range(4):
                    sl = slice(tb * 512, (tb + 1) * 512)
                    pt, pk = proj_cm(wu, wuk, cc * 96, 96, tb)
                    CP(upad[0:96, 3 + tb * 512:3 + (tb + 1) * 512], pt[0:96, :], [pk], [uk], eng='act')
                    CP(ub[0:96, sl], upad[0:96, 3 + tb * 512:3 + (tb + 1) * 512], [uk], [ubk], eng='pool')

        def ml_head(h, next_u=None):
            ch0 = NL + 2 * h
            ucb, qTb, kTb = hba, hbq, hbk
            acc = mb[2]
            for cc in range(2):
                upad = mb[cc]
                uk = f"mb{cc}"
                pc = PPO['ml'] + (h * 2 + cc) * 5
                TS(acc[0:96, 0:T], upad[0:96, 0:T], pp[0:96, pc:pc + 1], None, ALU.mult, None, [uk, 'pp'], ['mb2'])
                for j in range(1, 4):
                    STT(acc[0:96, 0:T], upad[0:96, j:j + T], pp[0:96, pc + j:pc + j + 1], acc[0:96, 0:T],
                        ALU.mult, ALU.add, [uk, 'mb2', 'pp'], ['mb2'])
                ACT(ucb[0:96, cc, :], acc[0:96, 0:T], AF.Silu, ['mb2', 'pp'], ['hba'], bias=pp[0:96, pc + 4:pc + 5])
                for tb in range(4):
                    sl = slice(tb * 512, (tb + 1) * 512)
                    pt, pk = PSF()
                    MM(pt[0:96, :], mlw[0:96, 0, cc, :], ucb[0:96, cc, sl], True, True, ['mlw', 'hba'], [pk])
                    CP(qTb[0:96, cc, sl], pt[0:96, :], [pk], ['hbq'], eng='act')
                    pt, pk = PSF()
                    MM(pt[0:96, :], mlw[0:96, 1, cc, :], ucb[0:96, cc, sl], True, True, ['mlw', 'hba'], [pk])
                    ACT(kTb[0:96, cc, sl], pt[0:96, :], AF.Identity, [pk], ['hbk'], scale=192.0 ** -0.5)
            MEMSET(vaug[:, :, 192:193], 1.0, ['vaug'])
            for c_ in range(16):
                sl = slice(c_ * 128, (c_ + 1) * 128)
                pt, pk = PSF()
                for cc in range(2):
                    MM(pt[:, cc * 96:(cc + 1) * 96], mbb[cc][0:96, sl], mlw[0:96, 2, cc, :], True, True,
                       [f"mbb{cc}", 'mlw'], [pk])
                CP(vaug[:, c_, 0:192], pt[:, 0:192], [pk], ['vaug'], eng=('act' if c_ % 2 else 'dve'))
            wo_, wok = load_w(w_in[l][:, SEG['mo'] + h * 192: SEG['mo'] + (h + 1) * 192], 192)
            wz_, wzk = load_w(w_in[l][:, SEG['mz'] + h * 192: SEG['mz'] + (h + 1) * 192], 192)
            zsb = mb[4][:, 0:1536].bitcast(BF16).rearrange("p (c e) -> p c e", c=16)
            MEMSET(Cst[0:96, :, :], 0.0, ['Cst'])
            MEMSET(Cb[0:96, :, :], 0.0, ['Cb'])
            def colf(c_, q):
                return colbuf[:, c_, q * NM + h:q * NM + h + 1]

            def partA(c_):
                sl = slice(c_ * 128, (c_ + 1) * 128)
                b2 = c_ % 2
                kw, kwk = kwb2[b2], f"kwb{b2}"
                St_, Stk = Stb2[b2], f"Stb{b2}"
                so_, sok = sob2[b2], f"sob{b2}"
                pt, pk = PSF()
                for cc in range(2):
                    MM(pt[:, cc * 96:(cc + 1) * 96], ucb[0:96, cc, sl], mlw[0:96, 1, cc, :], True, True,
                       ['hba', 'mlw'], [pk])
                ACT(kw[:, 0:192], pt[:, 0:192], AF.Identity, [pk, 'colbuf'], [kwk], scale=colf(c_, 1))
                pS, kS = PSF()
                for cc in range(2):
                    MM(pS[:, 0:128], kTb[0:96, cc, sl], qTb[0:96, cc, sl], cc == 0, cc == 1, ['hbk', 'hbq'], [kS])
                STT(St_[:, :], pS[:, 0:128], colf(c_, 0), tri_f, ALU.mult, ALU.mult, [kS, 'colbuf', 'cf'], [Stk])
                pH, kH = PSF()
                MM(pH[:, 0:193], St_[:, :], vaug[:, c_, :], True, True, [Stk, 'vaug'], [kH])
                ACT(hsb[:, c_, :], pH[:, 0:193], AF.Identity, [kH, 'colbuf'], [f"hs{c_}"], scale=colf(c_, 4))
                pO, kO = proj_tm(wo_, wok, 0, 192, c_)
                ACT(so_[:, :], pO[:, 0:192], AF.Sigmoid, [kO], [sok])
                pZ, kZ = proj_tm(wz_, wzk, 0, 192, c_)
                ACT(zsb[:, c_, :], pZ[:, 0:192], AF.Silu, [kZ], ['mb4'])

            def partB(c_):
                sl = slice(c_ * 128, (c_ + 1) * 128)
                b2 = c_ % 2
                kw, kwk = kwb2[b2], f"kwb{b2}"
                so_, sok = sob2[b2], f"sob{b2}"
                hk = f"hs{c_}"
                if c_ > 0:
                    pI, kI = PSF()
                    for cc in range(2):
                        MM(pI[:, 0:193], qTb[0:96, cc, sl], Cb[0:96, cc, :], cc == 0, cc == 1, ['hbq', 'Cb'], [kI])
                    STT(hsb[:, c_, :], pI[:, 0:193], colf(c_, 2), hsb[:, c_, :], ALU.mult, ALU.add,
                        [kI, 'colbuf', hk], [hk])
                TT(hsb[:, c_, 0:192], hsb[:, c_, 0:192], so_[:, :], ALU.mult, [hk, sok], [hk])
                if c_ < 15:
                    for cc in range(2):
                        pU, kU = PSF()
                        MM(pU[0:96, 0:193], kw[:, cc * 96:(cc + 1) * 96], vaug[:, c_, :], True, True,
                           [kwk, 'vaug'], [kU])
                        STT(Cst[0:96, cc, :], Cst[0:96, cc, :], soldbc[0:96, h * 16 + c_:h * 16 + c_ + 1],
                            pU[0:96, 0:193], ALU.mult, ALU.add, ['Cst', 'soldbc', kU], ['Cst'])
                    CP(Cb[0:96, :, :], Cst[0:96, :, :], ['Cst'], ['Cb'], eng='pool')

            HK = [f"hs{q}" for q in range(16)]
            P.add('pool', lambda e: e.memset(dn16[:, 60:61], 0.0), ['hsb'] + HK, ['hsb'] + HK)
            partA(0)
            for c_ in range(16):
                if c_ + 1 < 16:
                    partA(c_ + 1)
                partB(c_)
            if next_u is not None:
                next_u()
            P.add('pool', lambda e: e.memset(dn16[:, 61:62], 0.0), ['hsb'] + HK, ['hsb'] + HK)
            den = hsb[:, :, 192]
            TS(dn16[:, 0:16], den, -1.0, None, ALU.mult, None, ['hsb'], ['dn16a'])
            TT(dn16[:, 0:16], dn16[:, 0:16], den, ALU.max, ['hsb', 'dn16a'], ['dn16a'])
            TT(dn16[:, 0:16], dn16[:, 0:16], colbuf[:, :, 3 * NM + h], ALU.max, ['dn16a', 'colbuf'], ['dn16a'])
            RECIP(dn16[:, 16:32], dn16[:, 0:16], ['dn16a'], ['dn16b'])
            TT(hsb[:, :, 0:192], hsb[:, :, 0:192], dn16[:, 16:32].unsqueeze(2).to_broadcast([128, 16, 192]), ALU.mult,
               ['hsb', 'dn16b'], ['hsb'])
            for c_ in range(16):
                P.add('dve', lambda e, c_=c_: e.bn_stats(st6[:, c_, :], hsb[:, c_, 0:192]), ['hsb'], [f"st6_{c_}"])
                P.add('dve', lambda e, c_=c_: e.bn_aggr(mv[:, c_, :], st6[:, c_, :]), [f"st6_{c_}"], ['mv'])
            rs = sm[:, 128:144]
            ACT(rs, mv[:, :, 1], AF.Sqrt, ['mv', 'cf'], ['sm_rs'], bias=eps5)
            RECIP(rs, rs, ['sm_rs'], ['sm_rs'])
            for c_ in range(16):
                TS(hsb[:, c_, 0:192], hsb[:, c_, 0:192], mv[:, c_, 0:1], rs[:, c_:c_ + 1], ALU.subtract, ALU.mult,
                   ['hsb', 'mv', 'sm_rs'], ['hsb'])
            ngv = ngbc[:, h * 192:(h + 1) * 192].unsqueeze(1).to_broadcast([128, 16, 192])
            TT(hsb[:, :, 0:192], hsb[:, :, 0:192], ngv, ALU.mult, ['hsb', 'ngbc'], ['hsb'])
            ytm = hbk[:, :, :].rearrange("p a t -> p (a t)")[:, 0:16 * 192].rearrange("p (c e) -> p c e", c=16)
            TT(ytm, hsb[:, :, 0:192], zsb, ALU.mult, ['hsb', 'mb4'], ['hbk'])
            yTb = hbq
            for cc in range(2):
                for half in range(2):
                    pb, pk = PSB()
                    for kk in range(8):
                        c_ = half * 8 + kk
                        TR(pb[0:96, kk * 128:(kk + 1) * 128], ytm[:, c_, cc * 96:(cc + 1) * 96], identb[:, :],
                           ['hbk', 'identb'], [pk])
                    CP(yTb[0:96, cc, half * 1024:(half + 1) * 1024], pb[0:96, :], [pk], ['hbq'],
                       eng=('act' if half else 'dve'))
                DMA(yT_d[l, ch0 + cc, 0:96, :], yTb[0:96, cc, :], ['hbq'], ['yTd'])

        ml_partU(0)
        for h in range(NM):
            ml_head(h, next_u=((lambda h=h: ml_partU(h + 1)) if h + 1 < NM else None))
            if stop == 'mlh1':
                return
        if stop == 'ml':
            return

        kmb = A.alloc([128, 16], BF16)
        kmr = A.alloc([128, 8])
        vaug1 = mb[0][:, 0:1032].bitcast(BF16).rearrange("p (c e) -> p c e", c=16)
        MSET = [
            dict(qkb=hba, qk='hba', zs=mbb[0], zk='mbb0', va=vaug[:, :, 0:129], vk='vaug'),
            dict(qkb=hbk, qk='hbk', zs=mbb[1], zk='mbb1', va=vaug1, vk='mb0'),
        ]

        def proj_gen(h, S, done=None):
            qb = S['qkb'][:, 0, :]
            kb = S['qkb'][:, 1, :]
            for which, dst in (('oq', qb), ('ok', kb)):
                w_, wk_ = load_w(w_in[l][:, SEG[which] + h * 128: SEG[which] + (h + 1) * 128], 128)
                for tb in range(4):
                    sl = slice(tb * 512, (tb + 1) * 512)
                    pt, pk = proj_cm(w_, wk_, 0, 128, tb)
                    CP(dst[:, sl], pt[:, :], [pk], [S['qk']], eng='act')
                    yield
            wz_, kz = load_w(w_in[l][:, SEG['oz'] + h * 128: SEG['oz'] + (h + 1) * 128], 128)
            for tb in range(4):
                sl = slice(tb * 512, (tb + 1) * 512)
                pt, pk = proj_cm(wz_, kz, 0, 128, tb)
                ACT(S['zs'][:, sl], pt[:, :], AF.Silu, [pk], [S['zk']])
                yield
            wv_, kv = load_w(w_in[l][:, SEG['ov'] + h * 128: SEG['ov'] + (h + 1) * 128], 128)
            MEMSET(S['va'][:, :, 128:129], 1.0, [S['vk']])
            for st in range(16):
                pt, pk = proj_tm(wv_, kv, 0, 128, st)
                CP(S['va'][:, st, 0:128], pt[:, 0:128], [pk], [S['vk']], eng='pool' if False else 'act')
                yield
            if done is not None:
                done()

        def mo_main(h, S, step):
            chn = NL + 2 * NM + h
            qb = S['qkb'][:, 0, :]
            kb = S['qkb'][:, 1, :]
            zs = S['zs']
            va = S['va']
            qk, zk, vk = S['qk'], S['zk'], S['vk']
            P.add('dve', lambda e: e.tensor_reduce(km[:, 0:8], kb.rearrange("p (b s) -> p b s", s=256),
                                                   AX.X, ALU.add), [qk], ['km'])
            TS(kmb[:, 0:8], km[:, 0:8], 1.0 / 256, None, ALU.mult, None, ['km'], ['kmb'])
            STT(kmr[:, 0:8], km[:, 0:8], 1.0 / 256, kmb[:, 0:8], ALU.mult, ALU.subtract, ['km', 'kmb'], ['kmr'])
            CP(kmb[:, 8:16], kmr[:, 0:8], ['kmr'], ['kmb'])
            et = etab[:, h * 128:(h + 1) * 128].rearrange("p (q s) -> p q s", q=16)
            for qt in range(16):
                bq = qt // 2
                if bq == 0:
                    continue
                if bq >= 4:
                    pg, kg = PSF()
                    MM(pg[:, 0:8], qb[:, qt * 128:(qt + 1) * 128], kmb[:, 0:8], True, False, [qk, 'kmb'], [kg])
                    MM(pg[:, 0:8], qb[:, qt * 128:(qt + 1) * 128], kmb[:, 8:16], False, True, [qk, 'kmb'], [kg])
                    TT(gm[:, :], pg[:, 0:8], negmask[:, bq * 8:(bq + 1) * 8], ALU.add, [kg, 'cf'], ['gm'])
                    P.add('dve', lambda e: e.max(top8[:, :], gm[:, :]), ['gm'], ['top8'])
                    TS(sel[:, :], gm[:, :], top8[:, 2:3], None, ALU.is_ge, None, ['gm', 'top8'], ['sel'])
                    TT(Fq[:, qt, :], et[:, qt, :], sel[:, :], ALU.mult, ['sel', 'cf'], ['Fq'])
                else:
                    CP(Fq[:, qt, :], et[:, qt, :], ['cf'], ['Fq'])
            acc = hsb[:, :, 0:129]
            P.add('pool', lambda e: e.memset(dn[:, 2:3], 1.0), ['hsb'] + [f"acc{q}" for q in range(16)],
                  ['dn2', 'hsb'] + [f"acc{q}" for q in range(16)])
            sc = 128.0 ** -0.5
            bias = slopecol[:, 2 * h + 1:2 * h + 2]
            biasp = [slopecol[:, 2 * h:2 * h + 1], slopecol[:, 2 * h + 1:2 * h + 2]]
            items = []
            npast = 0
            for bq in range(8):
                items.append(('d', bq, 0, 0, 0))
                for j in range(bq):
                    items.append(('p', bq, j, 0, npast % 3))
                    npast += 1

            def s1(it):
                kind, bq, j, kt, pi = it
                qs = slice(bq * 256, (bq + 1) * 256)
                if kind == 'd':
                    for kt2 in range(2):
                        st = 2 * bq + kt2
                        pd = (bq % 2) * 2 + kt2
                        pS, kS = PSF()
                        MM(pS[:, 0:256], kb[:, st * 128:(st + 1) * 128], qb[:, qs], True, True, [qk], [kS])
                        ACT(Ptd4[pd][:, :], pS[:, 0:256], AF.Exp, [kS, 'cf'], [f"Ptd{pd}"], scale=sc, bias=bias)
                        TT(Ptd4[pd][:, :], Ptd4[pd][:, :], gdiag[:, (h * 2 + kt2) * 256:(h * 2 + kt2 + 1) * 256],
                           ALU.mult, [f"Ptd{pd}", 'gdiag'], [f"Ptd{pd}"], eng='pool')
                else:
                    for kt2 in range(2):
                        st = 2 * j + kt2
                        pp_ = pi * 2 + kt2
                        pS, kS = PSF()
                        MM(pS[:, 0:256], kb[:, st * 128:(st + 1) * 128], qb[:, qs], True, True, [qk], [kS])
                        ACT(Ptb[pp_][:, :], pS[:, 0:256], AF.Exp, [kS, 'cf'], [f"Ptb{pp_}"], scale=sc, bias=biasp[kt2])

            def s2(it):
                kind, bq, j, kt, pi = it
                if kind == 'd':
                    p0, p1 = (bq % 2) * 2, (bq % 2) * 2 + 1
                    pO, kO = PSF()
                    MM(pO[:, 0:129], Ptd4[p0][:, 0:128], va[:, 2 * bq, 0:129], True, True, [f"Ptd{p0}", vk], [kO])
                    CP(acc[:, 2 * bq, :], pO[:, 0:129], [kO], [f"acc{2 * bq}"], eng='act')
                    pO, kO = PSF()
                    MM(pO[:, 0:129], Ptd4[p0][:, 128:256], va[:, 2 * bq, 0:129], True, False, [f"Ptd{p0}", vk], [kO])
                    MM(pO[:, 0:129], Ptd4[p1][:, 128:256], va[:, 2 * bq + 1, 0:129], False, True, [f"Ptd{p1}", vk], [kO])
                    CP(acc[:, 2 * bq + 1, :], pO[:, 0:129], [kO], [f"acc{2 * bq + 1}"], eng='act')
                else:
                    for qh in range(2):
                        qt = 2 * bq + qh
                        pO, kO = PSF()
                        for kt2 in range(2):
                            pp_ = pi * 2 + kt2
                            MM(pO[:, 0:129], Ptb[pp_][:, qh * 128:(qh + 1) * 128], va[:, 2 * j + kt2, 0:129],
                               kt2 == 0, kt2 == 1, [f"Ptb{pp_}", vk], [kO])
                        STT(acc[:, qt, :], pO[:, 0:129], Fq[:, qt, j:j + 1], acc[:, qt, :], ALU.mult, ALU.add,
                            [kO, 'Fq', f"acc{qt}"], [f"acc{qt}"])

            LA = 2
            for i in range(min(LA, len(items))):
                s1(items[i])
            for i in range(len(items)):
                if i + LA < len(items):
                    s1(items[i + LA])
                s2(items[i])
                step()
            yb = hbq[:, 0, :]
            for qt in range(16):
                RECIP(dn[:, 2:3], acc[:, qt, 128:129], [f"acc{qt}"], ['dn2'])
                TS(ofb[:, :], acc[:, qt, 0:128], dn[:, 2:3], None, ALU.mult, None, [f"acc{qt}", 'dn2'], ['ofb'])
                pT, kT = PSF()
                TR(pT[:, 0:128], ofb[:, :], ident_f, ['ofb', 'cf'], [kT])
                TT(yb[:, qt * 128:(qt + 1) * 128], pT[:, 0:128], zs[:, qt * 128:(qt + 1) * 128], ALU.mult,
                   [kT, zk], ['hbq'])
                step()
            DMA(yT_d[l, chn, :, :], yb, ['hbq'], ['yTd'])

        def prefetch_wout():
            save = A.off
            A.off = persist_mark
            wv = A.alloc([128, NCH, D], BF16)
            A.off = save
            for ci in range(16):
                r0, K = chunks[ci]
                DMA(wv[0:K, ci, :], w_out[l, r0:r0 + K, :], (), ['hT0', 'hT1', 'hT2', 'hT3', 'wout'], eng='pool')

        use_pf = cfg.get('pf', True) and stop in ('all', 'out')
        g0 = proj_gen(0, MSET[0], done=(prefetch_wout if (NO == 1 and use_pf) else None))
        for _ in g0:
            pass
        for h in range(NO):
            gn = None
            if h + 1 < NO:
                gn = proj_gen(h + 1, MSET[(h + 1) % 2],
                              done=(prefetch_wout if (h + 1 == NO - 1 and use_pf) else None))

            def step(gn=gn):
                if gn is not None:
                    next(gn, None)

            mo_main(h, MSET[h % 2], step)
            if gn is not None:
                for _ in gn:
                    pass

    chunks = []
    for g in range(NL):
        chunks.append((g * 128, 128))
    for h in range(NM):
        for cc in range(2):
            chunks.append((512 + h * 192 + cc * 96, 96))
    for h in range(NO):
        chunks.append((1280 + h * 128, 128))

    def out_phase(l, xsrc, xdst, final):
        A.off = persist_mark
        P.barrier()
        wout = A.alloc([128, NCH, D], BF16)
        wq = A.alloc([128, KD, 512], BF16)
        wo = A.alloc([128, 4, D], BF16)
        hmT = A.alloc([128, KD, MEM], BF16)
        kT = A.alloc([128, 4, MEM], BF16)
        vx = A.alloc([128, 2, 4, 129], BF16)
        xblk = A.alloc([128, 2, D])
        yblk = A.alloc([128, NCH, 256], BF16)
        hx = A.alloc([128, 2, D], BF16)
        hxT = A.alloc([128, KD, 256], BF16)
        qT = A.alloc([128, 4, 256], BF16)
        PT = A.alloc([128, 2, 2, 256], BF16)
        oh = A.alloc([128, 2, 512], BF16)
        ohT = A.alloc([128, 4, 256], BF16)
        gbx = A.alloc([128, D])
        gbf = A.alloc([128, D])
        rd = A.alloc([128, 2])
        for ci, (r0, K) in enumerate(chunks):
            if ci < 16 and cfg.get('pf', True):
                continue
            DMA(wout[0:K, ci, :], w_out[l, r0:r0 + K, :], (), ['wout'], eng='pool')
        DMA(wq[:, :, :], xa_wq[l].rearrange("(k p) c -> p k c", p=128), (), ['wq'], eng='pool')
        DMA(wo[:, :, :], xa_wo[l].rearrange("(h p) n -> p h n", p=128), (), ['wo'], eng='pool')
        DMA(gbx[:, :], gains[l, 2].partition_broadcast(128), (), ['gbx'])
        for i in range(2):
            DMA(xblk[:, i, :], mem_in[i * 128:(i + 1) * 128, :], (), [f"xblk{i}"])
            norm_tile(xblk[:, i, :], f"xblk{i}", gbx[:, :], 'gbx', hx[:, i, :], f"hx{i}", hx[:, i, :], f"hx{i}")
            transpose_16(hx[:, i, :], f"hx{i}", lambda half, i=i: hmT[:, half * 8:(half + 1) * 8, i * 128:(i + 1) * 128],
                         'hmT')
        wkv = xblk[:, :, :].rearrange("p a d -> p (a d)").bitcast(BF16).rearrange("p (k c) -> p k c", k=KD)
        DMA(wkv, xa_wkv[l][:, 0:512].rearrange("(k p) c -> p k c", p=128), (), ['xblk0', 'xblk1'], eng='pool')
        for h in range(4):
            pt, pk = PSF()
            for k in range(KD):
                MM(pt[:, 0:MEM], wkv[:, k, h * 128:(h + 1) * 128], hmT[:, k, :], k == 0, k == KD - 1, ['xblk0', 'xblk1', 'hmT'], [pk])
            CP(kT[:, h, :], pt[:, 0:MEM], [pk], ['kT'], eng='act')
        DMA(wkv, xa_wkv[l][:, 512:1024].rearrange("(k p) c -> p k c", p=128), (), ['xblk0', 'xblk1'], eng='pool')
        MEMSET(vx[:, :, :, 128:129], 1.0, ['vx'])
        for mt in range(2):
            pt, pk = PSF()
            for k in range(KD):
                MM(pt[:, 0:512], hmT[:, k, mt * 128:(mt + 1) * 128], wkv[:, k, :], k == 0, k == KD - 1, ['xblk0', 'xblk1', 'hmT'], [pk])
            CP(vx[:, mt, :, 0:128], pt[:, 0:512].rearrange("p (h e) -> p h e", h=4), [pk], ['vx'])
        DMA(gbx[:, :], gains[l, 1].partition_broadcast(128), ['gbx'], ['gbx'])
        if final:
            DMA(gbf[:, :], final_g.partition_broadcast(128), (), ['gbf'])
        sc = 128.0 ** -0.5
        XK = ['xblk0', 'xblk1']
        c1, c2 = NL, NL + 2 * NM

        def load_y(tb):
            rows = slice(tb * 256, (tb + 1) * 256)
            DMA(yblk[:, 0:c1, :], yT_d[l, 0:c1, :, rows].rearrange("c p t -> p c t"), ['yTd'], ['yblk'])
            DMA(yblk[0:96, c1:c2, :], yT_d[l, c1:c2, 0:96, rows].rearrange("c p t -> p c t"), ['yTd'], ['yblk'])
            DMA(yblk[:, c2:NCH, :], yT_d[l, c2:NCH, :, rows].rearrange("c p t -> p c t"), ['yTd'], ['yblk'])

        load_y(0)
        for tb in range(8):
            rows = slice(tb * 256, (tb + 1) * 256)
            for tt in range(2):
                DMA(xblk[:, tt, :], xsrc[tb * 256 + tt * 128: tb * 256 + (tt + 1) * 128, :], (), [XK[tt]])
            for tt in range(2):
                for nb in range(4):
                    ns = slice(nb * 512, (nb + 1) * 512)
                    pt, pk = PSF()
                    for ci, (r0, K) in enumerate(chunks):
                        MM(pt[:, :], yblk[0:K, ci, tt * 128:(tt + 1) * 128], wout[0:K, ci, ns], ci == 0, ci == NCH - 1,
                           ['yblk', 'wout'], [pk])
                    TT(xblk[:, tt, ns], xblk[:, tt, ns], pt[:, :], ALU.add, [XK[tt], pk], [XK[tt]])
            if tb + 1 < 8:
                load_y(tb + 1)
            for tt in range(2):
                norm_tile(xblk[:, tt, :], XK[tt], gbx[:, :], 'gbx', hx[:, tt, :], f"hx{tt}", hx[:, tt, :], f"hx{tt}")
                transpose_16(hx[:, tt, :], f"hx{tt}",
                             lambda half, tt=tt: hxT[:, half * 8:(half + 1) * 8, tt * 128:(tt + 1) * 128], 'hxT')
            for h in range(4):
                pt, pk = PSF()
                for k in range(KD):
                    MM(pt[:, 0:256], wq[:, k, h * 128:(h + 1) * 128], hxT[:, k, :], k == 0, k == KD - 1, ['wq', 'hxT'], [pk])
                CP(qT[:, h, :], pt[:, 0:256], [pk], [f"qT{h}"], eng='act')
            for h in range(4):
                for mt in range(2):
                    pS, kS = PSF()
                    MM(pS[:, 0:256], kT[:, h, mt * 128:(mt + 1) * 128], qT[:, h, :], True, True, ['kT', f"qT{h}"], [kS])
                    ACT(PT[:, h % 2, mt, :], pS[:, 0:256], AF.Exp, [kS], [f"PT{h % 2}"], scale=sc)
                for tt in range(2):
                    pO, kO = PSF()
                    for mt in range(2):
                        MM(pO[:, 0:129], PT[:, h % 2, mt, tt * 128:(tt + 1) * 128], vx[:, mt, h, :], mt == 0, mt == 1,
                           [f"PT{h % 2}", 'vx'], [kO])
                    RECIP(rd[:, tt:tt + 1], pO[:, 128:129], [kO], [f"rd{tt}"])
                    TS(oh[:, tt, h * 128:(h + 1) * 128], pO[:, 0:128], rd[:, tt:tt + 1], None, ALU.mult, None,
                       [kO, f"rd{tt}"], [f"oh{tt}"])
            for tt in range(2):
                pb, pk = PSB()
                for h in range(4):
                    TR(pb[:, h * 128:(h + 1) * 128], oh[:, tt, h * 128:(h + 1) * 128], identb[:, :], [f"oh{tt}", 'identb'], [pk])
                CP(ohT[:, :, tt * 128:(tt + 1) * 128], pb[:, 0:512].rearrange("p (h t) -> p h t", h=4), [pk], [f"ohT{tt}"],
                   eng=('act' if tt else 'dve'))
            for tt in range(2):
                for nb in range(4):
                    ns = slice(nb * 512, (nb + 1) * 512)
                    pt, pk = PSF()
                    for h in range(4):
                        MM(pt[:, :], ohT[:, h, tt * 128:(tt + 1) * 128], wo[:, h, ns], h == 0, h == 3, [f"ohT{tt}", 'wo'], [pk])
                    TT(xblk[:, tt, ns], xblk[:, tt, ns], pt[:, :], ALU.add, [XK[tt], pk], [XK[tt]])
                if final:
                    norm_tile(xblk[:, tt, :], XK[tt], gbf[:, :], 'gbf', xblk[:, tt, :], XK[tt], hx[:, tt, :], f"hx{tt}")
                DMA(xdst[tb * 256 + tt * 128: tb * 256 + (tt + 1) * 128, :], xblk[:, tt, :], [XK[tt]], ['xdst'])

    src = x_in
    for l in range(nlayers):
        mix_phase(l, src)
        if stop in ('A', 'lru', 'mlg', 'mlh1', 'mlh_a', 'mlh_c', 'mlh_a0', 'mlh_a1', 'ml', 'mo'):
            break
        last = (l == nlayers - 1)
        out_phase(l, src, out_d if last else xs0, final=(last and nlayers == L))
        src = xs0
        if stop == 'out':
            break
    P.emit()
    return nc, es


def alibi_slopes(n):
    def pow2(m):
        start = 2.0 ** (-8.0 / m)
        return [start ** (i + 1) for i in range(m)]
    if math.log2(n).is_integer():
        s = pow2(n)
    else:
        c = 2 ** int(math.floor(math.log2(n)))
        s = pow2(c) + pow2(2 * c)[0::2][:n - c]
    return np.array(s, dtype=np.float64)


def make_consts(mo_heads):
    NO = len(mo_heads)
    CO, NCF = const_layout(NO)
    cf = np.zeros((128, NCF), np.float32)
    cf[:, CO['ident']:CO['ident'] + 128] = np.eye(128, dtype=np.float32)
    s = np.arange(128)[:, None]
    j = np.arange(128)[None, :]
    cf[:, CO['tri']:CO['tri'] + 128] = (s <= j).astype(np.float32)
    nm = np.zeros((8, 8), np.float32)
    for bq in range(8):
        nm[bq, bq:] = NEG
    cf[:, CO['negmask']:CO['negmask'] + 64] = nm.reshape(1, 64)
    slopes = alibi_slopes(6)
    p = np.arange(128, dtype=np.float64)
    gd = np.zeros((128, NO, 2, 256), np.float64)
    for hi, hg in enumerate(mo_heads):
        sl = slopes[hg]
        cf[:, CO['slope'] + 2 * hi] = (sl * (p - 255.0)).astype(np.float32)
        cf[:, CO['slope'] + 2 * hi + 1] = (sl * (p - 127.0)).astype(np.float32)
        et = np.zeros((128, 16, 8), np.float64)
        for qt in range(16):
            bq = qt // 2
            for jb in range(bq):
                t = qt * 128 + p
                sref = jb * 256 + 255
                et[:, qt, jb] = np.exp(-sl * (t - sref))
        cf[:, CO['etab'] + hi * 128: CO['etab'] + (hi + 1) * 128] = et.reshape(128, 128).astype(np.float32)
        for kt in range(2):
            sabs = kt * 128 + p[:, None]
            sref = kt * 128 + 127
            t = np.arange(256, dtype=np.float64)[None, :]
            g = np.exp(-sl * (t - sref)) * (sabs <= t)
            gd[:, hi, kt, :] = g
    cf[:, CO['misc'] + 0] = 1e-6
    cf[:, CO['misc'] + 1] = 1e-5
    cf[:, CO['misc'] + 2] = 1.0
    gd = np.minimum(gd, 3.0e38).astype(np.float32).reshape(128, NO * 512).astype(ml_dtypes.bfloat16)
    return cf, gd


def pack_core(inp, b, lru_blocks, ml_heads, mo_heads):
    NL, NM, NO = len(lru_blocks), len(ml_heads), len(mo_heads)
    PPO, NPP = pp_layout(NL, NM)
    w_in = inp['w_in']
    o_lx, o_lz, o_mu, o_mo, o_mz, o_mi, o_mf, o_oq, o_ok, o_ov, o_oz = (
        0, 512, 1024, 1792, 2560, 3328, 3332, 3336, 4104, 4872, 5640)
    cols = []
    for base in (o_lx, o_lz):
        for g in lru_blocks:
            cols.append(np.arange(base + g * 128, base + (g + 1) * 128))
    for base in (o_mu, o_mo, o_mz):
        for h in ml_heads:
            cols.append(np.arange(base + h * 192, base + (h + 1) * 192))
    for base in (o_mi, o_mf):
        cols.append(np.array([base + h for h in ml_heads]))
    for base in (o_oq, o_ok, o_ov, o_oz):
        for h in mo_heads:
            cols.append(np.arange(base + h * 128, base + (h + 1) * 128))
    cols = np.concatenate(cols)
    full = (len(cols) == w_in.shape[2]) and np.array_equal(cols, np.arange(w_in.shape[2]))
    w_in_c = w_in if full else np.ascontiguousarray(w_in[:, :, cols])
    pp = np.zeros((L, 128, NPP), np.float32)
    for l in range(L):
        for gi, g in enumerate(lru_blocks):
            sl = slice(g * 128, (g + 1) * 128)
            o = PPO['lru'] + gi * 8
            pp[l, :, o:o + 4] = inp['lru_conv_w'][l][:, sl].T
            pp[l, :, o + 4] = inp['lru_conv_b'][l][sl]
            pp[l, :, o + 5] = inp['lru_ba'][l][sl]
            pp[l, :, o + 6] = inp['lru_bx'][l][sl]
            pp[l, :, o + 7] = inp['lru_lambda'][l][sl]
        for hi, h in enumerate(ml_heads):
            for cc in range(2):
                sl = slice(h * 192 + cc * 96, h * 192 + (cc + 1) * 96)
                o = PPO['ml'] + (hi * 2 + cc) * 5
                pp[l, 0:96, o:o + 4] = inp['ml_conv_w'][l][:, sl].T
                pp[l, 0:96, o + 4] = inp['ml_conv_b'][l][sl]
            pp[l, hi, PPO['gb']] = inp['ml_bi'][l][h]
            pp[l, hi, PPO['gb'] + 1] = -inp['ml_bf'][l][h]
    lru_w = np.stack([inp['lru_wa'][:, lru_blocks], inp['lru_wx'][:, lru_blocks]], axis=1)
    ml_w = np.zeros((L, 3, NM, 2, 96, 96), np.float32)
    for ai, nm_ in enumerate(('ml_wq', 'ml_wk', 'ml_wv')):
        w = inp[nm_]
        for hi, h in enumerate(ml_heads):
            for cc in range(2):
                for bl in range(24):
                    gidx = h * 48 + cc * 24 + bl
                    ml_w[:, ai, hi, cc, bl * 4:(bl + 1) * 4, bl * 4:(bl + 1) * 4] = w[:, gidx]
    ml_ng = np.concatenate([inp['ml_norm_g'][:, h * 192:(h + 1) * 192] for h in ml_heads], axis=1)
    gains = np.stack([inp['mix_norm_g'], inp['xa_norm_g'], inp['mem_norm_g']], axis=1)
    return {
        "x": np.ascontiguousarray(inp['x'][b]), "mem": np.ascontiguousarray(inp['mem'][b]),
        "w_in": w_in_c, "w_out": inp['w_out'], "gains": np.ascontiguousarray(gains),
        "final_g": inp['final_norm_g'], "pp": pp, "lru_w": np.ascontiguousarray(lru_w),
        "ml_w": ml_w, "ml_ng": np.ascontiguousarray(ml_ng), "xa_wq": inp['xa_wq'], "xa_wkv": inp['xa_wkv'],
        "xa_wo": inp['xa_wo'],
    }


def kernel(**inputs):
    inp = {k: np.asarray(v) for k, v in inputs.items()}
    cfg = dict(NL=4, NM=4, NO=6)
    nc, es = build(cfg)
    cf, gd = make_consts(list(range(6)))
    in_maps = []
    for c in range(8):
        m = pack_core(inp, c // 2, [0, 1, 2, 3], [0, 1, 2, 3], [0, 1, 2, 3, 4, 5])
        m["cst_f"] = cf
        m["cst_b"] = gd
        in_maps.append(m)
    res = run_bass_kernel_spmd(nc, in_maps, core_ids=list(range(8)))
    out = np.stack([res.results[2 * b]["out"] for b in range(4)], axis=0)
    return out.astype(np.float32)
```

```python
import math
import numpy as np
import ml_dtypes
from contextlib import ExitStack
import concourse.bass as bass
import concourse.mybir as mybir
from concourse.bass_utils import run_bass_kernel_spmd

F32 = mybir.dt.float32
BF16 = mybir.dt.bfloat16
AF = mybir.ActivationFunctionType
ALU = mybir.AluOpType
AX = mybir.AxisListType

T = 2048
D = 2048
KD = 16
MEM = 256
L = 2
ENG = ('pe', 'act', 'dve', 'pool', 'sp')
NEG = -1.0e30
ARENA_WORDS = 52400


class Op:
    __slots__ = ('eng', 'fn', 'deps', 'signal', 'sig', 'sigval', 'dma', 'sem', 'semval')


class Prog:
    EPOCH = 6000
    NQ = 12

    def __init__(self, nc, es):
        self.nc = nc
        self.es = es
        self.ops = {e: [] for e in ENG}
        self.state = {}
        self.dma_cnt = {e: 0 for e in ENG}
        self.dma_last = {e: {} for e in ENG}
        self.dma_sems = {}
        self.eng_sems = {}
        self.pending = {e: set() for e in ENG}

    def _sem(self, name):
        return self.es.enter_context(self.nc.semaphore(name))

    limit = None
    count = 0

    def add(self, eng, fn, r=(), w=(), dma=False):
        self.count += 1
        if self.limit is not None and self.count > self.limit:
            return None
        op = Op()
        op.eng = eng; op.fn = fn; op.dma = dma; op.signal = False
        op.sig = 0; op.sigval = 0; op.sem = None; op.semval = 0
        deps = set(self.pending[eng])
        self.pending[eng] = set()
        for k in r:
            st = self.state.get(k)
            if st is not None and st[0] is not None:
                deps.add(st[0])
        for k in w:
            st = self.state.get(k)
            if st is not None:
                if st[0] is not None:
                    deps.add(st[0])
                deps.update(st[1].values())
        for k in r:
            rd = self.state.setdefault(k, [None, {}])[1]
            if dma:
                rd[('dma', id(op))] = op
            else:
                rd[eng] = op
        for k in w:
            self.state[k] = [op, {}]
        if dma:
            if eng not in self.dma_sems:
                self.dma_sems[eng] = [self._sem(f"dq_{eng}_{i}") for i in range(self.NQ)]
            i = self.dma_cnt[eng]
            self.dma_cnt[eng] += 1
            slot = i % self.NQ
            op.sem = self.dma_sems[eng][slot]
            op.semval = 16 * (i // self.NQ + 1)
            prev = self.dma_last[eng].get(slot)
            if prev is not None:
                deps.add(prev)
            self.dma_last[eng][slot] = op
        deps.discard(op)
        fin = []
        for d in deps:
            if (not d.dma) and (not dma) and d.eng == 'pe' and eng == 'pe':
                continue
            if not d.dma:
                d.signal = True
            fin.append(d)
        op.deps = fin
        self.ops[eng].append(op)
        return op

    def barrier(self):
        last = []
        for e in ENG:
            comp = [o for o in self.ops[e] if not o.dma]
            if comp:
                last.append(comp[-1])
            last.extend(self.dma_last[e].values())
        for e in ENG:
            self.pending[e].update(last)

    def emit(self):
        for e in ENG:
            cnt = 0
            for op in self.ops[e]:
                if (not op.dma) and op.signal:
                    op.sig = cnt // self.EPOCH
                    op.sigval = cnt % self.EPOCH + 1
                    cnt += 1
                    if (e, op.sig) not in self.eng_sems:
                        self.eng_sems[(e, op.sig)] = self._sem(f"es_{e}_{op.sig}")
        prog = self

        def run(e, h):
            waited = {}
            for op in prog.ops[e]:
                need = {}
                for d in op.deps:
                    if d.dma:
                        s, v = d.sem, d.semval
                    else:
                        s, v = prog.eng_sems[(d.eng, d.sig)], d.sigval
                    if need.get(id(s), (None, 0))[1] < v:
                        need[id(s)] = (s, v)
                for k, (s, v) in need.items():
                    if waited.get(k, 0) < v:
                        h.wait_ge(s, v)
                        waited[k] = v
                ins = op.fn(h)
                if op.dma:
                    ins.then_inc(op.sem, 16)
                elif op.signal:
                    ins.then_inc(prog.eng_sems[(e, op.sig)], 1)
            if e == 'sp':
                for q in ENG:
                    for slot, o in prog.dma_last[q].items():
                        h.wait_ge(o.sem, o.semval)

        with self.nc.Block() as block:
            @block.tensor
            def _(h):
                run('pe', h)

            @block.scalar
            def _(h):
                run('act', h)

            @block.vector
            def _(h):
                run('dve', h)

            @block.gpsimd
            def _(h):
                run('pool', h)

            @block.sync
            def _(h):
                run('sp', h)


class Arena:
    def __init__(self, t, n):
        self.t = t; self.n = n; self.off = 0

    def alloc(self, shape, dt=F32):
        nel = 1
        for s in shape[1:]:
            nel *= s
        words = nel if dt == F32 else (nel + 1) // 2
        words = (words + 1) // 2 * 2
        assert self.off + words <= self.n, f"arena overflow {self.off}+{words}>{self.n}"
        v = self.t[0:shape[0], self.off:self.off + words]
        self.off += words
        if dt != F32:
            v = v.bitcast(dt)
        v = v[:, 0:nel]
        if len(shape) == 3:
            v = v.rearrange("p (a b) -> p a b", a=shape[1])
        elif len(shape) == 4:
            v = v.rearrange("p (a b c) -> p a b c", a=shape[1], b=shape[2])
        return v


def seg_offsets(NL, NM, NO):
    sizes = [('lx', NL * 128), ('lz', NL * 128), ('mu', NM * 192), ('mo', NM * 192), ('mz', NM * 192),
             ('mi', NM), ('mf', NM), ('oq', NO * 128), ('ok', NO * 128), ('ov', NO * 128), ('oz', NO * 128)]
    off = {}
    o = 0
    for n, s in sizes:
        off[n] = o
        o += s
    return off, o


def const_layout(NO):
    CO = {}
    o = 0
    for n, s in [('ident', 128), ('tri', 128), ('negmask', 64), ('slope', NO * 2), ('etab', NO * 128), ('misc', 4)]:
        CO[n] = o
        o += s
    return CO, o


def pp_layout(NL, NM):
    PPO = {'lru': 0, 'ml': NL * 8, 'gb': NL * 8 + NM * 10}
    return PPO, NL * 8 + NM * 10 + 2


def build(cfg):
    NL, NM, NO = cfg['NL'], cfg['NM'], cfg['NO']
    nlayers = cfg.get('layers', L)
    stop = cfg.get('stop', 'all')
    dbg = cfg.get('dbg', False)
    SEG, WC = seg_offsets(NL, NM, NO)
    NCH = NL + 2 * NM + NO
    CO, NCF = const_layout(NO)
    PPO, NPP = pp_layout(NL, NM)
    nc = bass.Bass("TRN2", target_bir_lowering=False)
    es = ExitStack()
    P = Prog(nc, es)
    P.limit = cfg.get('limit', None)

    def din(name, shape, dt=F32):
        return nc.dram_tensor(name, list(shape), dt, kind="ExternalInput").ap()

    def dscr(name, shape, dt=F32, out=False):
        return nc.dram_tensor(name, list(shape), dt, kind="ExternalOutput" if out else "Internal").ap()

    x_in = din("x", [T, D])
    mem_in = din("mem", [MEM, D])
    w_in = din("w_in", [L, D, WC])
    w_out = din("w_out", [L, D, D])
    gains = din("gains", [L, 3, D])
    final_g = din("final_g", [D])
    pp_in = din("pp", [L, 128, NPP])
    lru_w = din("lru_w", [L, 2, NL, 128, 128])
    ml_w = din("ml_w", [L, 3, NM, 2, 96, 96])
    ml_ng = din("ml_ng", [L, NM * 192])
    xa_wq = din("xa_wq", [L, D, 512])
    xa_wkv = din("xa_wkv", [L, D, 1024])
    xa_wo = din("xa_wo", [L, 512, D])
    cst_f = din("cst_f", [128, NCF])
    cst_b = din("cst_b", [128, NO * 512], BF16)
    out_d = nc.dram_tensor("out", [T, D], F32, kind="ExternalOutput").ap()
    xs0 = dscr("xs0", [T, D], F32, out=dbg)
    yT_d = dscr("yT", [L, NCH, 128, T], BF16, out=dbg)
    dbg_f = dscr("dbgf", [128, 4096], F32, out=True) if dbg else None

    arena_t = es.enter_context(nc.sbuf_tensor("arena", [128, ARENA_WORDS], F32))
    A = Arena(arena_t, ARENA_WORDS)

    def ps(name, shape, dt=F32):
        return es.enter_context(nc.psum_tensor(name, list(shape), dt))

    def DMA(out, in_, r, w, eng='sp', **kw):
        P.add(eng, lambda e: e.dma_start(out=out, in_=in_, **kw), r, w, dma=True)

    def MM(out, lhsT, rhs, start, stop, r, w, **kw):
        P.add('pe', lambda e: e.matmul(out, lhsT, rhs, start=start, stop=stop, **kw), r, w)

    def TR(out, in_, ident, r, w):
        P.add('pe', lambda e: e.transpose(out, in_, ident), r, w)

    def ACT(out, in_, func, r, w, **kw):
        P.add('act', lambda e: e.activation(out=out, in_=in_, func=func, **kw), r, w)

    def TS(out, in0, s1, s2, op0, op1, r, w, eng='dve', **kw):
        if op1 is None:
            P.add(eng, lambda e: e.tensor_scalar(out, in0, s1, None, op0, **kw), r, w)
        else:
            P.add(eng, lambda e: e.tensor_scalar(out, in0, s1, s2, op0, op1, **kw), r, w)

    def TT(out, in0, in1, op, r, w, eng='dve'):
        P.add(eng, lambda e: e.tensor_tensor(out, in0, in1, op), r, w)

    def STT(out, in0, scalar, in1, op0, op1, r, w, **kw):
        P.add('dve', lambda e: e.scalar_tensor_tensor(out, in0, scalar, in1, op0, op1, **kw), r, w)

    def CP(out, in_, r, w, eng='dve'):
        if eng == 'act':
            ACT(out, in_, AF.Copy, r, w)
        else:
            P.add(eng, lambda e: e.tensor_copy(out, in_), r, w)

    def RECIP(out, in_, r, w):
        P.add('dve', lambda e: e.reciprocal(out, in_), r, w)

    def SCAN(out, d0, d1, init, op0, op1, r, w):
        P.add('dve', lambda e: e.tensor_tensor_scan(out, d0, d1, init, op0, op1), r, w)

    def MEMSET(ap, val, w, eng='pool'):
        P.add(eng, lambda e: e.memset(ap, val), (), w)

    def chv(ap):
        return ap.rearrange("p (c t) -> p c t", t=128)

    cf = A.alloc([128, NCF])
    DMA(cf[:, :], cst_f[:, :], (), ['cf'])
    ident_f = cf[:, CO['ident']:CO['ident'] + 128]
    tri_f = cf[:, CO['tri']:CO['tri'] + 128]
    negmask = cf[:, CO['negmask']:CO['negmask'] + 64]
    slopecol = cf[:, CO['slope']:CO['slope'] + NO * 2]
    etab = cf[:, CO['etab']:CO['etab'] + NO * 128]
    eps6 = cf[:, CO['misc']:CO['misc'] + 1]
    eps5 = cf[:, CO['misc'] + 1:CO['misc'] + 2]
    one1 = cf[:, CO['misc'] + 2:CO['misc'] + 3]
    gdiag = A.alloc([128, NO * 512], BF16)
    DMA(gdiag[:, :], cst_b[:, :], (), ['gdiag'])
    identb = A.alloc([128, 128], BF16)
    CP(identb[:, :], ident_f, ['cf'], ['identb'])
    ones_f = A.alloc([128, 128])
    MEMSET(ones_f[:, :], 1.0, ['ones_f'])
    zeros_f = A.alloc([128, 128])
    MEMSET(zeros_f[:, :], 0.0, ['zeros_f'])
    pp = A.alloc([128, NPP])
    nst = A.alloc([128, 8])
    persist_mark = A.off

    NPS = 6
    psf = [ps(f"psf{i}", [128, 512]) for i in range(NPS)]
    psb = [ps(f"psb{i}", [128, 1024], BF16) for i in range(2)]
    pctr = [0, 0]

    def PSF():
        i = pctr[0] % NPS
        pctr[0] += 1
        return psf[i], f"psf{i}"

    def PSB():
        i = pctr[1] % 2
        pctr[1] += 1
        return psb[i], f"psb{i}"

    def norm_tile(xt_ap, xkey, gbc_ap, gkey, out_ap, okey, junk_ap, jkey):
        STT(junk_ap, xt_ap, 1.0, xt_ap, ALU.mult, ALU.mult, [xkey], [jkey, 'n_ss'], accum_out=nst[:, 0:1])
        ACT(nst[:, 1:2], nst[:, 0:1], AF.Sqrt, ['n_ss', 'cf'], ['n_sd'], scale=1.0 / D, bias=eps6)
        RECIP(nst[:, 2:3], nst[:, 1:2], ['n_sd'], ['n_rstd'])
        STT(out_ap, xt_ap, nst[:, 2:3], gbc_ap, ALU.mult, ALU.mult, [xkey, 'n_rstd', gkey], [okey])

    def transpose_16(hb, hbkey, dst_fn, dkey):
        for half in range(2):
            pb, pk = PSB()
            for kk in range(8):
                k = half * 8 + kk
                TR(pb[:, kk * 128:(kk + 1) * 128], hb[:, k * 128:(k + 1) * 128], identb[:, :],
                   [hbkey, 'identb'], [pk])
            src = pb[:, :].rearrange("p (k t) -> p k t", k=8)
            CP(dst_fn(half), src, [pk], [dkey], eng=('act' if half == 0 else 'dve'))

    def mix_phase(l, src):
        A.off = persist_mark
        P.barrier()
        hT = A.alloc([128, KD, T], BF16)
        mb = [A.alloc([128, T + 8]) for _ in range(5)]
        mbb = [A.alloc([128, T], BF16) for _ in range(2)]
        wbuf = [A.alloc([128, KD, 192], BF16) for _ in range(2)]
        hba = A.alloc([128, 2, T], BF16)
        hbq = A.alloc([128, 2, T], BF16)
        hbk = A.alloc([128, 2, T], BF16)
        vaug = A.alloc([128, 16, 193], BF16)
        hsb = A.alloc([128, 16, 193])
        ngbc = A.alloc([128, NM * 192])
        colbuf = A.alloc([128, 16, 5 * NM])
        soldbc = A.alloc([128, NM * 16])
        soldrow = A.alloc([1, NM * 16])
        sm = A.alloc([128, 160])
        lw = A.alloc([128, 2, 128], BF16)
        mlw = A.alloc([96, 3, 2, 96], BF16)
        Cst = A.alloc([96, 2, 193])
        Cb = A.alloc([96, 2, 193], BF16)
        Stb2 = [A.alloc([128, 128], BF16) for _ in range(2)]
        sob2 = [A.alloc([128, 192]) for _ in range(2)]
        kwb2 = [A.alloc([128, 192], BF16) for _ in range(2)]
        st6 = A.alloc([128, 16, 6])
        dn16 = A.alloc([128, 64])
        mv = A.alloc([128, 16, 2])
        dn = A.alloc([128, 4])
        km = A.alloc([128, 8])
        gm = A.alloc([128, 8])
        top8 = A.alloc([128, 8])
        sel = A.alloc([128, 8])
        Fq = A.alloc([128, 16, 8])
        Ptb = [A.alloc([128, 256], BF16) for _ in range(6)]
        Ptd4 = [A.alloc([128, 256], BF16) for _ in range(4)]
        ofb = A.alloc([128, 128])
        l_c8 = A.alloc([128, 4])

        DMA(pp[:, :], pp_in[l], (), ['pp'])
        DMA(ngbc[:, :], ml_ng[l].partition_broadcast(128), (), ['ngbc'])

        gbc = mb[3][:, 0:D]
        DMA(gbc, gains[l, 0].partition_broadcast(128), (), ['mb3'])
        for i in range(16):
            xt = mb[i % 2][:, 0:D]
            xk = f"mb{i % 2}"
            DMA(xt, src[i * 128:(i + 1) * 128, :], (), [xk])
            hb = mbb[i % 2][:, 0:D]
            hk = f"mbb{i % 2}"
            norm_tile(xt, xk, gbc, 'mb3', hb, hk, hb, hk)
            transpose_16(hb, hk, lambda half, i=i: hT[:, half * 8:(half + 1) * 8, i * 128:(i + 1) * 128],
                         f"hT{i // 4}")
        if stop == 'A':
            DMA(yT_d[l, 0:16].rearrange("c p t -> p c t"), hT[:, :, :], [f"hT{i}" for i in range(4)], ['yTd'])
            return

        wctr = [0]

        def load_w(src2d, ncols):
            i = wctr[0] % 2
            wctr[0] += 1
            wt = wbuf[i]
            key = f"wbuf{i}"
            DMA(wt[:, :, 0:ncols], src2d.rearrange("(k p) c -> p k c", p=128), (), [key], eng='pool')
            return wt, key

        def proj_cm(wt, wkey, c0, M, tb):
            pt, pk = PSF()
            for k in range(KD):
                MM(pt[0:M, 0:512], wt[:, k, c0:c0 + M], hT[:, k, tb * 512:(tb + 1) * 512], k == 0, k == KD - 1,
                   [wkey, f"hT{tb}"], [pk])
            return pt, pk

        def proj_tm(wt, wkey, c0, N, tt):
            pt, pk = PSF()
            for k in range(KD):
                MM(pt[:, 0:N], hT[:, k, tt * 128:(tt + 1) * 128], wt[:, k, c0:c0 + N], k == 0, k == KD - 1,
                   [wkey, f"hT{tt // 4}"], [pk])
            return pt, pk

        def lru_group(g):
            wx_t, wxk = load_w(w_in[l][:, SEG['lx'] + g * 128: SEG['lx'] + (g + 1) * 128], 128)
            wz_t, wzk = load_w(w_in[l][:, SEG['lz'] + g * 128: SEG['lz'] + (g + 1) * 128], 128)
            DMA(lw[:, 0, :], lru_w[l, 0, g], (), ['lw0'], eng='pool')
            DMA(lw[:, 1, :], lru_w[l, 1, g], (), ['lw1'], eng='pool')
            po = PPO['lru'] + g * 8
            xpad = mb[0]
            MEMSET(xpad[:, 0:3], 0.0, ['mb0'])
            for tb in range(4):
                pt, pk = proj_cm(wx_t, wxk, 0, 128, tb)
                CP(xpad[:, 3 + tb * 512: 3 + (tb + 1) * 512], pt[:, :], [pk], ['mb0'], eng='act')
            zsl = hba[:, 0, :]
            for tb in range(4):
                sl = slice(tb * 512, (tb + 1) * 512)
                pt, pk = proj_cm(wz_t, wzk, 0, 128, tb)
                ACT(zsl[:, sl], pt[:, :], AF.Silu, [pk], ['hba'])
            acc = mb[1]
            TS(acc[:, 0:T], xpad[:, 0:T], pp[:, po:po + 1], pp[:, po + 4:po + 5], ALU.mult, ALU.add,
               ['mb0', 'pp'], ['mb1'])
            for j in range(1, 4):
                STT(acc[:, 0:T], xpad[:, j:j + T], pp[:, po + j:po + j + 1], acc[:, 0:T], ALU.mult, ALU.add,
                    ['mb0', 'mb1', 'pp'], ['mb1'])
            xcb = mbb[0]
            CP(xcb[:, 0:T], acc[:, 0:T], ['mb1'], ['mbb0'], eng='pool')
            r_, ig = mb[2], mb[3]
            for tb in range(4):
                sl = slice(tb * 512, (tb + 1) * 512)
                pt, pk = PSF()
                MM(pt[:, :], lw[:, 0, :], xcb[:, sl], True, True, ['lw0', 'mbb0'], [pk])
                ACT(r_[:, sl], pt[:, :], AF.Sigmoid, [pk, 'pp'], ['mb2'], bias=pp[:, po + 5:po + 6])
                pt, pk = PSF()
                MM(pt[:, :], lw[:, 1, :], xcb[:, sl], True, True, ['lw1', 'mbb0'], [pk])
                ACT(ig[:, sl], pt[:, :], AF.Sigmoid, [pk, 'pp'], ['mb3'], bias=pp[:, po + 6:po + 7])
            ACT(l_c8[:, 0:1], pp[:, po + 7:po + 8], AF.Exp, ['pp'], ['l_c8'], scale=-1.0)
            ACT(l_c8[:, 1:2], l_c8[:, 0:1], AF.Ln, ['l_c8', 'cf'], ['l_c8b'], bias=one1)
            TS(l_c8[:, 2:3], l_c8[:, 1:2], -8.0, None, ALU.mult, None, ['l_c8b'], ['l_c8c'])
            a_ = mb[4]
            ACT(a_[:, 0:T], r_[:, 0:T], AF.Exp, ['mb2', 'l_c8c'], ['mb4'], scale=l_c8[:, 2:3])
            a2 = mb[2]
            TT(a2[:, 0:T], a_[:, 0:T], a_[:, 0:T], ALU.mult, ['mb4'], ['mb2'])
            ACT(a2[:, 0:T], a2[:, 0:T], AF.Sqrt, ['mb2', 'cf'], ['mb2'], scale=-1.0, bias=one1)
            TT(ig[:, 0:T], ig[:, 0:T], acc[:, 0:T], ALU.mult, ['mb3', 'mb1'], ['mb3'])
            TT(ig[:, 0:T], ig[:, 0:T], a2[:, 0:T], ALU.mult, ['mb3', 'mb2'], ['mb3'])
            hh = mb[1]
            SCAN(hh[:, 0:T], a_[:, 0:T], ig[:, 0:T], 0.0, ALU.mult, ALU.add, ['mb4', 'mb3'], ['mb1'])
            yb = mbb[1]
            TT(yb[:, 0:T], hh[:, 0:T], zsl, ALU.mult, ['mb1', 'hba'], ['mbb1'])
            DMA(yT_d[l, g, :, :], yb[:, 0:T], ['mbb1'], ['yTd'])

        for g in range(NL):
            lru_group(g)
        if stop == 'lru':
            return

        PK = mb[3]
        NR = 5 * NM

        def ml_gates():
            wgi, k1 = load_w(w_in[l][:, SEG['mi']:SEG['mi'] + NM], NM)
            wgf, k2 = load_w(w_in[l][:, SEG['mf']:SEG['mf'] + NM], NM)
            ig, sp_, bb, mm_ = mb[0], mb[1], mb[2], mb[4]
            gb = PPO['gb']
            for tb in range(4):
                sl = slice(tb * 512, (tb + 1) * 512)
                pt, pk = proj_cm(wgi, k1, 0, NM, tb)
                ACT(ig[0:NM, sl], pt[0:NM, :], AF.Identity, [pk, 'pp'], ['mb0'], bias=pp[0:NM, gb:gb + 1])
                pt, pk = proj_cm(wgf, k2, 0, NM, tb)
                ACT(sp_[0:NM, sl], pt[0:NM, :], AF.Exp, [pk, 'pp'], ['mb1'], scale=-1.0,
                    bias=pp[0:NM, gb + 1:gb + 2])
            ACT(sp_[0:NM, 0:T], sp_[0:NM, 0:T], AF.Ln, ['mb1', 'cf'], ['mb1'], bias=one1[0:NM, :])
            for c_ in range(16):
                sl = slice(c_ * 128, (c_ + 1) * 128)
                SCAN(bb[0:NM, sl], ones_f[0:NM, 0:128], sp_[0:NM, sl], 0.0, ALU.mult, ALU.subtract,
                     ['mb1', 'ones_f'], ['mb2'])
            beta = mb[0]
            TT(beta[0:NM, 0:T], ig[0:NM, 0:T], bb[0:NM, 0:T], ALU.subtract, ['mb0', 'mb2'], ['mb0'])
            cmx = mb[1]
            for c_ in range(16):
                sl = slice(c_ * 128, (c_ + 1) * 128)
                SCAN(cmx[0:NM, sl], zeros_f[0:NM, 0:128], beta[0:NM, sl], NEG, ALU.add, ALU.max,
                     ['mb0', 'zeros_f'], ['mb1'])
            gc, mxb, mloc, mnew, mprev, d1, sold = [sm[0:NM, 16 * i:16 * (i + 1)] for i in range(7)]
            CP(gc, chv(bb[0:NM, 0:T])[:, :, 127], ['mb2'], ['sm_gc'])
            CP(mxb, chv(cmx[0:NM, 0:T])[:, :, 127], ['mb1'], ['sm_mxb'])
            TT(mloc, gc, mxb, ALU.add, ['sm_gc', 'sm_mxb'], ['sm_mloc'])
            SCAN(mnew, gc, mloc, 0.0, ALU.add, ALU.max, ['sm_gc', 'sm_mloc'], ['sm_mnew'])
            MEMSET(mprev[:, 0:1], 0.0, ['sm_mprev'], eng='dve')
            CP(mprev[:, 1:16], mnew[:, 0:15], ['sm_mnew'], ['sm_mprev'])
            bc = lambda a: a.unsqueeze(2).to_broadcast([NM, 16, 128])
            TT(chv(mm_[0:NM, 0:T]), chv(cmx[0:NM, 0:T]), bc(mprev), ALU.max, ['mb1', 'sm_mprev'], ['mb4'])
            tmp = mb[1]
            rows = []
            ACT(tmp[0:NM, 0:T], beta[0:NM, 0:T], AF.Exp, ['mb0'], ['mb1'])
            for h in range(NM):
                DMA(PK[0 * NM + h:0 * NM + h + 1, 0:T], tmp[h:h + 1, 0:T], ['mb1'], ['mb3'])
            TT(tmp[0:NM, 0:T], mm_[0:NM, 0:T], bb[0:NM, 0:T], ALU.add, ['mb4', 'mb2'], ['mb1'])
            ACT(tmp[0:NM, 0:T], tmp[0:NM, 0:T], AF.Exp, ['mb1'], ['mb1'], scale=-1.0)
            for h in range(NM):
                DMA(PK[3 * NM + h:3 * NM + h + 1, 0:T], tmp[h:h + 1, 0:T], ['mb1'], ['mb3'])
            TT(chv(tmp[0:NM, 0:T]), bc(mprev), chv(mm_[0:NM, 0:T]), ALU.subtract, ['mb4', 'sm_mprev'], ['mb1'])
            ACT(tmp[0:NM, 0:T], tmp[0:NM, 0:T], AF.Exp, ['mb1'], ['mb1'])
            for h in range(NM):
                DMA(PK[2 * NM + h:2 * NM + h + 1, 0:T], tmp[h:h + 1, 0:T], ['mb1'], ['mb3'])
            ACT(tmp[0:NM, 0:T], mm_[0:NM, 0:T], AF.Exp, ['mb4'], ['mb1'], scale=-1.0)
            for h in range(NM):
                DMA(PK[4 * NM + h:4 * NM + h + 1, 0:T], tmp[h:h + 1, 0:T], ['mb1'], ['mb3'])
            TT(d1, gc, mnew, ALU.subtract, ['sm_gc', 'sm_mnew'], ['sm_d1'])
            TT(chv(tmp[0:NM, 0:T]), chv(beta[0:NM, 0:T]), bc(d1), ALU.add, ['mb0', 'sm_d1'], ['mb1'])
            ACT(tmp[0:NM, 0:T], tmp[0:NM, 0:T], AF.Exp, ['mb1'], ['mb1'])
            TS(tmp[0:NM, 0:T], tmp[0:NM, 0:T], 192.0 ** -0.5, None, ALU.mult, None, ['mb1'], ['mb1'])
            for h in range(NM):
                DMA(PK[1 * NM + h:1 * NM + h + 1, 0:T], tmp[h:h + 1, 0:T], ['mb1'], ['mb3'])
            TT(sold, d1, mprev, ALU.add, ['sm_d1', 'sm_mprev'], ['sm_sold'])
            ACT(sold, sold, AF.Exp, ['sm_sold'], ['sm_sold'])
            for h in range(NM):
                DMA(soldrow[0:1, h * 16:(h + 1) * 16], sold[h:h + 1, 0:16], ['sm_sold'], ['soldrow'])
            pt, pk = PSF()
            MM(pt[0:96, 0:NM * 16], ones_f[0:1, 0:96], soldrow[0:1, :], True, True, ['ones_f', 'soldrow'], [pk])
            CP(soldbc[0:96, :], pt[0:96, 0:NM * 16], [pk], ['soldbc'])
            for c_ in range(16):
                sl = slice(c_ * 128, (c_ + 1) * 128)
                pt, pk = PSF()
                TR(pt[:, 0:NR], PK[0:NR, sl], ident_f[0:NR, 0:NR], ['mb3', 'cf'], [pk])
                CP(colbuf[:, c_, :], pt[:, 0:NR], [pk], ['colbuf'], eng=('act' if c_ % 2 else 'dve'))

        ml_gates()
        if stop == 'mlg':
            DMA(dbg_f[:, 0:16 * NR], colbuf[:, :, :].rearrange("p c q -> p (c q)"), ['colbuf'], ['dbgf'])
            DMA(dbg_f[0:96, 2048:2048 + NM * 16], soldbc[0:96, :], ['soldbc'], ['dbgf'])
            return

        def ml_head(h):
            ch0 = NL + 2 * h
            wu, wuk = load_w(w_in[l][:, SEG['mu'] + h * 192: SEG['mu'] + (h + 1) * 192], 192)
            for a_i in range(3):
                DMA(mlw[0:96, a_i, :, :], ml_w[l, a_i, h].rearrange("c i o -> i c o"), (), ['mlw'], eng='pool')
            ucb, qTb, kTb = hba, hbq, hbk
            acc = mb[2]
            if cfg.get('verbose'):
                print("count at mlh_a0", P.count)
            if stop == 'mlh_a0':
                CP(sm[0:96, 0:96], mlw[0:96, 0, 0, :], ['mlw'], ['sm_x'])
                return
            for cc in range(2):
                upad = mb[cc]
                uk = f"mb{cc}"
                ub = mbb[cc]
                ubk = f"mbb{cc}"
                MEMSET(upad[0:96, 0:3], 0.0, [uk])
                for tb in range(4):
                    sl = slice(tb * 512, (tb + 1) * 512)
                    pt, pk = proj_cm(wu, wuk, cc * 96, 96, tb)
                    CP(upad[0:96, 3 + tb * 512:3 + (tb + 1) * 512], pt[0:96, :], [pk], [uk], eng='act')
                    CP(ub[0:96, sl], upad[0:96, 3 + tb * 512:3 + (tb + 1) * 512], [uk], [ubk], eng='pool')
                pc = PPO['ml'] + (h * 2 + cc) * 5
                TS(acc[0:96, 0:T], upad[0:96, 0:T], pp[0:96, pc:pc + 1], None, ALU.mult, None, [uk, 'pp'], ['mb2'])
                for j in range(1, 4):
                    STT(acc[0:96, 0:T], upad[0:96, j:j + T], pp[0:96, pc + j:pc + j + 1], acc[0:96, 0:T],
                        ALU.mult, ALU.add, [uk, 'mb2', 'pp'], ['mb2'])
                ACT(ucb[0:96, cc, :], acc[0:96, 0:T], AF.Silu, ['mb2', 'pp'], ['hba'], bias=pp[0:96, pc + 4:pc + 5])
                if cfg.get('verbose'):
                    print("count at mlh_a1", P.count)
                if stop == 'mlh_a1':
                    return
                for tb in range(4):
                    sl = slice(tb * 512, (tb + 1) * 512)
                    pt, pk = PSF()
                    MM(pt[0:96, :], mlw[0:96, 0, cc, :], ucb[0:96, cc, sl], True, True, ['mlw', 'hba'], [pk])
                    CP(qTb[0:96, cc, sl], pt[0:96, :], [pk], ['hbq'], eng='act')
                    pt, pk = PSF()
                    MM(pt[0:96, :], mlw[0:96, 1, cc, :], ucb[0:96, cc, sl], True, True, ['mlw', 'hba'], [pk])
                    ACT(kTb[0:96, cc, sl], pt[0:96, :], AF.Identity, [pk], ['hbk'], scale=192.0 ** -0.5)
            if stop == 'mlh_a':
                return
            MEMSET(vaug[:, :, 192:193], 1.0, ['vaug'])
            for c_ in range(16):
                sl = slice(c_ * 128, (c_ + 1) * 128)
                pt, pk = PSF()
                for cc in range(2):
                    MM(pt[:, cc * 96:(cc + 1) * 96], mbb[cc][0:96, sl], mlw[0:96, 2, cc, :], True, True,
                       [f"mbb{cc}", 'mlw'], [pk])
                CP(vaug[:, c_, 0:192], pt[:, 0:192], [pk], ['vaug'], eng=('act' if c_ % 2 else 'dve'))
            wo_, wok = load_w(w_in[l][:, SEG['mo'] + h * 192: SEG['mo'] + (h + 1) * 192], 192)
            wz_, wzk = load_w(w_in[l][:, SEG['mz'] + h * 192: SEG['mz'] + (h + 1) * 192], 192)
            zsb = mb[4][:, 0:1536].bitcast(BF16).rearrange("p (c e) -> p c e", c=16)
            MEMSET(Cst[0:96, :, :], 0.0, ['Cst'])
            MEMSET(Cb[0:96, :, :], 0.0, ['Cb'])
            def colf(c_, q):
                return colbuf[:, c_, q * NM + h:q * NM + h + 1]

            def partA(c_):
                sl = slice(c_ * 128, (c_ + 1) * 128)
                b2 = c_ % 2
                kw, kwk = kwb2[b2], f"kwb{b2}"
                St_, Stk = Stb2[b2], f"Stb{b2}"
                so_, sok = sob2[b2], f"sob{b2}"
                pt, pk = PSF()
                for cc in range(2):
                    MM(pt[:, cc * 96:(cc + 1) * 96], ucb[0:96, cc, sl], mlw[0:96, 1, cc, :], True, True,
                       ['hba', 'mlw'], [pk])
                ACT(kw[:, 0:192], pt[:, 0:192], AF.Identity, [pk, 'colbuf'], [kwk], scale=colf(c_, 1))
                pS, kS = PSF()
                for cc in range(2):
                    MM(pS[:, 0:128], kTb[0:96, cc, sl], qTb[0:96, cc, sl], cc == 0, cc == 1, ['hbk', 'hbq'], [kS])
                STT(St_[:, :], pS[:, 0:128], colf(c_, 0), tri_f, ALU.mult, ALU.mult, [kS, 'colbuf', 'cf'], [Stk])
                pH, kH = PSF()
                MM(pH[:, 0:193], St_[:, :], vaug[:, c_, :], True, True, [Stk, 'vaug'], [kH])
                ACT(hsb[:, c_, :], pH[:, 0:193], AF.Identity, [kH, 'colbuf'], [f"hs{c_}"], scale=colf(c_, 4))
                pO, kO = proj_tm(wo_, wok, 0, 192, c_)
                ACT(so_[:, :], pO[:, 0:192], AF.Sigmoid, [kO], [sok])
                pZ, kZ = proj_tm(wz_, wzk, 0, 192, c_)
                ACT(zsb[:, c_, :], pZ[:, 0:192], AF.Silu, [kZ], ['mb4'])

            def partB(c_):
                sl = slice(c_ * 128, (c_ + 1) * 128)
                b2 = c_ % 2
                kw, kwk = kwb2[b2], f"kwb{b2}"
                so_, sok = sob2[b2], f"sob{b2}"
                hk = f"hs{c_}"
                if c_ > 0:
                    pI, kI = PSF()
                    for cc in range(2):
                        MM(pI[:, 0:193], qTb[0:96, cc, sl], Cb[0:96, cc, :], cc == 0, cc == 1, ['hbq', 'Cb'], [kI])
                    STT(hsb[:, c_, :], pI[:, 0:193], colf(c_, 2), hsb[:, c_, :], ALU.mult, ALU.add,
                        [kI, 'colbuf', hk], [hk])
                TT(hsb[:, c_, 0:192], hsb[:, c_, 0:192], so_[:, :], ALU.mult, [hk, sok], [hk])
                if c_ < 15:
                    for cc in range(2):
                        pU, kU = PSF()
                        MM(pU[0:96, 0:193], kw[:, cc * 96:(cc + 1) * 96], vaug[:, c_, :], True, True,
                           [kwk, 'vaug'], [kU])
                        STT(Cst[0:96, cc, :], Cst[0:96, cc, :], soldbc[0:96, h * 16 + c_:h * 16 + c_ + 1],
                            pU[0:96, 0:193], ALU.mult, ALU.add, ['Cst', 'soldbc', kU], ['Cst'])
                    CP(Cb[0:96, :, :], Cst[0:96, :, :], ['Cst'], ['Cb'], eng='pool')

            HK = [f"hs{q}" for q in range(16)]
            P.add('pool', lambda e: e.memset(dn16[:, 60:61], 0.0), ['hsb'] + HK, ['hsb'] + HK)
            partA(0)
            for c_ in range(16):
                if c_ + 1 < 16:
                    partA(c_ + 1)
                partB(c_)
            if stop == 'mlh_c':
                return
            P.add('pool', lambda e: e.memset(dn16[:, 61:62], 0.0), ['hsb'] + HK, ['hsb'] + HK)
            den = hsb[:, :, 192]
            TS(dn16[:, 0:16], den, -1.0, None, ALU.mult, None, ['hsb'], ['dn16a'])
            TT(dn16[:, 0:16], dn16[:, 0:16], den, ALU.max, ['hsb', 'dn16a'], ['dn16a'])
            TT(dn16[:, 0:16], dn16[:, 0:16], colbuf[:, :, 3 * NM + h], ALU.max, ['dn16a', 'colbuf'], ['dn16a'])
            RECIP(dn16[:, 16:32], dn16[:, 0:16], ['dn16a'], ['dn16b'])
            TT(hsb[:, :, 0:192], hsb[:, :, 0:192], dn16[:, 16:32].unsqueeze(2).to_broadcast([128, 16, 192]), ALU.mult,
               ['hsb', 'dn16b'], ['hsb'])
            for c_ in range(16):
                P.add('dve', lambda e, c_=c_: e.bn_stats(st6[:, c_, :], hsb[:, c_, 0:192]), ['hsb'], [f"st6_{c_}"])
                P.add('dve', lambda e, c_=c_: e.bn_aggr(mv[:, c_, :], st6[:, c_, :]), [f"st6_{c_}"], ['mv'])
            rs = sm[:, 128:144]
            ACT(rs, mv[:, :, 1], AF.Sqrt, ['mv', 'cf'], ['sm_rs'], bias=eps5)
            RECIP(rs, rs, ['sm_rs'], ['sm_rs'])
            for c_ in range(16):
                TS(hsb[:, c_, 0:192], hsb[:, c_, 0:192], mv[:, c_, 0:1], rs[:, c_:c_ + 1], ALU.subtract, ALU.mult,
                   ['hsb', 'mv', 'sm_rs'], ['hsb'])
            ngv = ngbc[:, h * 192:(h + 1) * 192].unsqueeze(1).to_broadcast([128, 16, 192])
            TT(hsb[:, :, 0:192], hsb[:, :, 0:192], ngv, ALU.mult, ['hsb', 'ngbc'], ['hsb'])
            ytm = hbk[:, :, :].rearrange("p a t -> p (a t)")[:, 0:16 * 192].rearrange("p (c e) -> p c e", c=16)
            TT(ytm, hsb[:, :, 0:192], zsb, ALU.mult, ['hsb', 'mb4'], ['hbk'])
            yTb = hbq
            for cc in range(2):
                for half in range(2):
                    pb, pk = PSB()
                    for kk in range(8):
                        c_ = half * 8 + kk
                        TR(pb[0:96, kk * 128:(kk + 1) * 128], ytm[:, c_, cc * 96:(cc + 1) * 96], identb[:, :],
                           ['hbk', 'identb'], [pk])
                    CP(yTb[0:96, cc, half * 1024:(half + 1) * 1024], pb[0:96, :], [pk], ['hbq'],
                       eng=('act' if half else 'dve'))
                DMA(yT_d[l, ch0 + cc, 0:96, :], yTb[0:96, cc, :], ['hbq'], ['yTd'])

        for h in range(NM):
            ml_head(h)
            if stop in ('mlh1', 'mlh_a', 'mlh_c', 'mlh_a0', 'mlh_a1'):
                return
        if stop == 'ml':
            return

        kmb = A.alloc([128, 16], BF16)
        kmr = A.alloc([128, 8])
        vaug1 = mb[0][:, 0:1032].bitcast(BF16).rearrange("p (c e) -> p c e", c=16)
        MSET = [
            dict(qkb=hba, qk='hba', zs=mbb[0], zk='mbb0', va=vaug[:, :, 0:129], vk='vaug'),
            dict(qkb=hbk, qk='hbk', zs=mbb[1], zk='mbb1', va=vaug1, vk='mb0'),
        ]

        def proj_gen(h, S, done=None):
            qb = S['qkb'][:, 0, :]
            kb = S['qkb'][:, 1, :]
            for which, dst in (('oq', qb), ('ok', kb)):
                w_, wk_ = load_w(w_in[l][:, SEG[which] + h * 128: SEG[which] + (h + 1) * 128], 128)
                for tb in range(4):
                    sl = slice(tb * 512, (tb + 1) * 512)
                    pt, pk = proj_cm(w_, wk_, 0, 128, tb)
                    CP(dst[:, sl], pt[:, :], [pk], [S['qk']], eng='act')
                    yield
            wz_, kz = load_w(w_in[l][:, SEG['oz'] + h * 128: SEG['oz'] + (h + 1) * 128], 128)
            for tb in range(4):
                sl = slice(tb * 512, (tb + 1) * 512)
                pt, pk = proj_cm(wz_, kz, 0, 128, tb)
                ACT(S['zs'][:, sl], pt[:, :], AF.Silu, [pk], [S['zk']])
                yield
            wv_, kv = load_w(w_in[l][:, SEG['ov'] + h * 128: SEG['ov'] + (h + 1) * 128], 128)
            MEMSET(S['va'][:, :, 128:129], 1.0, [S['vk']])
            for st in range(16):
                pt, pk = proj_tm(wv_, kv, 0, 128, st)
                CP(S['va'][:, st, 0:128], pt[:, 0:128], [pk], [S['vk']], eng='pool' if False else 'act')
                yield
            if done is not None:
                done()

        def mo_main(h, S, step):
            chn = NL + 2 * NM + h
            qb = S['qkb'][:, 0, :]
            kb = S['qkb'][:, 1, :]
            zs = S['zs']
            va = S['va']
            qk, zk, vk = S['qk'], S['zk'], S['vk']
            P.add('dve', lambda e: e.tensor_reduce(km[:, 0:8], kb.rearrange("p (b s) -> p b s", s=256),
                                                   AX.X, ALU.add), [qk], ['km'])
            TS(kmb[:, 0:8], km[:, 0:8], 1.0 / 256, None, ALU.mult, None, ['km'], ['kmb'])
            STT(kmr[:, 0:8], km[:, 0:8], 1.0 / 256, kmb[:, 0:8], ALU.mult, ALU.subtract, ['km', 'kmb'], ['kmr'])
            CP(kmb[:, 8:16], kmr[:, 0:8], ['kmr'], ['kmb'])
            et = etab[:, h * 128:(h + 1) * 128].rearrange("p (q s) -> p q s", q=16)
            for qt in range(16):
                bq = qt // 2
                if bq == 0:
                    continue
                if bq >= 4:
                    pg, kg = PSF()
                    MM(pg[:, 0:8], qb[:, qt * 128:(qt + 1) * 128], kmb[:, 0:8], True, False, [qk, 'kmb'], [kg])
                    MM(pg[:, 0:8], qb[:, qt * 128:(qt + 1) * 128], kmb[:, 8:16], False, True, [qk, 'kmb'], [kg])
                    TT(gm[:, :], pg[:, 0:8], negmask[:, bq * 8:(bq + 1) * 8], ALU.add, [kg, 'cf'], ['gm'])
                    P.add('dve', lambda e: e.max(top8[:, :], gm[:, :]), ['gm'], ['top8'])
                    TS(sel[:, :], gm[:, :], top8[:, 2:3], None, ALU.is_ge, None, ['gm', 'top8'], ['sel'])
                    TT(Fq[:, qt, :], et[:, qt, :], sel[:, :], ALU.mult, ['sel', 'cf'], ['Fq'])
                else:
                    CP(Fq[:, qt, :], et[:, qt, :], ['cf'], ['Fq'])
            acc = hsb[:, :, 0:129]
            P.add('pool', lambda e: e.memset(dn[:, 2:3], 1.0), ['hsb'] + [f"acc{q}" for q in range(16)],
                  ['dn2', 'hsb'] + [f"acc{q}" for q in range(16)])
            sc = 128.0 ** -0.5
            bias = slopecol[:, 2 * h + 1:2 * h + 2]
            biasp = [slopecol[:, 2 * h:2 * h + 1], slopecol[:, 2 * h + 1:2 * h + 2]]
            items = []
            npast = 0
            for bq in range(8):
                items.append(('d', bq, 0, 0, 0))
                for j in range(bq):
                    items.append(('p', bq, j, 0, npast % 3))
                    npast += 1

            def s1(it):
                kind, bq, j, kt, pi = it
                qs = slice(bq * 256, (bq + 1) * 256)
                if kind == 'd':
                    for kt2 in range(2):
                        st = 2 * bq + kt2
                        pd = (bq % 2) * 2 + kt2
                        pS, kS = PSF()
                        MM(pS[:, 0:256], kb[:, st * 128:(st + 1) * 128], qb[:, qs], True, True, [qk], [kS])
                        ACT(Ptd4[pd][:, :], pS[:, 0:256], AF.Exp, [kS, 'cf'], [f"Ptd{pd}"], scale=sc, bias=bias)
                        TT(Ptd4[pd][:, :], Ptd4[pd][:, :], gdiag[:, (h * 2 + kt2) * 256:(h * 2 + kt2 + 1) * 256],
                           ALU.mult, [f"Ptd{pd}", 'gdiag'], [f"Ptd{pd}"], eng='pool')
                else:
                    for kt2 in range(2):
                        st = 2 * j + kt2
                        pp_ = pi * 2 + kt2
                        pS, kS = PSF()
                        MM(pS[:, 0:256], kb[:, st * 128:(st + 1) * 128], qb[:, qs], True, True, [qk], [kS])
                        ACT(Ptb[pp_][:, :], pS[:, 0:256], AF.Exp, [kS, 'cf'], [f"Ptb{pp_}"], scale=sc, bias=biasp[kt2])

            def s2(it):
                kind, bq, j, kt, pi = it
                if kind == 'd':
                    p0, p1 = (bq % 2) * 2, (bq % 2) * 2 + 1
                    pO, kO = PSF()
                    MM(pO[:, 0:129], Ptd4[p0][:, 0:128], va[:, 2 * bq, 0:129], True, True, [f"Ptd{p0}", vk], [kO])
                    CP(acc[:, 2 * bq, :], pO[:, 0:129], [kO], [f"acc{2 * bq}"], eng='act')
                    pO, kO = PSF()
                    MM(pO[:, 0:129], Ptd4[p0][:, 128:256], va[:, 2 * bq, 0:129], True, False, [f"Ptd{p0}", vk], [kO])
                    MM(pO[:, 0:129], Ptd4[p1][:, 128:256], va[:, 2 * bq + 1, 0:129], False, True, [f"Ptd{p1}", vk], [kO])
                    CP(acc[:, 2 * bq + 1, :], pO[:, 0:129], [kO], [f"acc{2 * bq + 1}"], eng='act')
                else:
                    for qh in range(2):
                        qt = 2 * bq + qh
                        pO, kO = PSF()
                        for kt2 in range(2):
                            pp_ = pi * 2 + kt2
                            MM(pO[:, 0:129], Ptb[pp_][:, qh * 128:(qh + 1) * 128], va[:, 2 * j + kt2, 0:129],
                               kt2 == 0, kt2 == 1, [f"Ptb{pp_}", vk], [kO])
                        STT(acc[:, qt, :], pO[:, 0:129], Fq[:, qt, j:j + 1], acc[:, qt, :], ALU.mult, ALU.add,
                            [kO, 'Fq', f"acc{qt}"], [f"acc{qt}"])

            LA = 2
            for i in range(min(LA, len(items))):
                s1(items[i])
            for i in range(len(items)):
                if i + LA < len(items):
                    s1(items[i + LA])
                s2(items[i])
                step()
            yb = hbq[:, 0, :]
            for qt in range(16):
                RECIP(dn[:, 2:3], acc[:, qt, 128:129], [f"acc{qt}"], ['dn2'])
                TS(ofb[:, :], acc[:, qt, 0:128], dn[:, 2:3], None, ALU.mult, None, [f"acc{qt}", 'dn2'], ['ofb'])
                pT, kT = PSF()
                TR(pT[:, 0:128], ofb[:, :], ident_f, ['ofb', 'cf'], [kT])
                TT(yb[:, qt * 128:(qt + 1) * 128], pT[:, 0:128], zs[:, qt * 128:(qt + 1) * 128], ALU.mult,
                   [kT, zk], ['hbq'])
                step()
            DMA(yT_d[l, chn, :, :], yb, ['hbq'], ['yTd'])

        def prefetch_wout():
            save = A.off
            A.off = persist_mark
            wv = A.alloc([128, NCH, D], BF16)
            A.off = save
            for ci in range(16):
                r0, K = chunks[ci]
                DMA(wv[0:K, ci, :], w_out[l, r0:r0 + K, :], (), ['hT0', 'hT1', 'hT2', 'hT3', 'wout'], eng='pool')

        use_pf = cfg.get('pf', True) and stop in ('all', 'out')
        g0 = proj_gen(0, MSET[0], done=(prefetch_wout if (NO == 1 and use_pf) else None))
        for _ in g0:
            pass
        for h in range(NO):
            gn = None
            if h + 1 < NO:
                gn = proj_gen(h + 1, MSET[(h + 1) % 2],
                              done=(prefetch_wout if (h + 1 == NO - 1 and use_pf) else None))

            def step(gn=gn):
                if gn is not None:
                    next(gn, None)

            mo_main(h, MSET[h % 2], step)
            if gn is not None:
                for _ in gn:
                    pass

    chunks = []
    for g in range(NL):
        chunks.append((g * 128, 128))
    for h in range(NM):
        for cc in range(2):
            chunks.append((512 + h * 192 + cc * 96, 96))
    for h in range(NO):
        chunks.append((1280 + h * 128, 128))

    def out_phase(l, xsrc, xdst, final):
        A.off = persist_mark
        P.barrier()
        wout = A.alloc([128, NCH, D], BF16)
        wq = A.alloc([128, KD, 512], BF16)
        wo = A.alloc([128, 4, D], BF16)
        hmT = A.alloc([128, KD, MEM], BF16)
        kT = A.alloc([128, 4, MEM], BF16)
        vx = A.alloc([128, 2, 4, 129], BF16)
        xblk = A.alloc([128, 2, D])
        yblk = A.alloc([128, NCH, 256], BF16)
        hx = A.alloc([128, 2, D], BF16)
        hxT = A.alloc([128, KD, 256], BF16)
        qT = A.alloc([128, 4, 256], BF16)
        PT = A.alloc([128, 2, 2, 256], BF16)
        oh = A.alloc([128, 2, 512], BF16)
        ohT = A.alloc([128, 4, 256], BF16)
        gbx = A.alloc([128, D])
        gbf = A.alloc([128, D])
        rd = A.alloc([128, 2])
        for ci, (r0, K) in enumerate(chunks):
            if ci < 16 and cfg.get('pf', True):
                continue
            DMA(wout[0:K, ci, :], w_out[l, r0:r0 + K, :], (), ['wout'], eng='pool')
        DMA(wq[:, :, :], xa_wq[l].rearrange("(k p) c -> p k c", p=128), (), ['wq'], eng='pool')
        DMA(wo[:, :, :], xa_wo[l].rearrange("(h p) n -> p h n", p=128), (), ['wo'], eng='pool')
        DMA(gbx[:, :], gains[l, 2].partition_broadcast(128), (), ['gbx'])
        for i in range(2):
            DMA(xblk[:, i, :], mem_in[i * 128:(i + 1) * 128, :], (), [f"xblk{i}"])
            norm_tile(xblk[:, i, :], f"xblk{i}", gbx[:, :], 'gbx', hx[:, i, :], f"hx{i}", hx[:, i, :], f"hx{i}")
            transpose_16(hx[:, i, :], f"hx{i}", lambda half, i=i: hmT[:, half * 8:(half + 1) * 8, i * 128:(i + 1) * 128],
                         'hmT')
        wkv = xblk[:, :, :].rearrange("p a d -> p (a d)").bitcast(BF16).rearrange("p (k c) -> p k c", k=KD)
        DMA(wkv, xa_wkv[l][:, 0:512].rearrange("(k p) c -> p k c", p=128), (), ['xblk0', 'xblk1'], eng='pool')
        for h in range(4):
            pt, pk = PSF()
            for k in range(KD):
                MM(pt[:, 0:MEM], wkv[:, k, h * 128:(h + 1) * 128], hmT[:, k, :], k == 0, k == KD - 1, ['xblk0', 'xblk1', 'hmT'], [pk])
            CP(kT[:, h, :], pt[:, 0:MEM], [pk], ['kT'], eng='act')
        DMA(wkv, xa_wkv[l][:, 512:1024].rearrange("(k p) c -> p k c", p=128), (), ['xblk0', 'xblk1'], eng='pool')
        MEMSET(vx[:, :, :, 128:129], 1.0, ['vx'])
        for mt in range(2):
            pt, pk = PSF()
            for k in range(KD):
                MM(pt[:, 0:512], hmT[:, k, mt * 128:(mt + 1) * 128], wkv[:, k, :], k == 0, k == KD - 1, ['xblk0', 'xblk1', 'hmT'], [pk])
            CP(vx[:, mt, :, 0:128], pt[:, 0:512].rearrange("p (h e) -> p h e", h=4), [pk], ['vx'])
        DMA(gbx[:, :], gains[l, 1].partition_broadcast(128), ['gbx'], ['gbx'])
        if final:
            DMA(gbf[:, :], final_g.partition_broadcast(128), (), ['gbf'])
        sc = 128.0 ** -0.5
        XK = ['xblk0', 'xblk1']
        c1, c2 = NL, NL + 2 * NM

        def load_y(tb):
            rows = slice(tb * 256, (tb + 1) * 256)
            DMA(yblk[:, 0:c1, :], yT_d[l, 0:c1, :, rows].rearrange("c p t -> p c t"), ['yTd'], ['yblk'])
            DMA(yblk[0:96, c1:c2, :], yT_d[l, c1:c2, 0:96, rows].rearrange("c p t -> p c t"), ['yTd'], ['yblk'])
            DMA(yblk[:, c2:NCH, :], yT_d[l, c2:NCH, :, rows].rearrange("c p t -> p c t"), ['yTd'], ['yblk'])

        load_y(0)
        for tb in range(8):
            rows = slice(tb * 256, (tb + 1) * 256)
            for tt in range(2):
                DMA(xblk[:, tt, :], xsrc[tb * 256 + tt * 128: tb * 256 + (tt + 1) * 128, :], (), [XK[tt]])
            for tt in range(2):
                for nb in range(4):
                    ns = slice(nb * 512, (nb + 1) * 512)
                    pt, pk = PSF()
                    for ci, (r0, K) in enumerate(chunks):
                        MM(pt[:, :], yblk[0:K, ci, tt * 128:(tt + 1) * 128], wout[0:K, ci, ns], ci == 0, ci == NCH - 1,
                           ['yblk', 'wout'], [pk])
                    TT(xblk[:, tt, ns], xblk[:, tt, ns], pt[:, :], ALU.add, [XK[tt], pk], [XK[tt]])
            if tb + 1 < 8:
                load_y(tb + 1)
            for tt in range(2):
                norm_tile(xblk[:, tt, :], XK[tt], gbx[:, :], 'gbx', hx[:, tt, :], f"hx{tt}", hx[:, tt, :], f"hx{tt}")
                transpose_16(hx[:, tt, :], f"hx{tt}",
                             lambda half, tt=tt: hxT[:, half * 8:(half + 1) * 8, tt * 128:(tt + 1) * 128], 'hxT')
            for h in range(4):
                pt, pk = PSF()
                for k in range(KD):
                    MM(pt[:, 0:256], wq[:, k, h * 128:(h + 1) * 128], hxT[:, k, :], k == 0, k == KD - 1, ['wq', 'hxT'], [pk])
                CP(qT[:, h, :], pt[:, 0:256], [pk], [f"qT{h}"], eng='act')
            for h in range(4):
                for mt in range(2):
                    pS, kS = PSF()
                    MM(pS[:, 0:256], kT[:, h, mt * 128:(mt + 1) * 128], qT[:, h, :], True, True, ['kT', f"qT{h}"], [kS])
                    ACT(PT[:, h % 2, mt, :], pS[:, 0:256], AF.Exp, [kS], [f"PT{h % 2}"], scale=sc)
                for tt in range(2):
                    pO, kO = PSF()
                    for mt in range(2):
                        MM(pO[:, 0:129], PT[:, h % 2, mt, tt * 128:(tt + 1) * 128], vx[:, mt, h, :], mt == 0, mt == 1,
                           [f"PT{h % 2}", 'vx'], [kO])
                    RECIP(rd[:, tt:tt + 1], pO[:, 128:129], [kO], [f"rd{tt}"])
                    TS(oh[:, tt, h * 128:(h + 1) * 128], pO[:, 0:128], rd[:, tt:tt + 1], None, ALU.mult, None,
                       [kO, f"rd{tt}"], [f"oh{tt}"])
            for tt in range(2):
                pb, pk = PSB()
                for h in range(4):
                    TR(pb[:, h * 128:(h + 1) * 128], oh[:, tt, h * 128:(h + 1) * 128], identb[:, :], [f"oh{tt}", 'identb'], [pk])
                CP(ohT[:, :, tt * 128:(tt + 1) * 128], pb[:, 0:512].rearrange("p (h t) -> p h t", h=4), [pk], [f"ohT{tt}"],
                   eng=('act' if tt else 'dve'))
            for tt in range(2):
                for nb in range(4):
                    ns = slice(nb * 512, (nb + 1) * 512)
                    pt, pk = PSF()
                    for h in range(4):
                        MM(pt[:, :], ohT[:, h, tt * 128:(tt + 1) * 128], wo[:, h, ns], h == 0, h == 3, [f"ohT{tt}", 'wo'], [pk])
                    TT(xblk[:, tt, ns], xblk[:, tt, ns], pt[:, :], ALU.add, [XK[tt], pk], [XK[tt]])
                if final:
                    norm_tile(xblk[:, tt, :], XK[tt], gbf[:, :], 'gbf', xblk[:, tt, :], XK[tt], hx[:, tt, :], f"hx{tt}")
                DMA(xdst[tb * 256 + tt * 128: tb * 256 + (tt + 1) * 128, :], xblk[:, tt, :], [XK[tt]], ['xdst'])

    src = x_in
    for l in range(nlayers):
        mix_phase(l, src)
        if stop in ('A', 'lru', 'mlg', 'mlh1', 'mlh_a', 'mlh_c', 'mlh_a0', 'mlh_a1', 'ml', 'mo'):
            break
        last = (l == nlayers - 1)
        out_phase(l, src, out_d if last else xs0, final=(last and nlayers == L))
        src = xs0
        if stop == 'out':
            break
    P.emit()
    return nc, es


def alibi_slopes(n):
    def pow2(m):
        start = 2.0 ** (-8.0 / m)
        return [start ** (i + 1) for i in range(m)]
    if math.log2(n).is_integer():
        s = pow2(n)
    else:
        c = 2 ** int(math.floor(math.log2(n)))
        s = pow2(c) + pow2(2 * c)[0::2][:n - c]
    return np.array(s, dtype=np.float64)


def make_consts(mo_heads):
    NO = len(mo_heads)
    CO, NCF = const_layout(NO)
    cf = np.zeros((128, NCF), np.float32)
    cf[:, CO['ident']:CO['ident'] + 128] = np.eye(128, dtype=np.float32)
    s = np.arange(128)[:, None]
    j = np.arange(128)[None, :]
    cf[:, CO['tri']:CO['tri'] + 128] = (s <= j).astype(np.float32)
    nm = np.zeros((8, 8), np.float32)
    for bq in range(8):
        nm[bq, bq:] = NEG
    cf[:, CO['negmask']:CO['negmask'] + 64] = nm.reshape(1, 64)
    slopes = alibi_slopes(6)
    p = np.arange(128, dtype=np.float64)
    gd = np.zeros((128, NO, 2, 256), np.float64)
    for hi, hg in enumerate(mo_heads):
        sl = slopes[hg]
        cf[:, CO['slope'] + 2 * hi] = (sl * (p - 255.0)).astype(np.float32)
        cf[:, CO['slope'] + 2 * hi + 1] = (sl * (p - 127.0)).astype(np.float32)
        et = np.zeros((128, 16, 8), np.float64)
        for qt in range(16):
            bq = qt // 2
            for jb in range(bq):
                t = qt * 128 + p
                sref = jb * 256 + 255
                et[:, qt, jb] = np.exp(-sl * (t - sref))
        cf[:, CO['etab'] + hi * 128: CO['etab'] + (hi + 1) * 128] = et.reshape(128, 128).astype(np.float32)
        for kt in range(2):
            sabs = kt * 128 + p[:, None]
            sref = kt * 128 + 127
            t = np.arange(256, dtype=np.float64)[None, :]
            g = np.exp(-sl * (t - sref)) * (sabs <= t)
            gd[:, hi, kt, :] = g
    cf[:, CO['misc'] + 0] = 1e-6
    cf[:, CO['misc'] + 1] = 1e-5
    cf[:, CO['misc'] + 2] = 1.0
    gd = np.minimum(gd, 3.0e38).astype(np.float32).reshape(128, NO * 512).astype(ml_dtypes.bfloat16)
    return cf, gd


def pack_core(inp, b, lru_blocks, ml_heads, mo_heads):
    NL, NM, NO = len(lru_blocks), len(ml_heads), len(mo_heads)
    PPO, NPP = pp_layout(NL, NM)
    w_in = inp['w_in']
    o_lx, o_lz, o_mu, o_mo, o_mz, o_mi, o_mf, o_oq, o_ok, o_ov, o_oz = (
        0, 512, 1024, 1792, 2560, 3328, 3332, 3336, 4104, 4872, 5640)
    cols = []
    for base in (o_lx, o_lz):
        for g in lru_blocks:
            cols.append(np.arange(base + g * 128, base + (g + 1) * 128))
    for base in (o_mu, o_mo, o_mz):
        for h in ml_heads:
            cols.append(np.arange(base + h * 192, base + (h + 1) * 192))
    for base in (o_mi, o_mf):
        cols.append(np.array([base + h for h in ml_heads]))
    for base in (o_oq, o_ok, o_ov, o_oz):
        for h in mo_heads:
            cols.append(np.arange(base + h * 128, base + (h + 1) * 128))
    cols = np.concatenate(cols)
    full = (len(cols) == w_in.shape[2]) and np.array_equal(cols, np.arange(w_in.shape[2]))
    w_in_c = w_in if full else np.ascontiguousarray(w_in[:, :, cols])
    pp = np.zeros((L, 128, NPP), np.float32)
    for l in range(L):
        for gi, g in enumerate(lru_blocks):
            sl = slice(g * 128, (g + 1) * 128)
            o = PPO['lru'] + gi * 8
            pp[l, :, o:o + 4] = inp['lru_conv_w'][l][:, sl].T
            pp[l, :, o + 4] = inp['lru_conv_b'][l][sl]
            pp[l, :, o + 5] = inp['lru_ba'][l][sl]
            pp[l, :, o + 6] = inp['lru_bx'][l][sl]
            pp[l, :, o + 7] = inp['lru_lambda'][l][sl]
        for hi, h in enumerate(ml_heads):
            for cc in range(2):
                sl = slice(h * 192 + cc * 96, h * 192 + (cc + 1) * 96)
                o = PPO['ml'] + (hi * 2 + cc) * 5
                pp[l, 0:96, o:o + 4] = inp['ml_conv_w'][l][:, sl].T
                pp[l, 0:96, o + 4] = inp['ml_conv_b'][l][sl]
            pp[l, hi, PPO['gb']] = inp['ml_bi'][l][h]
            pp[l, hi, PPO['gb'] + 1] = -inp['ml_bf'][l][h]
    lru_w = np.stack([inp['lru_wa'][:, lru_blocks], inp['lru_wx'][:, lru_blocks]], axis=1)
    ml_w = np.zeros((L, 3, NM, 2, 96, 96), np.float32)
    for ai, nm_ in enumerate(('ml_wq', 'ml_wk', 'ml_wv')):
        w = inp[nm_]
        for hi, h in enumerate(ml_heads):
            for cc in range(2):
                for bl in range(24):
                    gidx = h * 48 + cc * 24 + bl
                    ml_w[:, ai, hi, cc, bl * 4:(bl + 1) * 4, bl * 4:(bl + 1) * 4] = w[:, gidx]
    ml_ng = np.concatenate([inp['ml_norm_g'][:, h * 192:(h + 1) * 192] for h in ml_heads], axis=1)
    gains = np.stack([inp['mix_norm_g'], inp['xa_norm_g'], inp['mem_norm_g']], axis=1)
    return {
        "x": np.ascontiguousarray(inp['x'][b]), "mem": np.ascontiguousarray(inp['mem'][b]),
        "w_in": w_in_c, "w_out": inp['w_out'], "gains": np.ascontiguousarray(gains),
        "final_g": inp['final_norm_g'], "pp": pp, "lru_w": np.ascontiguousarray(lru_w),
        "ml_w": ml_w, "ml_ng": np.ascontiguousarray(ml_ng), "xa_wq": inp['xa_wq'], "xa_wkv": inp['xa_wkv'],
        "xa_wo": inp['xa_wo'],
    }


def kernel(**inputs):
    inp = {k: np.asarray(v) for k, v in inputs.items()}
    cfg = dict(NL=4, NM=4, NO=6)
    nc, es = build(cfg)
    cf, gd = make_consts(list(range(6)))
    in_maps = []
    for c in range(8):
        m = pack_core(inp, c // 2, [0, 1, 2, 3], [0, 1, 2, 3], [0, 1, 2, 3, 4, 5])
        m["cst_f"] = cf
        m["cst_b"] = gd
        in_maps.append(m)
    res = run_bass_kernel_spmd(nc, in_maps, core_ids=list(range(8)))
    out = np.stack([res.results[2 * b]["out"] for b in range(4)], axis=0)
    return out.astype(np.float32)
```

```python
import math
import numpy as np
import ml_dtypes
from contextlib import ExitStack
import concourse.bass as bass
import concourse.mybir as mybir
from concourse.bass_utils import run_bass_kernel_spmd

F32 = mybir.dt.float32
BF16 = mybir.dt.bfloat16
AF = mybir.ActivationFunctionType
ALU = mybir.AluOpType
AX = mybir.AxisListType

T = 2048
D = 2048
KD = 16
MEM = 256
L = 2
ENG = ('pe', 'act', 'dve', 'pool', 'sp')
NEG = -1.0e30
ARENA_WORDS = 52400


class Op:
    __slots__ = ('eng', 'fn', 'deps', 'signal', 'sig', 'sigval', 'dma', 'sem', 'semval')


class Prog:
    EPOCH = 6000
    NQ = 12

    def __init__(self, nc, es):
        self.nc = nc
        self.es = es
        self.ops = {e: [] for e in ENG}
        self.state = {}
        self.dma_cnt = {e: 0 for e in ENG}
        self.dma_last = {e: {} for e in ENG}
        self.dma_sems = {}
        self.eng_sems = {}
        self.pending = {e: set() for e in ENG}

    def _sem(self, name):
        return self.es.enter_context(self.nc.semaphore(name))

    limit = None
    count = 0

    def add(self, eng, fn, r=(), w=(), dma=False):
        self.count += 1
        if self.limit is not None and self.count > self.limit:
            return None
        op = Op()
        op.eng = eng; op.fn = fn; op.dma = dma; op.signal = False
        op.sig = 0; op.sigval = 0; op.sem = None; op.semval = 0
        deps = set(self.pending[eng])
        self.pending[eng] = set()
        for k in r:
            st = self.state.get(k)
            if st is not None and st[0] is not None:
                deps.add(st[0])
        for k in w:
            st = self.state.get(k)
            if st is not None:
                if st[0] is not None:
                    deps.add(st[0])
                deps.update(st[1].values())
        for k in r:
            rd = self.state.setdefault(k, [None, {}])[1]
            if dma:
                rd[('dma', id(op))] = op
            else:
                rd[eng] = op
        for k in w:
            self.state[k] = [op, {}]
        if dma:
            if eng not in self.dma_sems:
                self.dma_sems[eng] = [self._sem(f"dq_{eng}_{i}") for i in range(self.NQ)]
            i = self.dma_cnt[eng]
            self.dma_cnt[eng] += 1
            slot = i % self.NQ
            op.sem = self.dma_sems[eng][slot]
            op.semval = 16 * (i // self.NQ + 1)
            prev = self.dma_last[eng].get(slot)
            if prev is not None:
                deps.add(prev)
            self.dma_last[eng][slot] = op
        deps.discard(op)
        fin = []
        for d in deps:
            if (not d.dma) and (not dma) and d.eng == 'pe' and eng == 'pe':
                continue
            if not d.dma:
                d.signal = True
            fin.append(d)
        op.deps = fin
        self.ops[eng].append(op)
        return op

    def barrier(self):
        last = []
        for e in ENG:
            comp = [o for o in self.ops[e] if not o.dma]
            if comp:
                last.append(comp[-1])
            last.extend(self.dma_last[e].values())
        for e in ENG:
            self.pending[e].update(last)

    def emit(self):
        for e in ENG:
            cnt = 0
            for op in self.ops[e]:
                if (not op.dma) and op.signal:
                    op.sig = cnt // self.EPOCH
                    op.sigval = cnt % self.EPOCH + 1
                    cnt += 1
                    if (e, op.sig) not in self.eng_sems:
                        self.eng_sems[(e, op.sig)] = self._sem(f"es_{e}_{op.sig}")
        prog = self

        def run(e, h):
            waited = {}
            for op in prog.ops[e]:
                need = {}
                for d in op.deps:
                    if d.dma:
                        s, v = d.sem, d.semval
                    else:
                        s, v = prog.eng_sems[(d.eng, d.sig)], d.sigval
                    if need.get(id(s), (None, 0))[1] < v:
                        need[id(s)] = (s, v)
                for k, (s, v) in need.items():
                    if waited.get(k, 0) < v:
                        h.wait_ge(s, v)
                        waited[k] = v
                ins = op.fn(h)
                if op.dma:
                    ins.then_inc(op.sem, 16)
                elif op.signal:
                    ins.then_inc(prog.eng_sems[(e, op.sig)], 1)
            if e == 'sp':
                for q in ENG:
                    for slot, o in prog.dma_last[q].items():
                        h.wait_ge(o.sem, o.semval)

        with self.nc.Block() as block:
            @block.tensor
            def _(h):
                run('pe', h)

            @block.scalar
            def _(h):
                run('act', h)

            @block.vector
            def _(h):
                run('dve', h)

            @block.gpsimd
            def _(h):
                run('pool', h)

            @block.sync
            def _(h):
                run('sp', h)


class Arena:
    def __init__(self, t, n):
        self.t = t; self.n = n; self.off = 0

    def alloc(self, shape, dt=F32):
        nel = 1
        for s in shape[1:]:
            nel *= s
        words = nel if dt == F32 else (nel + 1) // 2
        words = (words + 1) // 2 * 2
        assert self.off + words <= self.n, f"arena overflow {self.off}+{words}>{self.n}"
        v = self.t[0:shape[0], self.off:self.off + words]
        self.off += words
        if dt != F32:
            v = v.bitcast(dt)
        v = v[:, 0:nel]
        if len(shape) == 3:
            v = v.rearrange("p (a b) -> p a b", a=shape[1])
        elif len(shape) == 4:
            v = v.rearrange("p (a b c) -> p a b c", a=shape[1], b=shape[2])
        return v


def seg_offsets(NL, NM, NO):
    sizes = [('lx', NL * 128), ('lz', NL * 128), ('mu', NM * 192), ('mo', NM * 192), ('mz', NM * 192),
             ('mi', NM), ('mf', NM), ('oq', NO * 128), ('ok', NO * 128), ('ov', NO * 128), ('oz', NO * 128)]
    off = {}
    o = 0
    for n, s in sizes:
        off[n] = o
        o += s
    return off, o


def const_layout(NO):
    CO = {}
    o = 0
    for n, s in [('ident', 128), ('tri', 128), ('negmask', 64), ('slope', NO * 2), ('etab', NO * 128), ('misc', 4)]:
        CO[n] = o
        o += s
    return CO, o


def pp_layout(NL, NM):
    PPO = {'lru': 0, 'ml': NL * 8, 'gb': NL * 8 + NM * 10}
    return PPO, NL * 8 + NM * 10 + 2


def build(cfg):
    NL, NM, NO = cfg['NL'], cfg['NM'], cfg['NO']
    nlayers = cfg.get('layers', L)
    stop = cfg.get('stop', 'all')
    dbg = cfg.get('dbg', False)
    SEG, WC = seg_offsets(NL, NM, NO)
    NCH = NL + 2 * NM + NO
    CO, NCF = const_layout(NO)
    PPO, NPP = pp_layout(NL, NM)
    nc = bass.Bass("TRN2", target_bir_lowering=False)
    es = ExitStack()
    P = Prog(nc, es)
    P.limit = cfg.get('limit', None)

    def din(name, shape, dt=F32):
        return nc.dram_tensor(name, list(shape), dt, kind="ExternalInput").ap()

    def dscr(name, shape, dt=F32, out=False):
        return nc.dram_tensor(name, list(shape), dt, kind="ExternalOutput" if out else "Internal").ap()

    x_in = din("x", [T, D])
    mem_in = din("mem", [MEM, D])
    w_in = din("w_in", [L, D, WC])
    w_out = din("w_out", [L, D, D])
    gains = din("gains", [L, 3, D])
    final_g = din("final_g", [D])
    pp_in = din("pp", [L, 128, NPP])
    lru_w = din("lru_w", [L, 2, NL, 128, 128])
    ml_w = din("ml_w", [L, 3, NM, 2, 96, 96])
    ml_ng = din("ml_ng", [L, NM * 192])
    xa_wq = din("xa_wq", [L, D, 512])
    xa_wkv = din("xa_wkv", [L, D, 1024])
    xa_wo = din("xa_wo", [L, 512, D])
    cst_f = din("cst_f", [128, NCF])
    cst_b = din("cst_b", [128, NO * 512], BF16)
    out_d = nc.dram_tensor("out", [T, D], F32, kind="ExternalOutput").ap()
    xs0 = dscr("xs0", [T, D], F32, out=dbg)
    yT_d = dscr("yT", [L, NCH, 128, T], BF16, out=dbg)
    dbg_f = dscr("dbgf", [128, 4096], F32, out=True) if dbg else None

    arena_t = es.enter_context(nc.sbuf_tensor("arena", [128, ARENA_WORDS], F32))
    A = Arena(arena_t, ARENA_WORDS)

    def ps(name, shape, dt=F32):
        return es.enter_context(nc.psum_tensor(name, list(shape), dt))

    def DMA(out, in_, r, w, eng='sp', **kw):
        P.add(eng, lambda e: e.dma_start(out=out, in_=in_, **kw), r, w, dma=True)

    def MM(out, lhsT, rhs, start, stop, r, w, **kw):
        P.add('pe', lambda e: e.matmul(out, lhsT, rhs, start=start, stop=stop, **kw), r, w)

    def TR(out, in_, ident, r, w):
        P.add('pe', lambda e: e.transpose(out, in_, ident), r, w)

    def ACT(out, in_, func, r, w, **kw):
        P.add('act', lambda e: e.activation(out=out, in_=in_, func=func, **kw), r, w)

    def TS(out, in0, s1, s2, op0, op1, r, w, eng='dve', **kw):
        if op1 is None:
            P.add(eng, lambda e: e.tensor_scalar(out, in0, s1, None, op0, **kw), r, w)
        else:
            P.add(eng, lambda e: e.tensor_scalar(out, in0, s1, s2, op0, op1, **kw), r, w)

    def TT(out, in0, in1, op, r, w, eng='dve'):
        P.add(eng, lambda e: e.tensor_tensor(out, in0, in1, op), r, w)

    def STT(out, in0, scalar, in1, op0, op1, r, w, **kw):
        P.add('dve', lambda e: e.scalar_tensor_tensor(out, in0, scalar, in1, op0, op1, **kw), r, w)

    def CP(out, in_, r, w, eng='dve'):
        if eng == 'act':
            ACT(out, in_, AF.Copy, r, w)
        else:
            P.add(eng, lambda e: e.tensor_copy(out, in_), r, w)

    def RECIP(out, in_, r, w):
        P.add('dve', lambda e: e.reciprocal(out, in_), r, w)

    def SCAN(out, d0, d1, init, op0, op1, r, w):
        P.add('dve', lambda e: e.tensor_tensor_scan(out, d0, d1, init, op0, op1), r, w)

    def MEMSET(ap, val, w, eng='pool'):
        P.add(eng, lambda e: e.memset(ap, val), (), w)

    def chv(ap):
        return ap.rearrange("p (c t) -> p c t", t=128)

    cf = A.alloc([128, NCF])
    DMA(cf[:, :], cst_f[:, :], (), ['cf'])
    ident_f = cf[:, CO['ident']:CO['ident'] + 128]
    tri_f = cf[:, CO['tri']:CO['tri'] + 128]
    negmask = cf[:, CO['negmask']:CO['negmask'] + 64]
    slopecol = cf[:, CO['slope']:CO['slope'] + NO * 2]
    etab = cf[:, CO['etab']:CO['etab'] + NO * 128]
    eps6 = cf[:, CO['misc']:CO['misc'] + 1]
    eps5 = cf[:, CO['misc'] + 1:CO['misc'] + 2]
    one1 = cf[:, CO['misc'] + 2:CO['misc'] + 3]
    gdiag = A.alloc([128, NO * 512], BF16)
    DMA(gdiag[:, :], cst_b[:, :], (), ['gdiag'])
    identb = A.alloc([128, 128], BF16)
    CP(identb[:, :], ident_f, ['cf'], ['identb'])
    ones_f = A.alloc([128, 128])
    MEMSET(ones_f[:, :], 1.0, ['ones_f'])
    zeros_f = A.alloc([128, 128])
    MEMSET(zeros_f[:, :], 0.0, ['zeros_f'])
    pp = A.alloc([128, NPP])
    nst = A.alloc([128, 8])
    persist_mark = A.off

    NPS = 6
    psf = [ps(f"psf{i}", [128, 512]) for i in range(NPS)]
    psb = [ps(f"psb{i}", [128, 1024], BF16) for i in range(2)]
    pctr = [0, 0]

    def PSF():
        i = pctr[0] % NPS
        pctr[0] += 1
        return psf[i], f"psf{i}"

    def PSB():
        i = pctr[1] % 2
        pctr[1] += 1
        return psb[i], f"psb{i}"

    def norm_tile(xt_ap, xkey, gbc_ap, gkey, out_ap, okey, junk_ap, jkey):
        STT(junk_ap, xt_ap, 1.0, xt_ap, ALU.mult, ALU.mult, [xkey], [jkey, 'n_ss'], accum_out=nst[:, 0:1])
        ACT(nst[:, 1:2], nst[:, 0:1], AF.Sqrt, ['n_ss', 'cf'], ['n_sd'], scale=1.0 / D, bias=eps6)
        RECIP(nst[:, 2:3], nst[:, 1:2], ['n_sd'], ['n_rstd'])
        STT(out_ap, xt_ap, nst[:, 2:3], gbc_ap, ALU.mult, ALU.mult, [xkey, 'n_rstd', gkey], [okey])

    def transpose_16(hb, hbkey, dst_fn, dkey):
        for half in range(2):
            pb, pk = PSB()
            for kk in range(8):
                k = half * 8 + kk
                TR(pb[:, kk * 128:(kk + 1) * 128], hb[:, k * 128:(k + 1) * 128], identb[:, :],
                   [hbkey, 'identb'], [pk])
            src = pb[:, :].rearrange("p (k t) -> p k t", k=8)
            CP(dst_fn(half), src, [pk], [dkey], eng=('act' if half == 0 else 'dve'))

    def mix_phase(l, src):
        A.off = persist_mark
        P.barrier()
        hT = A.alloc([128, KD, T], BF16)
        mb = [A.alloc([128, T + 8]) for _ in range(5)]
        mbb = [A.alloc([128, T], BF16) for _ in range(2)]
        wbuf = [A.alloc([128, KD, 192], BF16) for _ in range(2)]
        hba = A.alloc([128, 2, T], BF16)
        hbq = A.alloc([128, 2, T], BF16)
        hbk = A.alloc([128, 2, T], BF16)
        vaug = A.alloc([128, 16, 193], BF16)
        hsb = A.alloc([128, 16, 193])
        ngbc = A.alloc([128, NM * 192])
        colbuf = A.alloc([128, 16, 5 * NM])
        soldbc = A.alloc([128, NM * 16])
        soldrow = A.alloc([1, NM * 16])
        sm = A.alloc([128, 160])
        lw = A.alloc([128, 2, 128], BF16)
        mlw = A.alloc([96, 3, 2, 96], BF16)
        Cst = A.alloc([96, 2, 193])
        Cb = A.alloc([96, 2, 193], BF16)
        Stb2 = [A.alloc([128, 128], BF16) for _ in range(2)]
        sob2 = [A.alloc([128, 192]) for _ in range(2)]
        kwb2 = [A.alloc([128, 192], BF16) for _ in range(2)]
        st6 = A.alloc([128, 16, 6])
        dn16 = A.alloc([128, 64])
        mv = A.alloc([128, 16, 2])
        dn = A.alloc([128, 4])
        km = A.alloc([128, 8])
        gm = A.alloc([128, 8])
        top8 = A.alloc([128, 8])
        sel = A.alloc([128, 8])
        Fq = A.alloc([128, 16, 8])
        Ptb = [A.alloc([128, 256], BF16) for _ in range(6)]
        Ptd4 = [A.alloc([128, 256], BF16) for _ in range(4)]
        ofb = A.alloc([128, 128])
        l_c8 = A.alloc([128, 4])

        DMA(pp[:, :], pp_in[l], (), ['pp'])
        DMA(ngbc[:, :], ml_ng[l].partition_broadcast(128), (), ['ngbc'])

        gbc = mb[3][:, 0:D]
        DMA(gbc, gains[l, 0].partition_broadcast(128), (), ['mb3'])
        for i in range(16):
            xt = mb[i % 2][:, 0:D]
            xk = f"mb{i % 2}"
            DMA(xt, src[i * 128:(i + 1) * 128, :], (), [xk])
            hb = mbb[i % 2][:, 0:D]
            hk = f"mbb{i % 2}"
            norm_tile(xt, xk, gbc, 'mb3', hb, hk, hb, hk)
            transpose_16(hb, hk, lambda half, i=i: hT[:, half * 8:(half + 1) * 8, i * 128:(i + 1) * 128],
                         f"hT{i // 4}")
        if stop == 'A':
            DMA(yT_d[l, 0:16].rearrange("c p t -> p c t"), hT[:, :, :], [f"hT{i}" for i in range(4)], ['yTd'])
            return

        wctr = [0]

        def load_w(src2d, ncols):
            i = wctr[0] % 2
            wctr[0] += 1
            wt = wbuf[i]
            key = f"wbuf{i}"
            DMA(wt[:, :, 0:ncols], src2d.rearrange("(k p) c -> p k c", p=128), (), [key], eng='pool')
            return wt, key

        def proj_cm(wt, wkey, c0, M, tb):
            pt, pk = PSF()
            for k in range(KD):
                MM(pt[0:M, 0:512], wt[:, k, c0:c0 + M], hT[:, k, tb * 512:(tb + 1) * 512], k == 0, k == KD - 1,
                   [wkey, f"hT{tb}"], [pk])
            return pt, pk

        def proj_tm(wt, wkey, c0, N, tt):
            pt, pk = PSF()
            for k in range(KD):
                MM(pt[:, 0:N], hT[:, k, tt * 128:(tt + 1) * 128], wt[:, k, c0:c0 + N], k == 0, k == KD - 1,
                   [wkey, f"hT{tt // 4}"], [pk])
            return pt, pk

        def lru_group(g):
            wx_t, wxk = load_w(w_in[l][:, SEG['lx'] + g * 128: SEG['lx'] + (g + 1) * 128], 128)
            wz_t, wzk = load_w(w_in[l][:, SEG['lz'] + g * 128: SEG['lz'] + (g + 1) * 128], 128)
            DMA(lw[:, 0, :], lru_w[l, 0, g], (), ['lw0'], eng='pool')
            DMA(lw[:, 1, :], lru_w[l, 1, g], (), ['lw1'], eng='pool')
            po = PPO['lru'] + g * 8
            xpad = mb[0]
            MEMSET(xpad[:, 0:3], 0.0, ['mb0'])
            for tb in range(4):
                pt, pk = proj_cm(wx_t, wxk, 0, 128, tb)
                CP(xpad[:, 3 + tb * 512: 3 + (tb + 1) * 512], pt[:, :], [pk], ['mb0'], eng='act')
            zsl = hba[:, 0, :]
            for tb in range(4):
                sl = slice(tb * 512, (tb + 1) * 512)
                pt, pk = proj_cm(wz_t, wzk, 0, 128, tb)
                ACT(zsl[:, sl], pt[:, :], AF.Silu, [pk], ['hba'])
            acc = mb[1]
            TS(acc[:, 0:T], xpad[:, 0:T], pp[:, po:po + 1], pp[:, po + 4:po + 5], ALU.mult, ALU.add,
               ['mb0', 'pp'], ['mb1'])
            for j in range(1, 4):
                STT(acc[:, 0:T], xpad[:, j:j + T], pp[:, po + j:po + j + 1], acc[:, 0:T], ALU.mult, ALU.add,
                    ['mb0', 'mb1', 'pp'], ['mb1'])
            xcb = mbb[0]
            CP(xcb[:, 0:T], acc[:, 0:T], ['mb1'], ['mbb0'], eng='pool')
            r_, ig = mb[2], mb[3]
            for tb in range(4):
                sl = slice(tb * 512, (tb + 1) * 512)
                pt, pk = PSF()
                MM(pt[:, :], lw[:, 0, :], xcb[:, sl], True, True, ['lw0', 'mbb0'], [pk])
                ACT(r_[:, sl], pt[:, :], AF.Sigmoid, [pk, 'pp'], ['mb2'], bias=pp[:, po + 5:po + 6])
                pt, pk = PSF()
                MM(pt[:, :], lw[:, 1, :], xcb[:, sl], True, True, ['lw1', 'mbb0'], [pk])
                ACT(ig[:, sl], pt[:, :], AF.Sigmoid, [pk, 'pp'], ['mb3'], bias=pp[:, po + 6:po + 7])
            ACT(l_c8[:, 0:1], pp[:, po + 7:po + 8], AF.Exp, ['pp'], ['l_c8'], scale=-1.0)
            ACT(l_c8[:, 1:2], l_c8[:, 0:1], AF.Ln, ['l_c8', 'cf'], ['l_c8b'], bias=one1)
            TS(l_c8[:, 2:3], l_c8[:, 1:2], -8.0, None, ALU.mult, None, ['l_c8b'], ['l_c8c'])
            a_ = mb[4]
            ACT(a_[:, 0:T], r_[:, 0:T], AF.Exp, ['mb2', 'l_c8c'], ['mb4'], scale=l_c8[:, 2:3])
            a2 = mb[2]
            TT(a2[:, 0:T], a_[:, 0:T], a_[:, 0:T], ALU.mult, ['mb4'], ['mb2'])
            ACT(a2[:, 0:T], a2[:, 0:T], AF.Sqrt, ['mb2', 'cf'], ['mb2'], scale=-1.0, bias=one1)
            TT(ig[:, 0:T], ig[:, 0:T], acc[:, 0:T], ALU.mult, ['mb3', 'mb1'], ['mb3'])
            TT(ig[:, 0:T], ig[:, 0:T], a2[:, 0:T], ALU.mult, ['mb3', 'mb2'], ['mb3'])
            hh = mb[1]
            SCAN(hh[:, 0:T], a_[:, 0:T], ig[:, 0:T], 0.0, ALU.mult, ALU.add, ['mb4', 'mb3'], ['mb1'])
            yb = mbb[1]
            TT(yb[:, 0:T], hh[:, 0:T], zsl, ALU.mult, ['mb1', 'hba'], ['mbb1'])
            DMA(yT_d[l, g, :, :], yb[:, 0:T], ['mbb1'], ['yTd'])

        for g in range(NL):
            lru_group(g)
        if stop == 'lru':
            return

        PK = mb[3]
        NR = 5 * NM

        def ml_gates():
            wgi, k1 = load_w(w_in[l][:, SEG['mi']:SEG['mi'] + NM], NM)
            wgf, k2 = load_w(w_in[l][:, SEG['mf']:SEG['mf'] + NM], NM)
            ig, sp_, bb, mm_ = mb[0], mb[1], mb[2], mb[4]
            gb = PPO['gb']
            for tb in range(4):
                sl = slice(tb * 512, (tb + 1) * 512)
                pt, pk = proj_cm(wgi, k1, 0, NM, tb)
                ACT(ig[0:NM, sl], pt[0:NM, :], AF.Identity, [pk, 'pp'], ['mb0'], bias=pp[0:NM, gb:gb + 1])
                pt, pk = proj_cm(wgf, k2, 0, NM, tb)
                ACT(sp_[0:NM, sl], pt[0:NM, :], AF.Exp, [pk, 'pp'], ['mb1'], scale=-1.0,
                    bias=pp[0:NM, gb + 1:gb + 2])
            ACT(sp_[0:NM, 0:T], sp_[0:NM, 0:T], AF.Ln, ['mb1', 'cf'], ['mb1'], bias=one1[0:NM, :])
            for c_ in range(16):
                sl = slice(c_ * 128, (c_ + 1) * 128)
                SCAN(bb[0:NM, sl], ones_f[0:NM, 0:128], sp_[0:NM, sl], 0.0, ALU.mult, ALU.subtract,
                     ['mb1', 'ones_f'], ['mb2'])
            beta = mb[0]
            TT(beta[0:NM, 0:T], ig[0:NM, 0:T], bb[0:NM, 0:T], ALU.subtract, ['mb0', 'mb2'], ['mb0'])
            cmx = mb[1]
            for c_ in range(16):
                sl = slice(c_ * 128, (c_ + 1) * 128)
                SCAN(cmx[0:NM, sl], zeros_f[0:NM, 0:128], beta[0:NM, sl], NEG, ALU.add, ALU.max,
                     ['mb0', 'zeros_f'], ['mb1'])
            gc, mxb, mloc, mnew, mprev, d1, sold = [sm[0:NM, 16 * i:16 * (i + 1)] for i in range(7)]
            CP(gc, chv(bb[0:NM, 0:T])[:, :, 127], ['mb2'], ['sm_gc'])
            CP(mxb, chv(cmx[0:NM, 0:T])[:, :, 127], ['mb1'], ['sm_mxb'])
            TT(mloc, gc, mxb, ALU.add, ['sm_gc', 'sm_mxb'], ['sm_mloc'])
            SCAN(mnew, gc, mloc, 0.0, ALU.add, ALU.max, ['sm_gc', 'sm_mloc'], ['sm_mnew'])
            MEMSET(mprev[:, 0:1], 0.0, ['sm_mprev'], eng='dve')
            CP(mprev[:, 1:16], mnew[:, 0:15], ['sm_mnew'], ['sm_mprev'])
            bc = lambda a: a.unsqueeze(2).to_broadcast([NM, 16, 128])
            TT(chv(mm_[0:NM, 0:T]), chv(cmx[0:NM, 0:T]), bc(mprev), ALU.max, ['mb1', 'sm_mprev'], ['mb4'])
            tmp = mb[1]
            rows = []
            ACT(tmp[0:NM, 0:T], beta[0:NM, 0:T], AF.Exp, ['mb0'], ['mb1'])
            for h in range(NM):
                DMA(PK[0 * NM + h:0 * NM + h + 1, 0:T], tmp[h:h + 1, 0:T], ['mb1'], ['mb3'])
            TT(tmp[0:NM, 0:T], mm_[0:NM, 0:T], bb[0:NM, 0:T], ALU.add, ['mb4', 'mb2'], ['mb1'])
            ACT(tmp[0:NM, 0:T], tmp[0:NM, 0:T], AF.Exp, ['mb1'], ['mb1'], scale=-1.0)
            for h in range(NM):
                DMA(PK[3 * NM + h:3 * NM + h + 1, 0:T], tmp[h:h + 1, 0:T], ['mb1'], ['mb3'])
            TT(chv(tmp[0:NM, 0:T]), bc(mprev), chv(mm_[0:NM, 0:T]), ALU.subtract, ['mb4', 'sm_mprev'], ['mb1'])
            ACT(tmp[0:NM, 0:T], tmp[0:NM, 0:T], AF.Exp, ['mb1'], ['mb1'])
            for h in range(NM):
                DMA(PK[2 * NM + h:2 * NM + h + 1, 0:T], tmp[h:h + 1, 0:T], ['mb1'], ['mb3'])
            ACT(tmp[0:NM, 0:T], mm_[0:NM, 0:T], AF.Exp, ['mb4'], ['mb1'], scale=-1.0)
            for h in range(NM):
                DMA(PK[4 * NM + h:4 * NM + h + 1, 0:T], tmp[h:h + 1, 0:T], ['mb1'], ['mb3'])
            TT(d1, gc, mnew, ALU.subtract, ['sm_gc', 'sm_mnew'], ['sm_d1'])
            TT(chv(tmp[0:NM, 0:T]), chv(beta[0:NM, 0:T]), bc(d1), ALU.add, ['mb0', 'sm_d1'], ['mb1'])
            ACT(tmp[0:NM, 0:T], tmp[0:NM, 0:T], AF.Exp, ['mb1'], ['mb1'])
            TS(tmp[0:NM, 0:T], tmp[0:NM, 0:T], 192.0 ** -0.5, None, ALU.mult, None, ['mb1'], ['mb1'])
            for h in range(NM):
                DMA(PK[1 * NM + h:1 * NM + h + 1, 0:T], tmp[h:h + 1, 0:T], ['mb1'], ['mb3'])
            TT(sold, d1, mprev, ALU.add, ['sm_d1', 'sm_mprev'], ['sm_sold'])
            ACT(sold, sold, AF.Exp, ['sm_sold'], ['sm_sold'])
            for h in range(NM):
                DMA(soldrow[0:1, h * 16:(h + 1) * 16], sold[h:h + 1, 0:16], ['sm_sold'], ['soldrow'])
            pt, pk = PSF()
            MM(pt[0:96, 0:NM * 16], ones_f[0:1, 0:96], soldrow[0:1, :], True, True, ['ones_f', 'soldrow'], [pk])
            CP(soldbc[0:96, :], pt[0:96, 0:NM * 16], [pk], ['soldbc'])
            for c_ in range(16):
                sl = slice(c_ * 128, (c_ + 1) * 128)
                pt, pk = PSF()
                TR(pt[:, 0:NR], PK[0:NR, sl], ident_f[0:NR, 0:NR], ['mb3', 'cf'], [pk])
                CP(colbuf[:, c_, :], pt[:, 0:NR], [pk], ['colbuf'], eng=('act' if c_ % 2 else 'dve'))

        ml_gates()
        if stop == 'mlg':
            DMA(dbg_f[:, 0:16 * NR], colbuf[:, :, :].rearrange("p c q -> p (c q)"), ['colbuf'], ['dbgf'])
            DMA(dbg_f[0:96, 2048:2048 + NM * 16], soldbc[0:96, :], ['soldbc'], ['dbgf'])
            return

        def ml_head(h):
            ch0 = NL + 2 * h
            wu, wuk = load_w(w_in[l][:, SEG['mu'] + h * 192: SEG['mu'] + (h + 1) * 192], 192)
            for a_i in range(3):
                DMA(mlw[0:96, a_i, :, :], ml_w[l, a_i, h].rearrange("c i o -> i c o"), (), ['mlw'], eng='pool')
            ucb, qTb, kTb = hba, hbq, hbk
            acc = mb[2]
            if cfg.get('verbose'):
                print("count at mlh_a0", P.count)
            if stop == 'mlh_a0':
                CP(sm[0:96, 0:96], mlw[0:96, 0, 0, :], ['mlw'], ['sm_x'])
                return
            for cc in range(2):
                upad = mb[cc]
                uk = f"mb{cc}"
                ub = mbb[cc]
                ubk = f"mbb{cc}"
                MEMSET(upad[0:96, 0:3], 0.0, [uk])
                for tb in range(4):
                    sl = slice(tb * 512, (tb + 1) * 512)
                    pt, pk = proj_cm(wu, wuk, cc * 96, 96, tb)
                    CP(upad[0:96, 3 + tb * 512:3 + (tb + 1) * 512], pt[0:96, :], [pk], [uk], eng='act')
                    CP(ub[0:96, sl], upad[0:96, 3 + tb * 512:3 + (tb + 1) * 512], [uk], [ubk], eng='pool')
                pc = PPO['ml'] + (h * 2 + cc) * 5
                TS(acc[0:96, 0:T], upad[0:96, 0:T], pp[0:96, pc:pc + 1], None, ALU.mult, None, [uk, 'pp'], ['mb2'])
                for j in range(1, 4):
                    STT(acc[0:96, 0:T], upad[0:96, j:j + T], pp[0:96, pc + j:pc + j + 1], acc[0:96, 0:T],
                        ALU.mult, ALU.add, [uk, 'mb2', 'pp'], ['mb2'])
                ACT(ucb[0:96, cc, :], acc[0:96, 0:T], AF.Silu, ['mb2', 'pp'], ['hba'], bias=pp[0:96, pc + 4:pc + 5])
                if cfg.get('verbose'):
                    print("count at mlh_a1", P.count)
                if stop == 'mlh_a1':
                    return
                for tb in range(4):
                    sl = slice(tb * 512, (tb + 1) * 512)
                    pt, pk = PSF()
                    MM(pt[0:96, :], mlw[0:96, 0, cc, :], ucb[0:96, cc, sl], True, True, ['mlw', 'hba'], [pk])
                    CP(qTb[0:96, cc, sl], pt[0:96, :], [pk], ['hbq'], eng='act')
                    pt, pk = PSF()
                    MM(pt[0:96, :], mlw[0:96, 1, cc, :], ucb[0:96, cc, sl], True, True, ['mlw', 'hba'], [pk])
                    ACT(kTb[0:96, cc, sl], pt[0:96, :], AF.Identity, [pk], ['hbk'], scale=192.0 ** -0.5)
            if stop == 'mlh_a':
                return
            MEMSET(vaug[:, :, 192:193], 1.0, ['vaug'])
            for c_ in range(16):
                sl = slice(c_ * 128, (c_ + 1) * 128)
                pt, pk = PSF()
                for cc in range(2):
                    MM(pt[:, cc * 96:(cc + 1) * 96], mbb[cc][0:96, sl], mlw[0:96, 2, cc, :], True, True,
                       [f"mbb{cc}", 'mlw'], [pk])
                CP(vaug[:, c_, 0:192], pt[:, 0:192], [pk], ['vaug'], eng=('act' if c_ % 2 else 'dve'))
            wo_, wok = load_w(w_in[l][:, SEG['mo'] + h * 192: SEG['mo'] + (h + 1) * 192], 192)
            wz_, wzk = load_w(w_in[l][:, SEG['mz'] + h * 192: SEG['mz'] + (h + 1) * 192], 192)
            zsb = mb[4][:, 0:1536].bitcast(BF16).rearrange("p (c e) -> p c e", c=16)
            MEMSET(Cst[0:96, :, :], 0.0, ['Cst'])
            MEMSET(Cb[0:96, :, :], 0.0, ['Cb'])
            def colf(c_, q):
                return colbuf[:, c_, q * NM + h:q * NM + h + 1]

            def partA(c_):
                sl = slice(c_ * 128, (c_ + 1) * 128)
                b2 = c_ % 2
                kw, kwk = kwb2[b2], f"kwb{b2}"
                St_, Stk = Stb2[b2], f"Stb{b2}"
                so_, sok = sob2[b2], f"sob{b2}"
                pt, pk = PSF()
                for cc in range(2):
                    MM(pt[:, cc * 96:(cc + 1) * 96], ucb[0:96, cc, sl], mlw[0:96, 1, cc, :], True, True,
                       ['hba', 'mlw'], [pk])
                ACT(kw[:, 0:192], pt[:, 0:192], AF.Identity, [pk, 'colbuf'], [kwk], scale=colf(c_, 1))
                pS, kS = PSF()
                for cc in range(2):
                    MM(pS[:, 0:128], kTb[0:96, cc, sl], qTb[0:96, cc, sl], cc == 0, cc == 1, ['hbk', 'hbq'], [kS])
                STT(St_[:, :], pS[:, 0:128], colf(c_, 0), tri_f, ALU.mult, ALU.mult, [kS, 'colbuf', 'cf'], [Stk])
                pH, kH = PSF()
                MM(pH[:, 0:193], St_[:, :], vaug[:, c_, :], True, True, [Stk, 'vaug'], [kH])
                ACT(hsb[:, c_, :], pH[:, 0:193], AF.Identity, [kH, 'colbuf'], [f"hs{c_}"], scale=colf(c_, 4))
                pO, kO = proj_tm(wo_, wok, 0, 192, c_)
                ACT(so_[:, :], pO[:, 0:192], AF.Sigmoid, [kO], [sok])
                pZ, kZ = proj_tm(wz_, wzk, 0, 192, c_)
                ACT(zsb[:, c_, :], pZ[:, 0:192], AF.Silu, [kZ], ['mb4'])

            def partB(c_):
                sl = slice(c_ * 128, (c_ + 1) * 128)
                b2 = c_ % 2
                kw, kwk = kwb2[b2], f"kwb{b2}"
                so_, sok = sob2[b2], f"sob{b2}"
                hk = f"hs{c_}"
                if c_ > 0:
                    pI, kI = PSF()
                    for cc in range(2):
                        MM(pI[:, 0:193], qTb[0:96, cc, sl], Cb[0:96, cc, :], cc == 0, cc == 1, ['hbq', 'Cb'], [kI])
                    STT(hsb[:, c_, :], pI[:, 0:193], colf(c_, 2), hsb[:, c_, :], ALU.mult, ALU.add,
                        [kI, 'colbuf', hk], [hk])
                TT(hsb[:, c_, 0:192], hsb[:, c_, 0:192], so_[:, :], ALU.mult, [hk, sok], [hk])
                if c_ < 15:
                    for cc in range(2):
                        pU, kU = PSF()
                        MM(pU[0:96, 0:193], kw[:, cc * 96:(cc + 1) * 96], vaug[:, c_, :], True, True,
                           [kwk, 'vaug'], [kU])
                        STT(Cst[0:96, cc, :], Cst[0:96, cc, :], soldbc[0:96, h * 16 + c_:h * 16 + c_ + 1],
                            pU[0:96, 0:193], ALU.mult, ALU.add, ['Cst', 'soldbc', kU], ['Cst'])
                    CP(Cb[0:96, :, :], Cst[0:96, :, :], ['Cst'], ['Cb'], eng='act')

            HK = [f"hs{q}" for q in range(16)]
            P.add('pool', lambda e: e.memset(dn16[:, 60:61], 0.0), ['hsb'] + HK, ['hsb'] + HK)
            partA(0)
            for c_ in range(16):
                if c_ + 1 < 16:
                    partA(c_ + 1)
                partB(c_)
            if stop == 'mlh_c':
                return
            P.add('pool', lambda e: e.memset(dn16[:, 61:62], 0.0), ['hsb'] + HK, ['hsb'] + HK)
            den = hsb[:, :, 192]
            TS(dn16[:, 0:16], den, -1.0, None, ALU.mult, None, ['hsb'], ['dn16a'])
            TT(dn16[:, 0:16], dn16[:, 0:16], den, ALU.max, ['hsb', 'dn16a'], ['dn16a'])
            TT(dn16[:, 0:16], dn16[:, 0:16], colbuf[:, :, 3 * NM + h], ALU.max, ['dn16a', 'colbuf'], ['dn16a'])
            RECIP(dn16[:, 16:32], dn16[:, 0:16], ['dn16a'], ['dn16b'])
            TT(hsb[:, :, 0:192], hsb[:, :, 0:192], dn16[:, 16:32].unsqueeze(2).to_broadcast([128, 16, 192]), ALU.mult,
               ['hsb', 'dn16b'], ['hsb'])
            for c_ in range(16):
                P.add('dve', lambda e, c_=c_: e.bn_stats(st6[:, c_, :], hsb[:, c_, 0:192]), ['hsb'], [f"st6_{c_}"])
                P.add('dve', lambda e, c_=c_: e.bn_aggr(mv[:, c_, :], st6[:, c_, :]), [f"st6_{c_}"], ['mv'])
            rs = sm[:, 128:144]
            ACT(rs, mv[:, :, 1], AF.Sqrt, ['mv', 'cf'], ['sm_rs'], bias=eps5)
            RECIP(rs, rs, ['sm_rs'], ['sm_rs'])
            for c_ in range(16):
                TS(hsb[:, c_, 0:192], hsb[:, c_, 0:192], mv[:, c_, 0:1], rs[:, c_:c_ + 1], ALU.subtract, ALU.mult,
                   ['hsb', 'mv', 'sm_rs'], ['hsb'])
            ngv = ngbc[:, h * 192:(h + 1) * 192].unsqueeze(1).to_broadcast([128, 16, 192])
            TT(hsb[:, :, 0:192], hsb[:, :, 0:192], ngv, ALU.mult, ['hsb', 'ngbc'], ['hsb'])
            ytm = hbk[:, :, :].rearrange("p a t -> p (a t)")[:, 0:16 * 192].rearrange("p (c e) -> p c e", c=16)
            TT(ytm, hsb[:, :, 0:192], zsb, ALU.mult, ['hsb', 'mb4'], ['hbk'])
            yTb = hbq
            for cc in range(2):
                for half in range(2):
                    pb, pk = PSB()
                    for kk in range(8):
                        c_ = half * 8 + kk
                        TR(pb[0:96, kk * 128:(kk + 1) * 128], ytm[:, c_, cc * 96:(cc + 1) * 96], identb[:, :],
                           ['hbk', 'identb'], [pk])
                    CP(yTb[0:96, cc, half * 1024:(half + 1) * 1024], pb[0:96, :], [pk], ['hbq'],
                       eng=('act' if half else 'dve'))
                DMA(yT_d[l, ch0 + cc, 0:96, :], yTb[0:96, cc, :], ['hbq'], ['yTd'])

        for h in range(NM):
            ml_head(h)
            if stop in ('mlh1', 'mlh_a', 'mlh_c', 'mlh_a0', 'mlh_a1'):
                return
        if stop == 'ml':
            return

        kmb = A.alloc([128, 16], BF16)
        kmr = A.alloc([128, 8])
        vaug1 = mb[0][:, 0:1032].bitcast(BF16).rearrange("p (c e) -> p c e", c=16)
        MSET = [
            dict(qkb=hba, qk='hba', zs=mbb[0], zk='mbb0', va=vaug[:, :, 0:129], vk='vaug'),
            dict(qkb=hbk, qk='hbk', zs=mbb[1], zk='mbb1', va=vaug1, vk='mb0'),
        ]

        def proj_gen(h, S, done=None):
            qb = S['qkb'][:, 0, :]
            kb = S['qkb'][:, 1, :]
            for which, dst in (('oq', qb), ('ok', kb)):
                w_, wk_ = load_w(w_in[l][:, SEG[which] + h * 128: SEG[which] + (h + 1) * 128], 128)
                for tb in range(4):
                    sl = slice(tb * 512, (tb + 1) * 512)
                    pt, pk = proj_cm(w_, wk_, 0, 128, tb)
                    CP(dst[:, sl], pt[:, :], [pk], [S['qk']], eng='act')
                    yield
            wz_, kz = load_w(w_in[l][:, SEG['oz'] + h * 128: SEG['oz'] + (h + 1) * 128], 128)
            for tb in range(4):
                sl = slice(tb * 512, (tb + 1) * 512)
                pt, pk = proj_cm(wz_, kz, 0, 128, tb)
                ACT(S['zs'][:, sl], pt[:, :], AF.Silu, [pk], [S['zk']])
                yield
            wv_, kv = load_w(w_in[l][:, SEG['ov'] + h * 128: SEG['ov'] + (h + 1) * 128], 128)
            MEMSET(S['va'][:, :, 128:129], 1.0, [S['vk']])
            for st in range(16):
                pt, pk = proj_tm(wv_, kv, 0, 128, st)
                CP(S['va'][:, st, 0:128], pt[:, 0:128], [pk], [S['vk']], eng='pool' if False else 'act')
                yield
            if done is not None:
                done()

        def mo_main(h, S, step):
            chn = NL + 2 * NM + h
            qb = S['qkb'][:, 0, :]
            kb = S['qkb'][:, 1, :]
            zs = S['zs']
            va = S['va']
            qk, zk, vk = S['qk'], S['zk'], S['vk']
            P.add('dve', lambda e: e.tensor_reduce(km[:, 0:8], kb.rearrange("p (b s) -> p b s", s=256),
                                                   AX.X, ALU.add), [qk], ['km'])
            TS(kmb[:, 0:8], km[:, 0:8], 1.0 / 256, None, ALU.mult, None, ['km'], ['kmb'])
            STT(kmr[:, 0:8], km[:, 0:8], 1.0 / 256, kmb[:, 0:8], ALU.mult, ALU.subtract, ['km', 'kmb'], ['kmr'])
            CP(kmb[:, 8:16], kmr[:, 0:8], ['kmr'], ['kmb'])
            et = etab[:, h * 128:(h + 1) * 128].rearrange("p (q s) -> p q s", q=16)
            for qt in range(16):
                bq = qt // 2
                if bq == 0:
                    continue
                if bq >= 4:
                    pg, kg = PSF()
                    MM(pg[:, 0:8], qb[:, qt * 128:(qt + 1) * 128], kmb[:, 0:8], True, False, [qk, 'kmb'], [kg])
                    MM(pg[:, 0:8], qb[:, qt * 128:(qt + 1) * 128], kmb[:, 8:16], False, True, [qk, 'kmb'], [kg])
                    TT(gm[:, :], pg[:, 0:8], negmask[:, bq * 8:(bq + 1) * 8], ALU.add, [kg, 'cf'], ['gm'])
                    P.add('dve', lambda e: e.max(top8[:, :], gm[:, :]), ['gm'], ['top8'])
                    TS(sel[:, :], gm[:, :], top8[:, 2:3], None, ALU.is_ge, None, ['gm', 'top8'], ['sel'])
                    TT(Fq[:, qt, :], et[:, qt, :], sel[:, :], ALU.mult, ['sel', 'cf'], ['Fq'])
                else:
                    CP(Fq[:, qt, :], et[:, qt, :], ['cf'], ['Fq'])
            acc = hsb[:, :, 0:129]
            P.add('pool', lambda e: e.memset(dn[:, 2:3], 1.0), ['hsb'] + [f"acc{q}" for q in range(16)],
                  ['dn2', 'hsb'] + [f"acc{q}" for q in range(16)])
            sc = 128.0 ** -0.5
            bias = slopecol[:, 2 * h + 1:2 * h + 2]
            biasp = [slopecol[:, 2 * h:2 * h + 1], slopecol[:, 2 * h + 1:2 * h + 2]]
            items = []
            npast = 0
            for bq in range(8):
                items.append(('d', bq, 0, 0, 0))
                for j in range(bq):
                    items.append(('p', bq, j, 0, npast % 3))
                    npast += 1

            def s1(it):
                kind, bq, j, kt, pi = it
                qs = slice(bq * 256, (bq + 1) * 256)
                if kind == 'd':
                    for kt2 in range(2):
                        st = 2 * bq + kt2
                        pd = (bq % 2) * 2 + kt2
                        pS, kS = PSF()
                        MM(pS[:, 0:256], kb[:, st * 128:(st + 1) * 128], qb[:, qs], True, True, [qk], [kS])
                        ACT(Ptd4[pd][:, :], pS[:, 0:256], AF.Exp, [kS, 'cf'], [f"Ptd{pd}"], scale=sc, bias=bias)
                        TT(Ptd4[pd][:, :], Ptd4[pd][:, :], gdiag[:, (h * 2 + kt2) * 256:(h * 2 + kt2 + 1) * 256],
                           ALU.mult, [f"Ptd{pd}", 'gdiag'], [f"Ptd{pd}"], eng='pool')
                else:
                    for kt2 in range(2):
                        st = 2 * j + kt2
                        pp_ = pi * 2 + kt2
                        pS, kS = PSF()
                        MM(pS[:, 0:256], kb[:, st * 128:(st + 1) * 128], qb[:, qs], True, True, [qk], [kS])
                        ACT(Ptb[pp_][:, :], pS[:, 0:256], AF.Exp, [kS, 'cf'], [f"Ptb{pp_}"], scale=sc, bias=biasp[kt2])

            def s2(it):
                kind, bq, j, kt, pi = it
                if kind == 'd':
                    p0, p1 = (bq % 2) * 2, (bq % 2) * 2 + 1
                    pO, kO = PSF()
                    MM(pO[:, 0:129], Ptd4[p0][:, 0:128], va[:, 2 * bq, 0:129], True, True, [f"Ptd{p0}", vk], [kO])
                    CP(acc[:, 2 * bq, :], pO[:, 0:129], [kO], [f"acc{2 * bq}"], eng='act')
                    pO, kO = PSF()
                    MM(pO[:, 0:129], Ptd4[p0][:, 128:256], va[:, 2 * bq, 0:129], True, False, [f"Ptd{p0}", vk], [kO])
                    MM(pO[:, 0:129], Ptd4[p1][:, 128:256], va[:, 2 * bq + 1, 0:129], False, True, [f"Ptd{p1}", vk], [kO])
                    CP(acc[:, 2 * bq + 1, :], pO[:, 0:129], [kO], [f"acc{2 * bq + 1}"], eng='act')
                else:
                    for qh in range(2):
                        qt = 2 * bq + qh
                        pO, kO = PSF()
                        for kt2 in range(2):
                            pp_ = pi * 2 + kt2
                            MM(pO[:, 0:129], Ptb[pp_][:, qh * 128:(qh + 1) * 128], va[:, 2 * j + kt2, 0:129],
                               kt2 == 0, kt2 == 1, [f"Ptb{pp_}", vk], [kO])
                        STT(acc[:, qt, :], pO[:, 0:129], Fq[:, qt, j:j + 1], acc[:, qt, :], ALU.mult, ALU.add,
                            [kO, 'Fq', f"acc{qt}"], [f"acc{qt}"])

            LA = 2
            for i in range(min(LA, len(items))):
                s1(items[i])
            for i in range(len(items)):
                if i + LA < len(items):
                    s1(items[i + LA])
                s2(items[i])
                step()
            yb = hbq[:, 0, :]
            for qt in range(16):
                RECIP(dn[:, 2:3], acc[:, qt, 128:129], [f"acc{qt}"], ['dn2'])
                TS(ofb[:, :], acc[:, qt, 0:128], dn[:, 2:3], None, ALU.mult, None, [f"acc{qt}", 'dn2'], ['ofb'])
                pT, kT = PSF()
                TR(pT[:, 0:128], ofb[:, :], ident_f, ['ofb', 'cf'], [kT])
                TT(yb[:, qt * 128:(qt + 1) * 128], pT[:, 0:128], zs[:, qt * 128:(qt + 1) * 128], ALU.mult,
                   [kT, zk], ['hbq'])
                step()
            DMA(yT_d[l, chn, :, :], yb, ['hbq'], ['yTd'])

        def prefetch_wout():
            save = A.off
            A.off = persist_mark
            wv = A.alloc([128, NCH, D], BF16)
            A.off = save
            for ci in range(16):
                r0, K = chunks[ci]
                DMA(wv[0:K, ci, :], w_out[l, r0:r0 + K, :], (), ['hT0', 'hT1', 'hT2', 'hT3', 'wout'], eng='pool')

        use_pf = cfg.get('pf', True) and stop in ('all', 'out')
        g0 = proj_gen(0, MSET[0], done=(prefetch_wout if (NO == 1 and use_pf) else None))
        for _ in g0:
            pass
        for h in range(NO):
            gn = None
            if h + 1 < NO:
                gn = proj_gen(h + 1, MSET[(h + 1) % 2],
                              done=(prefetch_wout if (h + 1 == NO - 1 and use_pf) else None))

            def step(gn=gn):
                if gn is not None:
                    next(gn, None)

            mo_main(h, MSET[h % 2], step)
            if gn is not None:
                for _ in gn:
                    pass

    chunks = []
    for g in range(NL):
        chunks.append((g * 128, 128))
    for h in range(NM):
        for cc in range(2):
            chunks.append((512 + h * 192 + cc * 96, 96))
    for h in range(NO):
        chunks.append((1280 + h * 128, 128))

    def out_phase(l, xsrc, xdst, final):
        A.off = persist_mark
        P.barrier()
        wout = A.alloc([128, NCH, D], BF16)
        wq = A.alloc([128, KD, 512], BF16)
        wo = A.alloc([128, 4, D], BF16)
        hmT = A.alloc([128, KD, MEM], BF16)
        kT = A.alloc([128, 4, MEM], BF16)
        vx = A.alloc([128, 2, 4, 129], BF16)
        xblk = A.alloc([128, 2, D])
        yblk = A.alloc([128, NCH, 256], BF16)
        hx = A.alloc([128, 2, D], BF16)
        hxT = A.alloc([128, KD, 256], BF16)
        qT = A.alloc([128, 4, 256], BF16)
        PT = A.alloc([128, 2, 2, 256], BF16)
        oh = A.alloc([128, 2, 512], BF16)
        ohT = A.alloc([128, 4, 256], BF16)
        gbx = A.alloc([128, D])
        gbf = A.alloc([128, D])
        rd = A.alloc([128, 2])
        for ci, (r0, K) in enumerate(chunks):
            if ci < 16 and cfg.get('pf', True):
                continue
            DMA(wout[0:K, ci, :], w_out[l, r0:r0 + K, :], (), ['wout'], eng='pool')
        DMA(wq[:, :, :], xa_wq[l].rearrange("(k p) c -> p k c", p=128), (), ['wq'], eng='pool')
        DMA(wo[:, :, :], xa_wo[l].rearrange("(h p) n -> p h n", p=128), (), ['wo'], eng='pool')
        DMA(gbx[:, :], gains[l, 2].partition_broadcast(128), (), ['gbx'])
        for i in range(2):
            DMA(xblk[:, i, :], mem_in[i * 128:(i + 1) * 128, :], (), [f"xblk{i}"])
            norm_tile(xblk[:, i, :], f"xblk{i}", gbx[:, :], 'gbx', hx[:, i, :], f"hx{i}", hx[:, i, :], f"hx{i}")
            transpose_16(hx[:, i, :], f"hx{i}", lambda half, i=i: hmT[:, half * 8:(half + 1) * 8, i * 128:(i + 1) * 128],
                         'hmT')
        wkv = xblk[:, :, :].rearrange("p a d -> p (a d)").bitcast(BF16).rearrange("p (k c) -> p k c", k=KD)
        DMA(wkv, xa_wkv[l][:, 0:512].rearrange("(k p) c -> p k c", p=128), (), ['xblk0', 'xblk1'], eng='pool')
        for h in range(4):
            pt, pk = PSF()
            for k in range(KD):
                MM(pt[:, 0:MEM], wkv[:, k, h * 128:(h + 1) * 128], hmT[:, k, :], k == 0, k == KD - 1, ['xblk0', 'xblk1', 'hmT'], [pk])
            CP(kT[:, h, :], pt[:, 0:MEM], [pk], ['kT'], eng='act')
        DMA(wkv, xa_wkv[l][:, 512:1024].rearrange("(k p) c -> p k c", p=128), (), ['xblk0', 'xblk1'], eng='pool')
        MEMSET(vx[:, :, :, 128:129], 1.0, ['vx'])
        for mt in range(2):
            pt, pk = PSF()
            for k in range(KD):
                MM(pt[:, 0:512], hmT[:, k, mt * 128:(mt + 1) * 128], wkv[:, k, :], k == 0, k == KD - 1, ['xblk0', 'xblk1', 'hmT'], [pk])
            CP(vx[:, mt, :, 0:128], pt[:, 0:512].rearrange("p (h e) -> p h e", h=4), [pk], ['vx'])
        DMA(gbx[:, :], gains[l, 1].partition_broadcast(128), ['gbx'], ['gbx'])
        if final:
            DMA(gbf[:, :], final_g.partition_broadcast(128), (), ['gbf'])
        sc = 128.0 ** -0.5
        XK = ['xblk0', 'xblk1']
        c1, c2 = NL, NL + 2 * NM

        def load_y(tb):
            rows = slice(tb * 256, (tb + 1) * 256)
            DMA(yblk[:, 0:c1, :], yT_d[l, 0:c1, :, rows].rearrange("c p t -> p c t"), ['yTd'], ['yblk'])
            DMA(yblk[0:96, c1:c2, :], yT_d[l, c1:c2, 0:96, rows].rearrange("c p t -> p c t"), ['yTd'], ['yblk'])
            DMA(yblk[:, c2:NCH, :], yT_d[l, c2:NCH, :, rows].rearrange("c p t -> p c t"), ['yTd'], ['yblk'])

        load_y(0)
        for tb in range(8):
            rows = slice(tb * 256, (tb + 1) * 256)
            for tt in range(2):
                DMA(xblk[:, tt, :], xsrc[tb * 256 + tt * 128: tb * 256 + (tt + 1) * 128, :], (), [XK[tt]])
            for tt in range(2):
                for nb in range(4):
                    ns = slice(nb * 512, (nb + 1) * 512)
                    pt, pk = PSF()
                    for ci, (r0, K) in enumerate(chunks):
                        MM(pt[:, :], yblk[0:K, ci, tt * 128:(tt + 1) * 128], wout[0:K, ci, ns], ci == 0, ci == NCH - 1,
                           ['yblk', 'wout'], [pk])
                    TT(xblk[:, tt, ns], xblk[:, tt, ns], pt[:, :], ALU.add, [XK[tt], pk], [XK[tt]])
            if tb + 1 < 8:
                load_y(tb + 1)
            for tt in range(2):
                norm_tile(xblk[:, tt, :], XK[tt], gbx[:, :], 'gbx', hx[:, tt, :], f"hx{tt}", hx[:, tt, :], f"hx{tt}")
                transpose_16(hx[:, tt, :], f"hx{tt}",
                             lambda half, tt=tt: hxT[:, half * 8:(half + 1) * 8, tt * 128:(tt + 1) * 128], 'hxT')
            for h in range(4):
                pt, pk = PSF()
                for k in range(KD):
                    MM(pt[:, 0:256], wq[:, k, h * 128:(h + 1) * 128], hxT[:, k, :], k == 0, k == KD - 1, ['wq', 'hxT'], [pk])
                CP(qT[:, h, :], pt[:, 0:256], [pk], [f"qT{h}"], eng='act')
            for h in range(4):
                for mt in range(2):
                    pS, kS = PSF()
                    MM(pS[:, 0:256], kT[:, h, mt * 128:(mt + 1) * 128], qT[:, h, :], True, True, ['kT', f"qT{h}"], [kS])
                    ACT(PT[:, h % 2, mt, :], pS[:, 0:256], AF.Exp, [kS], [f"PT{h % 2}"], scale=sc)
                for tt in range(2):
                    pO, kO = PSF()
                    for mt in range(2):
                        MM(pO[:, 0:129], PT[:, h % 2, mt, tt * 128:(tt + 1) * 128], vx[:, mt, h, :], mt == 0, mt == 1,
                           [f"PT{h % 2}", 'vx'], [kO])
                    RECIP(rd[:, tt:tt + 1], pO[:, 128:129], [kO], [f"rd{tt}"])
                    TS(oh[:, tt, h * 128:(h + 1) * 128], pO[:, 0:128], rd[:, tt:tt + 1], None, ALU.mult, None,
                       [kO, f"rd{tt}"], [f"oh{tt}"])
            for tt in range(2):
                pb, pk = PSB()
                for h in range(4):
                    TR(pb[:, h * 128:(h + 1) * 128], oh[:, tt, h * 128:(h + 1) * 128], identb[:, :], [f"oh{tt}", 'identb'], [pk])
                CP(ohT[:, :, tt * 128:(tt + 1) * 128], pb[:, 0:512].rearrange("p (h t) -> p h t", h=4), [pk], [f"ohT{tt}"],
                   eng=('act' if tt else 'dve'))
            for tt in range(2):
                for nb in range(4):
                    ns = slice(nb * 512, (nb + 1) * 512)
                    pt, pk = PSF()
                    for h in range(4):
                        MM(pt[:, :], ohT[:, h, tt * 128:(tt + 1) * 128], wo[:, h, ns], h == 0, h == 3, [f"ohT{tt}", 'wo'], [pk])
                    TT(xblk[:, tt, ns], xblk[:, tt, ns], pt[:, :], ALU.add, [XK[tt], pk], [XK[tt]])
                if final:
                    norm_tile(xblk[:, tt, :], XK[tt], gbf[:, :], 'gbf', xblk[:, tt, :], XK[tt], hx[:, tt, :], f"hx{tt}")
                DMA(xdst[tb * 256 + tt * 128: tb * 256 + (tt + 1) * 128, :], xblk[:, tt, :], [XK[tt]], ['xdst'])

    src = x_in
    for l in range(nlayers):
        mix_phase(l, src)
        if stop in ('A', 'lru', 'mlg', 'mlh1', 'mlh_a', 'mlh_c', 'mlh_a0', 'mlh_a1', 'ml', 'mo'):
            break
        last = (l == nlayers - 1)
        out_phase(l, src, out_d if last else xs0, final=(last and nlayers == L))
        src = xs0
        if stop == 'out':
            break
    P.emit()
    return nc, es


def alibi_slopes(n):
    def pow2(m):
        start = 2.0 ** (-8.0 / m)
        return [start ** (i + 1) for i in range(m)]
    if math.log2(n).is_integer():
        s = pow2(n)
    else:
        c = 2 ** int(math.floor(math.log2(n)))
        s = pow2(c) + pow2(2 * c)[0::2][:n - c]
    return np.array(s, dtype=np.float64)


def make_consts(mo_heads):
    NO = len(mo_heads)
    CO, NCF = const_layout(NO)
    cf = np.zeros((128, NCF), np.float32)
    cf[:, CO['ident']:CO['ident'] + 128] = np.eye(128, dtype=np.float32)
    s = np.arange(128)[:, None]
    j = np.arange(128)[None, :]
    cf[:, CO['tri']:CO['tri'] + 128] = (s <= j).astype(np.float32)
    nm = np.zeros((8, 8), np.float32)
    for bq in range(8):
        nm[bq, bq:] = NEG
    cf[:, CO['negmask']:CO['negmask'] + 64] = nm.reshape(1, 64)
    slopes = alibi_slopes(6)
    p = np.arange(128, dtype=np.float64)
    gd = np.zeros((128, NO, 2, 256), np.float64)
    for hi, hg in enumerate(mo_heads):
        sl = slopes[hg]
        cf[:, CO['slope'] + 2 * hi] = (sl * (p - 255.0)).astype(np.float32)
        cf[:, CO['slope'] + 2 * hi + 1] = (sl * (p - 127.0)).astype(np.float32)
        et = np.zeros((128, 16, 8), np.float64)
        for qt in range(16):
            bq = qt // 2
            for jb in range(bq):
                t = qt * 128 + p
                sref = jb * 256 + 255
                et[:, qt, jb] = np.exp(-sl * (t - sref))
        cf[:, CO['etab'] + hi * 128: CO['etab'] + (hi + 1) * 128] = et.reshape(128, 128).astype(np.float32)
        for kt in range(2):
            sabs = kt * 128 + p[:, None]
            sref = kt * 128 + 127
            t = np.arange(256, dtype=np.float64)[None, :]
            g = np.exp(-sl * (t - sref)) * (sabs <= t)
            gd[:, hi, kt, :] = g
    cf[:, CO['misc'] + 0] = 1e-6
    cf[:, CO['misc'] + 1] = 1e-5
    cf[:, CO['misc'] + 2] = 1.0
    gd = np.minimum(gd, 3.0e38).astype(np.float32).reshape(128, NO * 512).astype(ml_dtypes.bfloat16)
    return cf, gd


def pack_core(inp, b, lru_blocks, ml_heads, mo_heads):
    NL, NM, NO = len(lru_blocks), len(ml_heads), len(mo_heads)
    PPO, NPP = pp_layout(NL, NM)
    w_in = inp['w_in']
    o_lx, o_lz, o_mu, o_mo, o_mz, o_mi, o_mf, o_oq, o_ok, o_ov, o_oz = (
        0, 512, 1024, 1792, 2560, 3328, 3332, 3336, 4104, 4872, 5640)
    cols = []
    for base in (o_lx, o_lz):
        for g in lru_blocks:
            cols.append(np.arange(base + g * 128, base + (g + 1) * 128))
    for base in (o_mu, o_mo, o_mz):
        for h in ml_heads:
            cols.append(np.arange(base + h * 192, base + (h + 1) * 192))
    for base in (o_mi, o_mf):
        cols.append(np.array([base + h for h in ml_heads]))
    for base in (o_oq, o_ok, o_ov, o_oz):
        for h in mo_heads:
            cols.append(np.arange(base + h * 128, base + (h + 1) * 128))
    cols = np.concatenate(cols)
    full = (len(cols) == w_in.shape[2]) and np.array_equal(cols, np.arange(w_in.shape[2]))
    w_in_c = w_in if full else np.ascontiguousarray(w_in[:, :, cols])
    pp = np.zeros((L, 128, NPP), np.float32)
    for l in range(L):
        for gi, g in enumerate(lru_blocks):
            sl = slice(g * 128, (g + 1) * 128)
            o = PPO['lru'] + gi * 8
            pp[l, :, o:o + 4] = inp['lru_conv_w'][l][:, sl].T
            pp[l, :, o + 4] = inp['lru_conv_b'][l][sl]
            pp[l, :, o + 5] = inp['lru_ba'][l][sl]
            pp[l, :, o + 6] = inp['lru_bx'][l][sl]
            pp[l, :, o + 7] = inp['lru_lambda'][l][sl]
        for hi, h in enumerate(ml_heads):
            for cc in range(2):
                sl = slice(h * 192 + cc * 96, h * 192 + (cc + 1) * 96)
                o = PPO['ml'] + (hi * 2 + cc) * 5
                pp[l, 0:96, o:o + 4] = inp['ml_conv_w'][l][:, sl].T
                pp[l, 0:96, o + 4] = inp['ml_conv_b'][l][sl]
            pp[l, hi, PPO['gb']] = inp['ml_bi'][l][h]
            pp[l, hi, PPO['gb'] + 1] = -inp['ml_bf'][l][h]
    lru_w = np.stack([inp['lru_wa'][:, lru_blocks], inp['lru_wx'][:, lru_blocks]], axis=1)
    ml_w = np.zeros((L, 3, NM, 2, 96, 96), np.float32)
    for ai, nm_ in enumerate(('ml_wq', 'ml_wk', 'ml_wv')):
        w = inp[nm_]
        for hi, h in enumerate(ml_heads):
            for cc in range(2):
                for bl in range(24):
                    gidx = h * 48 + cc * 24 + bl
                    ml_w[:, ai, hi, cc, bl * 4:(bl + 1) * 4, bl * 4:(bl + 1) * 4] = w[:, gidx]
    ml_ng = np.concatenate([inp['ml_norm_g'][:, h * 192:(h + 1) * 192] for h in ml_heads], axis=1)
    gains = np.stack([inp['mix_norm_g'], inp['xa_norm_g'], inp['mem_norm_g']], axis=1)
    return {
        "x": np.ascontiguousarray(inp['x'][b]), "mem": np.ascontiguousarray(inp['mem'][b]),
        "w_in": w_in_c, "w_out": inp['w_out'], "gains": np.ascontiguousarray(gains),
        "final_g": inp['final_norm_g'], "pp": pp, "lru_w": np.ascontiguousarray(lru_w),
        "ml_w": ml_w, "ml_ng": np.ascontiguousarray(ml_ng), "xa_wq": inp['xa_wq'], "xa_wkv": inp['xa_wkv'],
        "xa_wo": inp['xa_wo'],
    }


def kernel(**inputs):
    inp = {k: np.asarray(v) for k, v in inputs.items()}
    cfg = dict(NL=4, NM=4, NO=6)
    nc, es = build(cfg)
    cf, gd = make_consts(list(range(6)))
    in_maps = []
    for c in range(8):
        m = pack_core(inp, c // 2, [0, 1, 2, 3], [0, 1, 2, 3], [0, 1, 2, 3, 4, 5])
        m["cst_f"] = cf
        m["cst_b"] = gd
        in_maps.append(m)
    res = run_bass_kernel_spmd(nc, in_maps, core_ids=list(range(8)))
    out = np.stack([res.results[2 * b]["out"] for b in range(4)], axis=0)
    return out.astype(np.float32)
```
